# Optimizing a Trainium2 kernel written in Bass

```python
import math
import jax
import jax.numpy as jnp
from jax import lax
import numpy as np

D_MODEL = 1024
BATCH = 8
SEQ = 2048
DEPTH = 2

GRID_W = 64
CTX_LEN = 256
D_MIX = D_MODEL

ML_HEADS = 4
ML_DH = 64
ML_W = ML_HEADS * ML_DH
ML_CHUNK = 64
ML_CONV = 3
ML_M_INIT = -1e30
ML_COLS = 4 * ML_W + 4 * ML_HEADS

RW_HEADS = 6
RW_DH = 64
RW_W = RW_HEADS * RW_DH
RW_W_LORA = 64
RW_A_LORA = 64
RW_G_LORA = 128
RW_DECAY_SCALE = math.exp(-0.5)
RW_GN_EPS = 64e-5
RW_COLS = 3 * RW_W + 2 * RW_W_LORA + 2 * RW_A_LORA + RW_G_LORA

MLA_HEADS = 6
MLA_NOPE = 64
MLA_ROPE = 32
MLA_V = 64
MLA_Q_RANK = 384
MLA_KV_RANK = 256
MLA_W = MLA_HEADS * MLA_V
MLA_Q_BLOCK = 128
MLA_COLS = MLA_Q_RANK + MLA_KV_RANK + MLA_ROPE
MLA_SCALE = (MLA_NOPE + MLA_ROPE) ** -0.5
ROPE_AXIS_FREQS = MLA_ROPE // 4
ROPE_THETA = 10000.0

N_IN = ML_COLS + RW_COLS + MLA_COLS

PEER_HEADS = 8
PEER_NKEYS = 128
PEER_EXPERTS = PEER_NKEYS * PEER_NKEYS
PEER_DK = 128
PEER_TOPK = 16
PEER_CHUNK = 128

DEEPNORM_ALPHA = (2 * DEPTH) ** 0.25
DEEPNORM_BETA = (8 * DEPTH) ** -0.25
LN_EPS = 1e-6

kernel_name = 'hybrid_dit_mlstm_rwkv7_mla_peer'


def _ln(x, eps=LN_EPS):
    xf = x.astype(jnp.float32)
    mu = jnp.mean(xf, axis=-1, keepdims=True)
    var = jnp.mean(jnp.square(xf - mu), axis=-1, keepdims=True)
    return (xf - mu) * lax.rsqrt(var + eps)


def _ln_affine(x, g, b):
    return (_ln(x) * g + b).astype(x.dtype)


def _rms(x, g):
    xf = x.astype(jnp.float32)
    return (xf * lax.rsqrt(jnp.mean(jnp.square(xf), axis=-1, keepdims=True) + LN_EPS) * g).astype(x.dtype)


def _modulate(x, shift, scale):
    return (_ln(x) * (1.0 + scale) + shift).astype(x.dtype)


def _split(a, sizes):
    return jnp.split(a, [int(s) for s in np.cumsum(sizes)[:-1]], axis=-1)


def _heads(a, n_heads):
    b, t, _ = a.shape
    return a.reshape(b, t, n_heads, -1).transpose(0, 2, 1, 3)


def _merge_heads(a):
    b, h, t, d = a.shape
    return a.transpose(0, 2, 1, 3).reshape(b, t, h * d)


def _shift3(x):
    xp = jnp.pad(x, ((0, 0), (1, 1), (0, 0)))
    return xp[:, :-2], xp[:, 2:]


def _flip_if(a, rev, axis):
    return jnp.flip(a, axis=axis) if rev else a


def _mlstm_scan(q, k, v, li, lf, state):
    b, h, t, d = q.shape
    nc = t // ML_CHUNK

    def chunks(a):
        return jnp.moveaxis(a.reshape((b, h, nc, ML_CHUNK) + a.shape[3:]), 2, 0)

    causal = jnp.tril(jnp.ones((ML_CHUNK, ML_CHUNK), dtype=bool))

    def step(carry, inp):
        C, n, m = carry
        qc, kc, vc, ic, fc = inp
        bcum = jnp.cumsum(fc, axis=-1)
        a_inter = bcum + m[..., None]
        dmat = jnp.where(causal, bcum[..., :, None] - bcum[..., None, :] + ic[..., None, :], -jnp.inf)
        m_t = jnp.maximum(a_inter, jnp.max(dmat, axis=-1))
        w_inter = jnp.exp(a_inter - m_t)
        s = jnp.einsum('bhtd,bhsd->bhts', qc, kc) * jnp.exp(dmat - m_t[..., None])
        num = w_inter[..., None] * jnp.einsum('bhed,bhtd->bhte', C, qc) + jnp.einsum('bhts,bhse->bhte', s, vc)
        den = w_inter * jnp.einsum('bhd,bhtd->bht', n, qc) + jnp.sum(s, axis=-1)
        h_out = num / jnp.maximum(jnp.abs(den), jnp.exp(-m_t))[..., None]
        b_end = bcum[..., -1]
        g = b_end[..., None] - bcum + ic
        m_new = jnp.maximum(b_end + m, jnp.max(g, axis=-1))
        decay = jnp.exp(b_end + m - m_new)
        wk = jnp.exp(g - m_new[..., None])
        C = decay[..., None, None] * C + jnp.einsum('bhs,bhse,bhsd->bhed', wk, vc, kc)
        n = decay[..., None] * n + jnp.einsum('bhs,bhsd->bhd', wk, kc)
        return (C, n, m_new), h_out

    state, hs = lax.scan(step, state, tuple(chunks(a) for a in (q, k, v, li, lf)))
    return jnp.moveaxis(hs, 0, 2).reshape(b, h, t, d), state


def _mlstm_prep(p, conv_w, conv_b, i_bias, f_bias):
    b, t, _ = p.shape
    qk, v, o, gates = _split(p, [2 * ML_W, ML_W, ML_W, 4 * ML_HEADS])
    prev, nxt = _shift3(qk)
    qk = jax.nn.silu(prev * conv_w[0] + qk * conv_w[1] + nxt * conv_w[2] + conv_b)
    q, k = jnp.split(qk, 2, axis=-1)
    q = _heads(q, ML_HEADS).astype(jnp.float32) * ML_DH ** -0.5
    k = _heads(k, ML_HEADS).astype(jnp.float32)
    v = _heads(v, ML_HEADS).astype(jnp.float32)
    gates = gates.astype(jnp.float32).reshape(b, t, 2, 2, ML_HEADS).transpose(2, 3, 0, 4, 1)
    li = gates[:, 0] + i_bias[:, None, :, None]
    lf = jax.nn.log_sigmoid(gates[:, 1] + f_bias[:, None, :, None])
    return q, k, v, o, li, lf


def _mlstm_out(h, o, norm_g):
    return (jax.nn.sigmoid(o.astype(jnp.float32)) * _merge_heads(_ln(h)) * norm_g).astype(o.dtype)


def _mlstm_mixer(p_ctx, p_lat, ml, need_ctx):
    conv_w, conv_b, i_bias, f_bias, norm_g = ml
    qc, kc, vc, oc, lic, lfc = _mlstm_prep(p_ctx, conv_w, conv_b, i_bias, f_bias)
    ql, kl, vl, ol, lil, lfl = _mlstm_prep(p_lat, conv_w, conv_b, i_bias, f_bias)
    b = p_lat.shape[0]
    init = (jnp.zeros((b, ML_HEADS, ML_DH, ML_DH), jnp.float32),
            jnp.zeros((b, ML_HEADS, ML_DH), jnp.float32),
            jnp.full((b, ML_HEADS), ML_M_INIT, jnp.float32))
    h_ctx = 0.0
    h_lat = 0.0
    for dr in range(2):
        rev = dr == 1
        f = lambda a: _flip_if(a, rev, 2)
        hc, st = _mlstm_scan(f(qc), f(kc), f(vc), f(lic[dr]), f(lfc[dr]), init)
        hl, _ = _mlstm_scan(f(ql), f(kl), f(vl), f(lil[dr]), f(lfl[dr]), st)
        h_lat = h_lat + f(hl)
        if need_ctx:
            h_ctx = h_ctx + f(hc)
    y_ctx = _mlstm_out(h_ctx, oc, norm_g) if need_ctx else None
    return y_ctx, _mlstm_out(h_lat, ol, norm_g)


def _rwkv_prep(p, mu, w0, w_up, a0, a_up, g_up, k_k, k_a):
    b, t, _ = p.shape
    prev, nxt = _shift3(p)
    p = p + mu[0] * (prev - p) + mu[1] * (nxt - p)
    r, k, v, wd, ad, gd = _split(p, [RW_W, RW_W, RW_W, 2 * RW_W_LORA, 2 * RW_A_LORA, RW_G_LORA])
    lora = lambda z, up: jnp.einsum('btdr,drc->btdc', z.reshape(b, t, 2, -1), up)
    w = jnp.exp(-RW_DECAY_SCALE * jax.nn.sigmoid((w0 + lora(jnp.tanh(wd), w_up)).astype(jnp.float32)))
    a = jax.nn.sigmoid((a0 + lora(ad, a_up)).astype(jnp.float32))
    g = jax.nn.sigmoid(gd) @ g_up
    k = k.astype(jnp.float32)
    kk = (k * k_k).reshape(b, t, RW_HEADS, RW_DH)
    kh = (kk * lax.rsqrt(jnp.sum(kk * kk, axis=-1, keepdims=True) + 1e-12)).reshape(b, t, RW_W)
    kt = k[:, :, None] * (1.0 + (a - 1.0) * k_a)
    ab = kh[:, :, None] * a
    tm1 = lambda z: z.reshape(b, t, RW_HEADS, RW_DH).transpose(1, 0, 2, 3).astype(jnp.float32)
    tm2 = lambda z: z.reshape(b, t, 2, RW_HEADS, RW_DH).transpose(2, 1, 0, 3, 4)
    return tm1(r), tm1(kh), tm1(v), tm2(w), tm2(ab), tm2(kt), g


def _rwkv_scan(r, w, kh, ab, v, kt, s0):
    def step(S, inp):
        r_t, w_t, kh_t, ab_t, v_t, kt_t = inp
        sa = jnp.einsum('bhvk,bhk->bhv', S, kh_t)
        S = S * w_t[..., None, :] - sa[..., None] * ab_t[..., None, :] + v_t[..., None] * kt_t[..., None, :]
        return S, jnp.einsum('bhvk,bhk->bhv', S, r_t)
    return lax.scan(step, s0, (r, w, kh, ab, v, kt))


def _rwkv_out(y, bonus, g, gn_g, gn_b):
    t, b = y.shape[:2]
    merge = lambda z: z.transpose(1, 0, 2, 3).reshape(b, t, RW_W)
    z = merge(_ln(y, RW_GN_EPS)) * gn_g + gn_b + merge(bonus)
    return (z * g).astype(g.dtype)


def _rwkv_mixer(p_ctx, p_lat, rw, need_ctx):
    mu, w0, w_up, a0, a_up, g_up, k_k, k_a, r_k, gn_g, gn_b = rw
    sc = _rwkv_prep(p_ctx, mu, w0, w_up, a0, a_up, g_up, k_k, k_a)
    sl = _rwkv_prep(p_lat, mu, w0, w_up, a0, a_up, g_up, k_k, k_a)
    rho = r_k.reshape(RW_HEADS, RW_DH).astype(jnp.float32)
    s0 = jnp.zeros((p_lat.shape[0], RW_HEADS, RW_DH, RW_DH), jnp.float32)

    def run(s, dr, s_init):
        r, kh, v, w, ab, kt, _ = s
        rev = dr == 1
        f = lambda a: _flip_if(a, rev, 0)
        s_fin, y = _rwkv_scan(f(r), f(w[dr]), f(kh), f(ab[dr]), f(v), f(kt[dr]), s_init)
        bonus = jnp.sum(r * rho * kt[dr], axis=-1, keepdims=True) * v
        return s_fin, f(y), bonus

    y_c = b_c = y_l = b_l = 0.0
    for dr in range(2):
        s_c, yc, bc = run(sc, dr, s0)
        _, yl, bl = run(sl, dr, s_c)
        y_l = y_l + yl
        b_l = b_l + bl
        if need_ctx:
            y_c = y_c + yc
            b_c = b_c + bc
    y_ctx = _rwkv_out(y_c, b_c, sc[6], gn_g, gn_b) if need_ctx else None
    return y_ctx, _rwkv_out(y_l, b_l, sl[6], gn_g, gn_b)


def _rope2d(x, ang):
    xf = x.astype(jnp.float32).reshape(x.shape[:-1] + (2, MLA_ROPE // 2))
    x1, x2 = jnp.split(xf, 2, axis=-1)
    cos = jnp.cos(ang)[None, :, None]
    sin = jnp.sin(ang)[None, :, None]
    out = jnp.concatenate([x1 * cos - x2 * sin, x2 * cos + x1 * sin], axis=-1)
    return out.reshape(x.shape).astype(x.dtype)


def _mla_prep(p, q_norm_g, q_up, kv_norm_g, kv_up, ang):
    b, t, _ = p.shape
    cq, ckv, k_rope = _split(p, [MLA_Q_RANK, MLA_KV_RANK, MLA_ROPE])
    q = (_rms(cq, q_norm_g) @ q_up).reshape(b, t, MLA_HEADS, MLA_NOPE + MLA_ROPE)
    kv = (_rms(ckv, kv_norm_g) @ kv_up).reshape(b, t, MLA_HEADS, MLA_NOPE + MLA_V)
    q_nope, q_rope = jnp.split(q, [MLA_NOPE], axis=-1)
    k_nope, v = jnp.split(kv, [MLA_NOPE], axis=-1)
    k_rope = k_rope[:, :, None, :]
    if ang is not None:
        q_rope = _rope2d(q_rope, ang)
        k_rope = _rope2d(k_rope, ang)
    k_rope = jnp.broadcast_to(k_rope, (b, t, MLA_HEADS, MLA_ROPE))
    return (jnp.concatenate([q_nope, q_rope], axis=-1),
            jnp.concatenate([k_nope, k_rope], axis=-1), v)


def _attend(q, k, v):
    s = jnp.einsum('bqhd,bkhd->bhqk', q, k).astype(jnp.float32) * MLA_SCALE
    pr = jax.nn.softmax(s, axis=-1).astype(v.dtype)
    return jnp.einsum('bhqk,bkhd->bqhd', pr, v)


def _mla_mixer(p_ctx, p_lat, mla, ang, need_ctx):
    q_norm_g, q_up, kv_norm_g, kv_up = mla
    qc, kc, vc = _mla_prep(p_ctx, q_norm_g, q_up, kv_norm_g, kv_up, None)
    ql, kl, vl = _mla_prep(p_lat, q_norm_g, q_up, kv_norm_g, kv_up, ang)
    b, t = ql.shape[:2]
    k_all = jnp.concatenate([kl, kc], axis=1)
    v_all = jnp.concatenate([vl, vc], axis=1)
    nb = t // MLA_Q_BLOCK
    qb = jnp.moveaxis(ql.reshape(b, nb, MLA_Q_BLOCK, MLA_HEADS, -1), 1, 0)
    ol = lax.map(lambda qi: _attend(qi, k_all, v_all), qb)
    y_lat = jnp.moveaxis(ol, 0, 1).reshape(b, t, MLA_W)
    y_ctx = _attend(qc, kc, vc).reshape(b, p_ctx.shape[1], MLA_W) if need_ctx else None
    return y_ctx, y_lat


def _hybrid_mixer(h_ctx, h_lat, ang, need_ctx, w_in, w_out, ml, rw, mla):
    pc_ml, pc_rw, pc_at = _split(h_ctx @ w_in, [ML_COLS, RW_COLS, MLA_COLS])
    pl_ml, pl_rw, pl_at = _split(h_lat @ w_in, [ML_COLS, RW_COLS, MLA_COLS])
    ml_c, ml_l = _mlstm_mixer(pc_ml, pl_ml, ml, need_ctx)
    rw_c, rw_l = _rwkv_mixer(pc_rw, pl_rw, rw, need_ctx)
    at_c, at_l = _mla_mixer(pc_at, pl_at, mla, ang, need_ctx)
    y_lat = jnp.concatenate([ml_l, rw_l, at_l], axis=-1) @ w_out
    y_ctx = jnp.concatenate([ml_c, rw_c, at_c], axis=-1) @ w_out if need_ctx else None
    return y_ctx, y_lat


def _peer(h, w_q, keys, u_tab, v_tab):
    n, d = h.shape

    def chunk(hc):
        tc = hc.shape[0]
        q = (hc @ w_q).reshape(tc, PEER_HEADS, 2, PEER_DK)
        s = jnp.einsum('thpd,hpkd->thpk', q, keys).astype(jnp.float32)
        s1, i1 = lax.top_k(s[:, :, 0], PEER_TOPK)
        s2, i2 = lax.top_k(s[:, :, 1], PEER_TOPK)
        cand = (s1[..., :, None] + s2[..., None, :]).reshape(tc, PEER_HEADS, PEER_TOPK * PEER_TOPK)
        cid = (i1[..., :, None] * PEER_NKEYS + i2[..., None, :]).reshape(tc, PEER_HEADS, PEER_TOPK * PEER_TOPK)
        best, pos = lax.top_k(cand, PEER_TOPK)
        eid = jnp.take_along_axis(cid, pos, axis=-1).reshape(tc, PEER_HEADS * PEER_TOPK)
        gate = jax.nn.softmax(best, axis=-1).reshape(tc, PEER_HEADS * PEER_TOPK)
        act = jax.nn.gelu(jnp.einsum('ted,td->te', u_tab[eid], hc).astype(jnp.float32))
        return jnp.einsum('te,ted->td', (gate * act).astype(hc.dtype), v_tab[eid])

    out = lax.map(chunk, h.reshape(n // PEER_CHUNK, PEER_CHUNK, d))
    return out.reshape(n, d)


def setup_inputs(seed: int = 0) -> dict:
    key = jax.random.key(seed)
    ks = iter(jax.random.split(key, 40))
    nrm = lambda shape, s: jax.random.normal(next(ks), shape, jnp.float32) * s
    uni = lambda shape, lo, hi: jax.random.uniform(next(ks), shape, jnp.float32, lo, hi)
    L, D = DEPTH, D_MODEL
    return {
        'x': nrm((BATCH, SEQ, D), 1.0),
        'c': nrm((BATCH, D), 1.0),
        'ctx': nrm((BATCH, CTX_LEN, D), 1.0),
        'c_ctx': nrm((D,), 1.0),
        'w_mod': nrm((L, D, 6 * D), 0.5 * D ** -0.5),
        'b_mod': nrm((L, 6 * D), 0.02),
        'w_in': nrm((L, D, N_IN), D ** -0.5),
        'ml_conv_w': nrm((L, ML_CONV, 2 * ML_W), ML_CONV ** -0.5),
        'ml_conv_b': nrm((L, 2 * ML_W), 0.02),
        'ml_i_bias': nrm((L, 2, ML_HEADS), 0.1),
        'ml_f_bias': uni((L, 2, ML_HEADS), 3.0, 6.0),
        'ml_norm_g': 1.0 + nrm((L, ML_W), 0.02),
        'rw_mu': uni((L, 2, RW_COLS), 0.0, 0.5),
        'rw_w0': uni((L, 2, RW_W), -6.0, -1.0),
        'rw_w_up': nrm((L, 2, RW_W_LORA, RW_W), 0.1),
        'rw_a0': nrm((L, 2, RW_W), 0.1),
        'rw_a_up': nrm((L, 2, RW_A_LORA, RW_W), 0.1),
        'rw_g_up': nrm((L, RW_G_LORA, RW_W), RW_G_LORA ** -0.5),
        'rw_k_k': 0.85 + nrm((L, RW_W), 0.02),
        'rw_k_a': 1.0 + nrm((L, RW_W), 0.02),
        'rw_r_k': nrm((L, RW_W), 0.1),
        'rw_gn_g': 1.0 + nrm((L, RW_W), 0.02),
        'rw_gn_b': nrm((L, RW_W), 0.02),
        'mla_q_norm_g': 1.0 + nrm((L, MLA_Q_RANK), 0.02),
        'mla_q_up': nrm((L, MLA_Q_RANK, MLA_HEADS * (MLA_NOPE + MLA_ROPE)), MLA_Q_RANK ** -0.5),
        'mla_kv_norm_g': 1.0 + nrm((L, MLA_KV_RANK), 0.02),
        'mla_kv_up': nrm((L, MLA_KV_RANK, MLA_HEADS * (MLA_NOPE + MLA_V)), MLA_KV_RANK ** -0.5),
        'w_out': nrm((L, D_MIX, D), DEEPNORM_BETA * D_MIX ** -0.5),
        'ln_mix_g': 1.0 + nrm((L, D), 0.02),
        'ln_mix_b': nrm((L, D), 0.02),
        'peer_w_q': nrm((L, D, PEER_HEADS * 2 * PEER_DK), D ** -0.5),
        'peer_keys': nrm((L, PEER_HEADS, 2, PEER_NKEYS, PEER_DK), PEER_DK ** -0.5),
        'peer_u': nrm((L, PEER_EXPERTS, D), D ** -0.5),
        'peer_v': nrm((L, PEER_EXPERTS, D), DEEPNORM_BETA),
        'ln_ffn_g': 1.0 + nrm((L, D), 0.02),
        'ln_ffn_b': nrm((L, D), 0.02),
    }


def reference(x, c, ctx, c_ctx, w_mod, b_mod, w_in, ml_conv_w, ml_conv_b, ml_i_bias, ml_f_bias, ml_norm_g,
              rw_mu, rw_w0, rw_w_up, rw_a0, rw_a_up, rw_g_up, rw_k_k, rw_k_a, rw_r_k, rw_gn_g, rw_gn_b,
              mla_q_norm_g, mla_q_up, mla_kv_norm_g, mla_kv_up, w_out, ln_mix_g, ln_mix_b,
              peer_w_q, peer_keys, peer_u, peer_v, ln_ffn_g, ln_ffn_b):
    b, t, d = x.shape
    n_ctx = ctx.shape[1]
    ROWS = t // GRID_W
    row = jnp.repeat(jnp.arange(ROWS), GRID_W)
    col = jnp.tile(jnp.arange(GRID_W), ROWS)
    inv_freq = ROPE_THETA ** (-jnp.arange(ROPE_AXIS_FREQS, dtype=jnp.float32) / ROPE_AXIS_FREQS)
    ang = jnp.stack([row[:, None] * inv_freq, col[:, None] * inv_freq], axis=1)

    s_ctx = ctx
    for l in range(DEPTH):
        need_ctx = l < DEPTH - 1
        sh_a, sc_a, g_a, sh_f, sc_f, g_f = jnp.split((jax.nn.silu(c) @ w_mod[l] + b_mod[l])[:, None, :], 6, axis=-1)
        csh_a, csc_a, cg_a, csh_f, csc_f, cg_f = jnp.split(jax.nn.silu(c_ctx) @ w_mod[l] + b_mod[l], 6, axis=-1)
        ml = (ml_conv_w[l], ml_conv_b[l], ml_i_bias[l], ml_f_bias[l], ml_norm_g[l])
        rw = (rw_mu[l], rw_w0[l], rw_w_up[l], rw_a0[l], rw_a_up[l], rw_g_up[l], rw_k_k[l], rw_k_a[l],
              rw_r_k[l], rw_gn_g[l], rw_gn_b[l])
        mla = (mla_q_norm_g[l], mla_q_up[l], mla_kv_norm_g[l], mla_kv_up[l])

        y_c, y_l = _hybrid_mixer(_modulate(s_ctx, csh_a, csc_a), _modulate(x, sh_a, sc_a), ang, need_ctx,
                                 w_in[l], w_out[l], ml, rw, mla)
        x = _ln_affine(DEEPNORM_ALPHA * x + g_a * y_l, ln_mix_g[l], ln_mix_b[l])
        h_l = _modulate(x, sh_f, sc_f).reshape(b * t, d)

        if need_ctx:
            s_ctx = _ln_affine(DEEPNORM_ALPHA * s_ctx + cg_a * y_c, ln_mix_g[l], ln_mix_b[l])
            h_c = _modulate(s_ctx, csh_f, csc_f).reshape(b * n_ctx, d)
            y = _peer(jnp.concatenate([h_c, h_l], axis=0), peer_w_q[l], peer_keys[l], peer_u[l], peer_v[l])
            y_c = y[: b * n_ctx].reshape(b, n_ctx, d)
            y_l = y[b * n_ctx:].reshape(b, t, d)
            s_ctx = _ln_affine(DEEPNORM_ALPHA * s_ctx + cg_f * y_c, ln_ffn_g[l], ln_ffn_b[l])
        else:
            y_l = _peer(h_l, peer_w_q[l], peer_keys[l], peer_u[l], peer_v[l]).reshape(b, t, d)
        x = _ln_affine(DEEPNORM_ALPHA * x + g_f * y_l, ln_ffn_g[l], ln_ffn_b[l])
    return x
```

```python
import numpy as np
import concourse.bass as bass
import concourse.mybir as mybir
from contextlib import ExitStack

F32 = mybir.dt.float32
BF16 = mybir.dt.bfloat16
U32 = mybir.dt.uint32
I32 = mybir.dt.int32
AF = mybir.ActivationFunctionType
OP = mybir.AluOpType
AX = mybir.AxisListType


class Res:
    __slots__ = ("name", "w", "r", "pe_row", "excl")

    def __init__(self, name=""):
        self.name = name
        self.pe_row = None
        self.excl = False
        self.w = None
        self.r = {}


class Sched:
    NDMA = 24
    NSW = 8

    def __init__(self, nc, es):
        self.nc = nc
        self.es = es
        self.eng = {"pe": nc.tensor, "dve": nc.vector, "act": nc.scalar,
                    "pool": nc.gpsimd, "sp": nc.sync}
        self.sem = {}
        self.cnt = {}
        for k in self.eng:
            self.sem[k] = es.enter_context(nc.semaphore("s_" + k))
            self.cnt[k] = 0
        self.dsem = [es.enter_context(nc.semaphore("d%d" % i)) for i in range(self.NDMA + self.NSW)]
        self.dcnt = [0] * (self.NDMA + self.NSW)
        self.dnext = 0
        self.swnext = 0
        self.known = {k: {} for k in self.eng}
        self.gen = {k: 0 for k in self.eng}
        self.ekey = {k: k for k in self.eng}
        self.semobj = dict(self.sem)
        for i, s in enumerate(self.dsem):
            self.semobj["d%d" % i] = s
        self.ninst = 0

    def _wait(self, e, tok):
        if tok is None:
            return
        key, val = tok
        kn = self.known[e]
        if kn.get(key, 0) >= val:
            return
        self.eng[e].wait_ge(self.semobj[key], val)
        kn[key] = val

    def _deps(self, e, reads, writes, same_ok=False):
        for r in reads:
            if r.w is not None and not (same_ok and r.w[0].split("#")[0] == e):
                self._wait(e, r.w)
            if r.excl:
                for key, val in r.r.items():
                    if key.split("#")[0] != e:
                        self._wait(e, (key, val))
        for w in writes:
            if w.w is not None and not (same_ok and w.w[0].split("#")[0] == e):
                self._wait(e, w.w)
            for key, val in w.r.items():
                if not (same_ok and key.split("#")[0] == e):
                    self._wait(e, (key, val))

    def _commit(self, tok, reads, writes):
        key, val = tok
        for r in reads:
            if r.r.get(key, 0) < val:
                r.r[key] = val
        for w in writes:
            w.w = tok
            w.r = {}

    SEM_LIMIT = 20000

    def op(self, e, fn, reads=(), writes=(), same_ok=False):
        if getattr(self, "mute", False):
            return None
        if self.cnt[e] >= self.SEM_LIMIT:
            self.gen[e] += 1
            key = "%s#%d" % (e, self.gen[e])
            self.sem[e] = self.es.enter_context(self.nc.semaphore("s_%s_%d" % (e, self.gen[e])))
            self.semobj[key] = self.sem[e]
            self.ekey[e] = key
            self.cnt[e] = 0
        self._deps(e, reads, writes, same_ok)
        ins = fn(self.eng[e])
        self.cnt[e] += 1
        ins.then_inc(self.sem[e], 1)
        self._commit((self.ekey[e], self.cnt[e]), reads, writes)
        self.ninst += 1
        return ins

    def dma(self, out, in_, reads=(), writes=(), q="sp", **kw):
        s = self.dnext
        self.dnext = (self.dnext + 1) % self.NDMA
        key = "d%d" % s
        if self.dcnt[s] > 0:
            self._wait(q, (key, self.dcnt[s]))
        self._deps(q, reads, writes)
        ins = self.eng[q].dma_start(out=out, in_=in_, **kw)
        self.dcnt[s] += 16
        ins.then_inc(self.dsem[s], 16)
        self._commit((key, self.dcnt[s]), reads, writes)
        self.ninst += 1
        return (key, self.dcnt[s])

    def _dma_token(self, ins, q, reads, writes):
        s = self.dnext
        self.dnext = (self.dnext + 1) % self.NDMA
        raise RuntimeError("use dma_custom")

    def dma_custom(self, q, build, reads=(), writes=()):
        if getattr(self, "mute", False):
            return None
        s = self.NDMA + self.swnext
        self.swnext = (self.swnext + 1) % self.NSW
        key = "d%d" % s
        if self.dcnt[s] > 0:
            self._wait(q, (key, self.dcnt[s]))
        self._deps(q, reads, writes)
        ins = build(self.eng[q])
        self.dcnt[s] += 16
        ins.then_inc(self.dsem[s], 16)
        self._commit((key, self.dcnt[s]), reads, writes)
        self.ninst += 1
        return (key, self.dcnt[s])

    def wait_all(self, e="sp"):
        for s in range(self.NDMA + self.NSW):
            if self.dcnt[s] > 0:
                self._wait(e, ("d%d" % s, self.dcnt[s]))
        for k in self.eng:
            if self.cnt[k] > 0:
                self._wait(e, (self.ekey[k], self.cnt[k]))


class V:
    __slots__ = ("ap", "res")

    def __init__(self, ap, res):
        self.ap = ap
        self.res = res

    def __getitem__(self, idx):
        return V(self.ap[idx], self.res)


class T:
    def __init__(self, h, name):
        self.h = h
        self.res = [Res(name)]

    def __getitem__(self, idx):
        return V(self.h[idx], self.res)

    def v(self):
        return V(self.h[:], self.res)


def _rs(*vs):
    out = []
    for v in vs:
        if isinstance(v, V):
            out.extend(v.res)
    return out


def _ap(v):
    return v.ap if isinstance(v, V) else v


class K(Sched):
    def sb(self, name, shape, dt=F32):
        return T(self.es.enter_context(self.nc.sbuf_tensor(name, shape, dt)), name)

    def ps(self, name, shape, dt=F32):
        t = T(self.es.enter_context(self.nc.psum_tensor(name, shape, dt)), name)
        t.res[0].excl = True
        return t

    def tt(self, e, out, in0, in1, op):
        return self.op(e, lambda g: g.tensor_tensor(_ap(out), _ap(in0), _ap(in1), op), _rs(in0, in1), _rs(out))

    def ts(self, e, out, in0, s1, op0, s2=None, op1=None, accum=None):
        def f(g):
            kw = {}
            if op1 is not None:
                kw["op1"] = op1
            if accum is not None:
                kw["accum_out"] = _ap(accum)
            return g.tensor_scalar(_ap(out), _ap(in0), _ap(s1), _ap(s2), op0=op0, **kw)
        return self.op(e, f, _rs(in0, s1, s2), _rs(out, accum))

    def stt(self, e, out, in0, s, in1, op0, op1):
        return self.op(e, lambda g: g.scalar_tensor_tensor(_ap(out), _ap(in0), _ap(s), _ap(in1), op0=op0, op1=op1),
                       _rs(in0, s, in1), _rs(out))

    def cp(self, e, out, in_):
        if e == "act":
            return self.op(e, lambda g: g.copy(_ap(out), _ap(in_)), _rs(in_), _rs(out))
        return self.op(e, lambda g: g.tensor_copy(_ap(out), _ap(in_)), _rs(in_), _rs(out))

    def act(self, out, in_, func, bias=0.0, scale=1.0, accum=None):
        def f(g):
            kw = {}
            if accum is not None:
                kw["accum_out"] = _ap(accum)
            return g.activation(_ap(out), _ap(in_), func, bias=_ap(bias), scale=_ap(scale), **kw)
        return self.op("act", f, _rs(in_, bias, scale), _rs(out, accum))

    def red(self, e, out, in_, op, axis=AX.X):
        return self.op(e, lambda g: g.tensor_reduce(_ap(out), _ap(in_), axis, op), _rs(in_), _rs(out))

    def recip(self, out, in_):
        return self.op("dve", lambda g: g.reciprocal(_ap(out), _ap(in_)), _rs(in_), _rs(out))

    def memset(self, e, out, val):
        return self.op(e, lambda g: g.memset(_ap(out), val), [], _rs(out))

    def mm(self, out, lhsT, rhs, start=True, stop=True):
        if getattr(self, "mute", False):
            return None
        la = _ap(lhsT)
        key = (la.base_partition(), la.partition_size())
        for r in out.res:
            prev = getattr(r, "pe_row", None)
            if prev is not None and prev != key and r.w is not None and r.w[0].split("#")[0] == "pe":
                self._wait("pe", r.w)
        ins = self.op("pe", lambda g: g.matmul(_ap(out), la, _ap(rhs), start=start, stop=stop),
                      _rs(lhsT, rhs), _rs(out), same_ok=True)
        for r in out.res:
            r.pe_row = key
        return ins

    def tr(self, out, in_, ident):
        return self.op("pe", lambda g: g.transpose(_ap(out), _ap(in_), _ap(ident)), _rs(in_, ident), _rs(out),
                       same_ok=True)

    def ld(self, out, in_ap, q="sp", **kw):
        return self.dma(_ap(out), in_ap, reads=[], writes=_rs(out), q=q, **kw)

    def st(self, out_ap, in_, q="sp", dram_res=None, **kw):
        return self.dma(out_ap, _ap(in_), reads=_rs(in_), writes=(dram_res or []), q=q, **kw)

    def barrier(self):
        for e in self.eng:
            self.wait_all(e)


import itertools
_ctr = itertools.count()


def usb(nc, name, shape, dt):
    return nc.sbuf_tensor("%s_u%d" % (name, next(_ctr)), shape, dt)


NT = 2304
NCTX = 256
D = 1024
N_IN = 3248
NTILE = NT // 128
NCH = NT // 64
ALPHA = 4 ** 0.25
LN_EPS = 1e-6
MOD_SHA, MOD_SCA, MOD_GA, MOD_SHF, MOD_SCF, MOD_GF = range(6)


def host_consts():
    c = {}
    c["ident"] = np.eye(128, dtype=np.float32)
    s = np.arange(64)[:, None]; t = np.arange(64)[None, :]
    triF = (s <= t).astype(np.float32); triR = (s >= t).astype(np.float32)
    c["c_tri"] = np.concatenate([triF, triR], 0)
    mF = np.where(t <= s, 0.0, -1e30).astype(np.float32)
    mR = np.where(t >= s, 0.0, -1e30).astype(np.float32)
    c["c_maskD"] = np.concatenate([mF, mR], 0)
    c["c_I2"] = np.concatenate([np.eye(64, dtype=np.float32)] * 2, 0)
    tt_ = np.arange(2048)
    inv = (10000.0 ** (-np.arange(8, dtype=np.float32) / 8)).astype(np.float32)
    ang = np.stack([(tt_ // 64)[:, None].astype(np.float32) * inv, (tt_ % 64)[:, None].astype(np.float32) * inv], 1)
    c["c_rope"] = np.concatenate([np.cos(ang).reshape(2048, 16), np.sin(ang).reshape(2048, 16)], 1).astype(np.float32)
    c["c_u32"] = np.ascontiguousarray(np.broadcast_to(np.array([15, 4], np.uint32)[None, :], (128, 2)))
    c["c_iota128"] = np.ascontiguousarray(np.broadcast_to(np.arange(128, dtype=np.float32)[None, :], (128, 128)))
    c["c_iota16"] = np.ascontiguousarray(np.broadcast_to(np.arange(16, dtype=np.float32)[None, :], (128, 16)))
    bd = np.zeros((128, 128), np.float32); bd[:64, :64] = 1; bd[64:, 64:] = 1
    c["c_BD2"] = bd
    r_ = np.arange(64)[:, None]; c_ = np.arange(64)[None, :]
    su = (r_ < c_).astype(np.float32); iu = (r_ <= c_).astype(np.float32)
    sl = (r_ > c_).astype(np.float32); il = (r_ >= c_).astype(np.float32)
    mf = np.concatenate([su, iu, su, iu, sl], 1); mr = np.concatenate([sl, il, sl, il, su], 1)
    m2 = np.stack([mf, mr], 0)
    c["c_rwmask"] = np.ascontiguousarray(np.concatenate([m2, m2], 1).transpose(1, 0, 2))
    return c


def host_params(inp):
    L = 2
    p = {}
    cw = np.concatenate([inp["ml_conv_w"], inp["ml_conv_b"][:, None, :]], 1)
    p["ml_cw"] = np.ascontiguousarray(cw.reshape(L, 4, 4, 128).transpose(0, 3, 2, 1))
    gb = np.zeros((L, 16), np.float32)
    for d in range(2):
        gb[:, d * 8:d * 8 + 4] = inp["ml_i_bias"][:, d]
        gb[:, d * 8 + 4:d * 8 + 8] = inp["ml_f_bias"][:, d]
    p["ml_gb"] = np.ascontiguousarray(np.broadcast_to(gb[:, None, :], (L, 128, 16)))
    p["ml_ng"] = np.ascontiguousarray(np.broadcast_to(inp["ml_norm_g"][:, None, :], (L, 128, 256)))
    p["mla_gq"] = np.ascontiguousarray(np.broadcast_to(inp["mla_q_norm_g"][:, None, :], (L, 128, 384)))
    p["mla_gkv"] = np.ascontiguousarray(np.broadcast_to(inp["mla_kv_norm_g"][:, None, :], (L, 128, 256)))
    for nm in ("ln_mix_g", "ln_mix_b", "ln_ffn_g", "ln_ffn_b"):
        p[nm] = np.ascontiguousarray(np.broadcast_to(inp[nm][:, None, :], (L, 128, 1024)))
    p["rw_muT"] = np.ascontiguousarray(inp["rw_mu"].reshape(L, 2, 12, 128).transpose(0, 3, 2, 1))
    p["rw_wup"] = np.ascontiguousarray(inp["rw_w_up"].reshape(L, 128, 384))
    p["rw_aup"] = np.ascontiguousarray(inp["rw_a_up"].reshape(L, 128, 384))
    p["rw_gup"] = np.ascontiguousarray(inp["rw_g_up"])
    cols = np.stack([inp["rw_k_k"], inp["rw_k_a"], inp["rw_r_k"], inp["rw_gn_g"], inp["rw_gn_b"],
                     inp["rw_w0"][:, 0], inp["rw_w0"][:, 1], inp["rw_a0"][:, 0], inp["rw_a0"][:, 1]], -1)
    p["rw_cols"] = np.ascontiguousarray(cols.reshape(L, 3, 128, 9).transpose(0, 2, 1, 3))
    return p


class Prog:
    def __init__(self, debug=()):
        self.debug = set(debug)
        self.nc = nc = bass.Bass("TRN2", target_bir_lowering=False)
        self.es = ExitStack()
        self.k = K(nc, self.es)
        self.din = {}
        self.dout = {}

    def inp(self, name, shape, dt=F32):
        self.din[name] = self.nc.dram_tensor(name, list(shape), dt, kind="ExternalInput").ap()
        return self.din[name]

    def outp(self, name, shape, dt=F32):
        self.dout[name] = self.nc.dram_tensor(name, list(shape), dt, kind="ExternalOutput").ap()
        return self.dout[name]

    def dump(self, name, view):
        ap = view.ap
        o = self.outp("o_" + name, list(ap.shape), ap.dtype)
        self.k.dma(o, ap, reads=view.res)

    def scratch(self, name, shape, dt=F32):
        kind = "ExternalOutput" if name in self.debug else "Internal"
        ap = self.nc.dram_tensor(name, list(shape), dt, kind=kind).ap()
        if kind == "ExternalOutput":
            self.dout[name] = ap
        return ap


def setup_common(P):
    k = P.k
    P.ident_d = P.inp("ident", [128, 128])
    P.ident = k.sb("ident_sb", [128, 128])
    k.ld(P.ident.v(), P.ident_d)
    P.identb = k.sb("identb", [128, 128], BF16)
    k.cp("dve", P.identb.v(), P.ident.v())
    P.ones = k.sb("ones", [128, 128])
    k.memset("dve", P.ones.v(), 1.0)
    P.pb = [k.ps("pb%d" % i, [128, 512]) for i in range(8)]
    P.bnst = k.sb("bnst", [128, 12])
    P.wstage = [k.sb("wstage%d" % i, [128, 8, 256]) for i in range(2)]
    P.wsi = 0
    for nm, shp in (("c_tri", [128, 64]), ("c_maskD", [128, 64]), ("c_I2", [128, 64]), ("c_BD2", [128, 128]), ("c_rwmask", [128, 2, 320]), ("c_rope", [2048, 32]), ("c_iota16", [128, 16]), ("c_iota128", [128, 128]), ("c_u32", [128, 2, "u32"])):
        if shp[-1] == "u32":
            P.inp(nm, shp[:-1], U32)
        else:
            P.inp(nm, shp)
    P.xin = P.inp("xin", [NT, D])
    P.cvec = P.inp("cvec", [128, 8, 2])
    P.XD = P.scratch("XD", [NT, D])
    P.XD_res = [Res("XD")]


def phase_mod(P, l, w_mod, b_mod, MODD, MOD_res):
    k = P.k
    nc = P.nc
    es = ExitStack()
    tile = lambda name, shape, dt=F32: T(es.enter_context(usb(nc, "t_" + name, shape, dt)), name)
    csilu = tile("csilu", [128, 8, 2])
    cv = tile("cvec_sb", [128, 8, 2])
    k.ld(cv.v(), P.cvec)
    k.act(csilu.v(), cv.v(), AF.Silu)
    clhs = [tile("clhs%d" % s_, [128, 8, 128]) for s_ in range(2)]
    for s_ in range(2):
        for kc in range(8):
            k.cp("dve", clhs[s_][:, kc, :], V(csilu.h[:, kc, s_:s_ + 1].to_broadcast([128, 128]), csilu.res))
    wbuf = [tile("wmodbuf%d" % i, [128, 8, 512]) for i in range(2)]
    ob = [tile("modout%d" % i, [128, 512]) for i in range(4)]
    bb = tile("bmodrow", [1, 6 * D])
    k.ld(bb.v(), b_mod[l:l + 1, :])
    for n in range(12):
        wb = wbuf[n % 2]
        k.ld(wb.v(), w_mod[l, :, n * 512:(n + 1) * 512].rearrange("(c p) n -> p c n", p=128))
        for s_ in range(2):
            pt = P.pb[(n * 2 + s_) % 4]
            o_ = ob[(n * 2 + s_) % 4]
            for kc in range(8):
                k.mm(pt.v(), clhs[s_][:, kc, :], wb[:, kc, :], start=(kc == 0), stop=False)
            k.mm(pt.v(), P.ones[0:1, :], bb[0:1, n * 512:(n + 1) * 512], start=False, stop=True)
            k.cp("act" if s_ else "dve", o_.v(), pt.v())
            k.dma(MODD[s_, n // 2, :, (n % 2) * 512:(n % 2 + 1) * 512], o_.h[:], reads=o_.res, writes=MOD_res)
    k.barrier()
    es.close()


def ln_stats(P, xt, st, nfeat=D):
    k = P.k
    nchunk = nfeat // 512
    bs = P.bnst
    for c in range(nchunk):
        k.op("dve", lambda g: g.bn_stats(bs.h[:, c * 6:(c + 1) * 6], xt.ap[:, c * 512:(c + 1) * 512]), xt.res, bs.res)
    k.op("dve", lambda g: g.bn_aggr(st.h[:, 0:2], bs.h[:, 0:6 * nchunk]), bs.res, st.res)
    k.ts("dve", st[:, 2:3], st[:, 1:2], LN_EPS, OP.add)
    k.act(st[:, 3:4], st[:, 2:3], AF.Sqrt)
    k.recip(st[:, 4:5], st[:, 3:4])
    k.stt("dve", st[:, 5:6], st[:, 0:1], -1.0, st[:, 4:5], OP.mult, OP.mult)
    return st[:, 0:1], st[:, 4:5], st[:, 5:6]


def phase_hT(P, src_ap, src_res, MODD, MOD_res, shj, scj, hT):
    k = P.k
    nc = P.nc
    es = ExitStack()
    xt = [T(es.enter_context(usb(nc, "ph_x%d" % i, [128, D], F32)), "ph_x%d" % i) for i in range(2)]
    hh = [T(es.enter_context(usb(nc, "ph_h%d" % i, [128, D], F32)), "ph_h%d" % i) for i in range(2)]
    st = [T(es.enter_context(usb(nc, "ph_st%d" % i, [128, 16], F32)), "ph_st%d" % i) for i in range(2)]
    sc1 = [T(es.enter_context(usb(nc, "ph_sc1_%d" % s, [128, D], F32)), "ph_sc1_%d" % s) for s in range(2)]
    sh1 = [T(es.enter_context(usb(nc, "ph_sh1_%d" % s, [128, D], F32)), "ph_sh1_%d" % s) for s in range(2)]
    for s in range(2):
        k.dma(sc1[s].h[:], MODD[s, scj], reads=MOD_res, writes=sc1[s].res)
        k.dma(sh1[s].h[:], MODD[s, shj], reads=MOD_res, writes=sh1[s].res)
        k.ts("pool", sc1[s].v(), sc1[s].v(), 1.0, OP.add)
    for ti in range(NTILE):
        s = 1 if ti < NCTX // 128 else 0
        x = xt[ti % 2]; h = hh[ti % 2]; stt_ = st[ti % 2]
        k.dma(x.h[:], src_ap[ti * 128:(ti + 1) * 128, :], reads=src_res[ti], writes=x.res)
        mean, rstd, nmr = ln_stats(P, x.v(), stt_)
        k.act(h.v(), x.v(), AF.Identity, bias=nmr, scale=rstd)
        k.tt("dve", h.v(), h.v(), sc1[s].v(), OP.mult)
        k.tt("pool", h.v(), h.v(), sh1[s].v(), OP.add)
        for g4 in range(2):
            pt = P.pb[4 + (ti * 2 + g4) % 2]
            for j in range(4):
                dc = g4 * 4 + j
                k.tr(pt[:, j * 128:(j + 1) * 128], h[:, dc * 128:(dc + 1) * 128], P.ident.v())
            k.cp("act", hT[:, g4 * 4:(g4 + 1) * 4, ti * 128:(ti + 1) * 128],
                 V(pt.h[:].rearrange("p (j t) -> p j t", j=4), pt.res))
    k.barrier()
    es.close()


def bc(view, shape):
    return V(view.ap.to_broadcast(shape), view.res)


def rr(view, pat, **kw):
    return V(view.ap.rearrange(pat, **kw), view.res)


def load_w_bf16(P, dst, w_ap, ncols, c_off=0):
    k = P.k
    for c0 in range(0, ncols, 256):
        n = min(256, ncols - c0)
        stg = P.wstage[P.wsi % 2]
        P.wsi += 1
        k.ld(stg[:, :, 0:n], w_ap[:, c0:c0 + n].rearrange("(c p) n -> p c n", p=128))
        k.cp("pool" if (P.wsi % 2) else "dve", dst[:, :, c_off + c0:c_off + c0 + n], stg[:, :, 0:n])


ORD_F = list(range(NCH))
ORD_R = [3, 2, 1, 0] + list(range(NCH - 1, 3, -1))


def phase_mlstm(P, l, hT, w_in, prm, MIXD, MIX_res):
    k = P.k
    nc = P.nc
    es = ExitStack()
    tile = lambda name, shape, dt=F32: T(es.enter_context(usb(nc, "t_" + name, shape, dt)), name)
    ident = P.ident
    wml = tile("wml", [128, 8, 1040], BF16)
    load_w_bf16(P, wml, w_in[l, :, 0:1040], 1040)
    qkT = tile("qkT", [128, 4, NT])
    raw = tile("mlraw", [128, NT])
    cw = tile("mlcw", [128, 4, 4]); k.ld(cw.v(), prm["ml_cw"][l])
    gb = tile("mlgb", [128, 16]); k.ld(gb.v(), prm["ml_gb"][l])
    ng = tile("mlng", [128, 256]); k.ld(ng.v(), prm["ml_ng"][l])
    tri = tile("mltri", [128, 64]); k.ld(tri.v(), P.din["c_tri"])
    maskD = tile("mlmask", [128, 64]); k.ld(maskD.v(), P.din["c_maskD"])
    I2 = tile("mlI2", [128, 64]); k.ld(I2.v(), P.din["c_I2"])
    pb = P.pb
    ib = 0
    for fc in range(4):
        for tb in range(0, NT, 512):
            n = min(512, NT - tb)
            pt = pb[ib % 2]; ib += 1
            for dc in range(8):
                k.mm(pt[:, 0:n], wml[:, dc, fc * 128:(fc + 1) * 128], hT[:, dc, tb:tb + n], start=(dc == 0), stop=(dc == 7))
            k.cp("act", raw[:, tb:tb + n], pt[:, 0:n])
        dst = qkT[:, fc, :]
        k.act(dst, raw.v(), AF.Identity, bias=cw[:, fc, 3:4], scale=cw[:, fc, 1:2])
        for (s0, s1) in ((0, NCTX), (NCTX, NT)):
            k.stt("dve", qkT[:, fc, s0 + 1:s1], raw[:, s0:s1 - 1], cw[:, fc, 0:1], qkT[:, fc, s0 + 1:s1], OP.mult, OP.add)
            k.stt("dve", qkT[:, fc, s0:s1 - 1], raw[:, s0 + 1:s1], cw[:, fc, 2:3], qkT[:, fc, s0:s1 - 1], OP.mult, OP.add)
        k.act(dst, dst, AF.Silu)
        if fc < 2:
            k.ts("dve", dst, dst, 0.125, OP.mult)
    if "ml_qkT" in P.debug:
        P.dump("ml_qkT", qkT.v())
    import os
    if os.environ.get("MLSTOP") == "1":
        k.barrier(); es.close(); return
    NSTEP = int(os.environ.get("MLSTEPS", NCH))
    CUT = int(os.environ.get("MLCUT", 99))
    def sec(n):
        k.mute = CUT < n
    CT = tile("mlCT", [128, 2, 2, 65]); k.memset("dve", CT.v(), 0.0)
    mfull = tile("mlm", [128, 8]); k.memset("dve", mfull.v(), -1e30)
    hsum = tile("mlhsum", [128, NTILE, 256]); k.memset("pool", hsum.v(), 0.0)
    vS = tile("mlvS", [128, 4, 65]); k.memset("dve", vS.v(), 1.0)
    kS = tile("mlkS", [128, 4, 64])
    gS = tile("mlgS", [128, 8])
    cs = tile("mlcs", [128, 12])
    lmb = tile("mllmb", [128, 4])
    D8 = tile("mlD8", [128, 4, 64])
    rmax = tile("mlrmax", [128, 8])
    dm = tile("mldm", [128, 4, 64])
    sm = tile("mlsmall", [128, 64])
    Sm = tile("mlSm", [128, 4, 64])
    ST = tile("mlST", [128, 4, 64])
    tI = tile("mltI", [128, 4, 65])
    nd = tile("mlnd", [128, 4, 65])
    hout = tile("mlhout", [128, 4, 64])
    kw = tile("mlkw", [128, 4, 64])
    mmax = tile("mlmmax", [128, 8])
    dec = tile("mldec", [128, 8])
    c4 = lambda i: sm[:, i * 4:(i + 1) * 4]
    mx, a1, mt, tmp, wint, emt, dabs, rden, wk, mmA = [c4(i) for i in range(10)]
    pA, pR, pS, pST, pN, pI, pU, pX = pb
    B4 = lambda v: bc(V(v.ap.unsqueeze(2), v.res), [128, 4, 64])
    for step in range(NSTEP):
        chunks = (ORD_F[step], ORD_R[step])
        sec(1)
        for dr in range(2):
            c = chunks[dr]
            H = slice(dr * 64, dr * 64 + 64)
            tok = slice(c * 64, c * 64 + 64)
            for dc in range(8):
                k.mm(pX[H, 0:256], hT[:, dc, tok], wml[:, dc, 512:768], start=(dc == 0), stop=(dc == 7))
            for dc in range(8):
                k.mm(pA[H, 16:32], hT[:, dc, tok], wml[:, dc, 1024:1040], start=(dc == 0), stop=(dc == 7))
            for hh in range(2):
                k.mm(pX[H, 256 + hh * 128:256 + (hh + 1) * 128], qkT[:, 2 + hh, tok], ident.v())
            k.tt("dve", gS[H, :], pA[H, 16 + dr * 8:16 + dr * 8 + 8], gb[H, dr * 8:dr * 8 + 8], OP.add)
        k.cp("act", vS[:, :, 0:64], rr(pX[:, 0:256], "p (h e) -> p h e", h=4))
        k.cp("dve", kS.v(), rr(pX[:, 256:512], "p (h e) -> p h e", h=4))
        k.act(gS[:, 4:8], gS[:, 4:8], AF.Exp, scale=-1.0)
        k.act(gS[:, 4:8], gS[:, 4:8], AF.Ln, bias=1.0)
        k.ts("dve", gS[:, 4:8], gS[:, 4:8], -1.0, OP.mult)
        sec(2)
        for dr in range(2):
            H = slice(dr * 64, dr * 64 + 64)
            k.mm(pA[H, 0:4], tri[H, :], gS[H, 4:8])
            k.mm(pA[:, 4 + dr * 4:8 + dr * 4], P.ones[H, :], gS[H, 4:8])
        k.cp("dve", cs.v(), pA[:, 0:12])
        k.tt("dve", lmb.v(), gS[:, 0:4], cs[:, 0:4], OP.subtract)
        k.tt("dve", D8.v(), bc(V(I2.h[:].unsqueeze(1), I2.res), [128, 4, 64]), B4(lmb.v()), OP.mult)
        for dr in range(2):
            H = slice(dr * 64, dr * 64 + 64)
            k.mm(rr(pR[:, dr * 256:(dr + 1) * 256], "p (h s) -> p h s", h=4), P.ones[H, :], D8[H, :, :])
        k.red("dve", rmax.v(), rr(pR.v(), "p (u s) -> p u s", u=8), OP.max)
        for dr in range(2):
            H = slice(dr * 64, dr * 64 + 64)
            k.tt("dve", dm[H, :, :], rr(pR[H, dr * 256:(dr + 1) * 256], "p (h s) -> p h s", h=4),
                 bc(V(maskD.h[H, :].unsqueeze(1), maskD.res), [64, 4, 64]), OP.add)
            k.tt("dve", a1[H, :] if False else V(sm.h[H, 4:8], sm.res), cs[H, 0:4], mfull[H, dr * 4:dr * 4 + 4], OP.add)
        k.tt("dve", dm.v(), dm.v(), B4(cs[:, 0:4]), OP.add)
        k.red("dve", mx, dm.v(), OP.max)
        k.tt("dve", mt, mx, a1, OP.max)
        k.tt("dve", dm.v(), dm.v(), B4(mt), OP.subtract)
        k.act(dm.v(), dm.v(), AF.Exp)
        k.tt("dve", tmp, a1, mt, OP.subtract)
        k.act(wint, tmp, AF.Exp)
        k.act(emt, mt, AF.Exp, scale=-1.0)
        sec(3)
        for dr in range(2):
            c = chunks[dr]
            H = slice(dr * 64, dr * 64 + 64)
            tok = slice(c * 64, c * 64 + 64)
            for h in range(4):
                hp = slice((h % 2) * 64, (h % 2) * 64 + 64)
                k.mm(pS[H, h * 64:(h + 1) * 64], qkT[hp, h // 2, tok], qkT[hp, 2 + h // 2, tok])
        k.tt("dve", Sm.v(), rr(pS[:, 0:256], "p (h s) -> p h s", h=4), dm.v(), OP.mult)
        sec(4)
        for dr in range(2):
            H = slice(dr * 64, dr * 64 + 64)
            for h in range(4):
                k.mm(pST[H, h * 64:(h + 1) * 64], Sm[H, h, :], ident[H, H])
        k.cp("act", ST.v(), rr(pST[:, 0:256], "p (h s) -> p h s", h=4))
        for dr in range(2):
            c = chunks[dr]
            H = slice(dr * 64, dr * 64 + 64)
            tok = slice(c * 64, c * 64 + 64)
            for h in range(4):
                hp = slice((h % 2) * 64, (h % 2) * 64 + 64)
                k.mm(pN[H, h * 65:(h + 1) * 65], ST[H, h, :], vS[H, h, :])
                k.mm(pI[H, h * 65:(h + 1) * 65], qkT[hp, h // 2, tok], CT[hp, dr, h // 2, :])
        sec(5)
        B65 = lambda v: bc(V(v.ap.unsqueeze(2), v.res), [128, 4, 65])
        k.tt("dve", tI.v(), rr(pI[:, 0:260], "p (h e) -> p h e", h=4), B65(wint), OP.mult)
        k.tt("dve", nd.v(), tI.v(), rr(pN[:, 0:260], "p (h e) -> p h e", h=4), OP.add)
        k.act(dabs, nd[:, :, 64], AF.Abs)
        k.tt("dve", dabs, dabs, emt, OP.max)
        k.recip(rden, dabs)
        k.tt("dve", hout.v(), nd[:, :, 0:64], B4(rden), OP.mult)
        sec(6)
        for dr in range(2):
            c = chunks[dr]
            H = slice(dr * 64, dr * 64 + 64)
            Hc = slice((c % 2) * 64, (c % 2) * 64 + 64)
            k.mm(pS[Hc, 256:512], ident[H, H], rr(hout[H, :, :], "p h e -> p (h e)"))
            k.tt("pool" if False else "dve", hsum[Hc, c // 2, :], hsum[Hc, c // 2, :], pS[Hc, 256:512], OP.add)
        sec(7)
        k.tt("dve", mmax.v(), mfull.v(), rmax.v(), OP.max)
        k.tt("dve", dec.v(), mfull.v(), mmax.v(), OP.subtract)
        k.act(dec.v(), dec.v(), AF.Exp)
        for dr in range(2):
            H = slice(dr * 64, dr * 64 + 64)
            k.tt("dve", V(sm.h[H, 36:40], sm.res), lmb[H, :], mmax[H, dr * 4:dr * 4 + 4], OP.subtract)
        k.act(wk, mmA, AF.Exp)
        k.tt("dve", kw.v(), kS.v(), B4(wk), OP.mult)
        for dr in range(2):
            H = slice(dr * 64, dr * 64 + 64)
            for h in range(4):
                hp = slice((h % 2) * 64, (h % 2) * 64 + 64)
                o0 = (dr * 2 + h // 2) * 65
                k.mm(pU[hp, o0:o0 + 65], kw[H, h, :], vS[H, h, :])
        for hpi in range(2):
            hp = slice(hpi * 64, hpi * 64 + 64)
            dview = V(dec.h[hp, :].rearrange("p (d hh hp) -> p d hh hp", d=2, hh=2, hp=2)[:, :, :, hpi].unsqueeze(3).to_broadcast([64, 2, 2, 65]), dec.res)
            k.tt("dve", CT[hp, :, :, :], CT[hp, :, :, :], dview, OP.mult)
        k.tt("dve", CT.v(), CT.v(), rr(pU[:, 0:260], "p (d hh e) -> p d hh e", d=2, hh=2), OP.add)
        k.tt("dve", mfull.v(), cs[:, 4:12], mmax.v(), OP.add)
    k.mute = False
    if "ml_hsum" in P.debug:
        P.dump("ml_hsum", hsum.v())
    if os.environ.get("MLSTOP") == "2":
        k.barrier(); es.close(); return
    osig = tile("mlosig", [128, NTILE, 256])
    for ti in range(NTILE):
        pt = pb[ti % 2]
        for dc in range(8):
            k.mm(pt[:, 0:256], hT[:, dc, ti * 128:(ti + 1) * 128], wml[:, dc, 768:1024], start=(dc == 0), stop=(dc == 7))
        k.act(osig[:, ti, :], pt[:, 0:256], AF.Sigmoid)
    NG = NTILE * 4
    h3 = rr(hsum.v(), "p t (h e) -> p (t h) e", h=4)
    st = tile("mlst", [128, 4, NG])
    sq = tile("mlsq", [128, NG, 64])
    BG = lambda v: bc(V(v.ap.unsqueeze(2), v.res), [128, NG, 64])
    k.red("dve", st[:, 0, :], h3, OP.add)
    k.ts("dve", st[:, 0, :], st[:, 0, :], 1.0 / 64, OP.mult)
    k.tt("dve", h3, h3, BG(st[:, 0, :]), OP.subtract)
    k.tt("pool", sq.v(), h3, h3, OP.mult)
    k.red("dve", st[:, 1, :], sq.v(), OP.add)
    k.ts("dve", st[:, 1, :], st[:, 1, :], 1.0 / 64, OP.mult, LN_EPS, OP.add)
    k.act(st[:, 2, :], st[:, 1, :], AF.Sqrt)
    k.recip(st[:, 3, :], st[:, 2, :])
    k.tt("dve", h3, h3, BG(st[:, 3, :]), OP.mult)
    k.tt("pool", hsum.v(), hsum.v(), bc(V(ng.h[:].unsqueeze(1), ng.res), [128, NTILE, 256]), OP.mult)
    k.tt("dve", hsum.v(), hsum.v(), osig.v(), OP.mult)
    oT = [tile("mloT%d" % i, [128, 2, 128]) for i in range(2)]
    for ti in range(NTILE):
        pt = pb[ti % 2]
        for c in range(2):
            k.mm(pt[:, c * 128:(c + 1) * 128], hsum[:, ti, c * 128:(c + 1) * 128], ident.v())
        o_ = oT[ti % 2]
        k.cp("act", o_.v(), rr(pt[:, 0:256], "p (c t) -> p c t", c=2))
        k.dma(MIXD[0:2, :, ti * 128:(ti + 1) * 128].rearrange("c p t -> p c t"), o_.h[:], reads=o_.res, writes=MIX_res)
    k.barrier()
    es.close()


RW_C = 0.6065306597126334


def phase_rwkv_proj(P, l, hT, w_in, prm, PRW, PRW_res):
    k = P.k
    nc = P.nc
    es = ExitStack()
    tile = lambda name, shape, dt=F32: T(es.enter_context(usb(nc, "t_" + name, shape, dt)), name)
    wrw = tile("wrw", [128, 8, 1536], BF16)
    load_w_bf16(P, wrw, w_in[l, :, 1040:2576], 1536)
    mu = tile("rwmu", [128, 12, 2]); k.ld(mu.v(), prm["rw_muT"][l])
    c0 = tile("rwc0", [128, 12])
    k.tt("dve", c0.v(), mu[:, :, 0], mu[:, :, 1], OP.add)
    k.ts("dve", c0.v(), c0.v(), -1.0, OP.mult, 1.0, OP.add)
    raw = [tile("rwraw%d" % i, [128, NT]) for i in range(2)]
    outb = [tile("rwout%d" % i, [128, NT]) for i in range(2)]
    ib = 0
    for ch in range(12):
        rw_ = raw[ch % 2]; ob = outb[ch % 2]
        for tb in range(0, NT, 512):
            n = min(512, NT - tb)
            pt = P.pb[ib % 2]; ib += 1
            for dc in range(8):
                k.mm(pt[:, 0:n], wrw[:, dc, ch * 128:(ch + 1) * 128], hT[:, dc, tb:tb + n], start=(dc == 0), stop=(dc == 7))
            k.cp("act", rw_[:, tb:tb + n], pt[:, 0:n])
        k.act(ob.v(), rw_.v(), AF.Identity, scale=c0[:, ch:ch + 1])
        for (s0, s1) in ((0, NCTX), (NCTX, NT)):
            k.stt("dve", ob[:, s0 + 1:s1], rw_[:, s0:s1 - 1], mu[:, ch, 0:1], ob[:, s0 + 1:s1], OP.mult, OP.add)
            k.stt("dve", ob[:, s0:s1 - 1], rw_[:, s0 + 1:s1], mu[:, ch, 1:2], ob[:, s0:s1 - 1], OP.mult, OP.add)
        k.dma(PRW[ch], ob.h[:], reads=ob.res, writes=PRW_res)
    k.barrier()
    es.close()


def phase_rwkv(P, l, prm, PRW, PRW_res, MIXT, MIXT_res):
    k = P.k
    nc = P.nc
    es = ExitStack()
    tile = lambda name, shape, dt=F32: T(es.enter_context(usb(nc, "t_" + name, shape, dt)), name)
    ident = P.ident
    pb = P.pb
    import os
    NSTEP = int(os.environ.get("RWSTEPS", NCH))
    NGRP = int(os.environ.get("RWGRPS", 3))
    tw = tile("rw_tw", [128, NT]); ad = tile("rw_ad", [128, NT]); sg = tile("rw_sg", [128, NT])
    k.dma(tw.h[:], PRW[9], reads=PRW_res, writes=tw.res)
    k.dma(ad.h[:], PRW[10], reads=PRW_res, writes=ad.res)
    k.dma(sg.h[:], PRW[11], reads=PRW_res, writes=sg.res)
    k.act(tw.v(), tw.v(), AF.Tanh)
    k.act(sg.v(), sg.v(), AF.Sigmoid)
    wup = tile("rw_wup_sb", [128, 384]); k.ld(wup.v(), prm["rw_wup"][l])
    aup = tile("rw_aup_sb", [128, 384]); k.ld(aup.v(), prm["rw_aup"][l])
    gup = tile("rw_gup_sb", [128, 384]); k.ld(gup.v(), prm["rw_gup"][l])
    rwc = tile("rw_cols_sb", [128, 3, 9]); k.ld(rwc.v(), prm["rw_cols"][l])
    omka = tile("rw_omka", [128, 3])
    k.ts("dve", omka.v(), rwc[:, :, 1], -1.0, OP.mult, 1.0, OP.add)
    BD2 = tile("rw_bd2", [128, 128]); k.ld(BD2.v(), P.din["c_BD2"])
    msk = tile("rw_mask", [128, 2, 320]); k.ld(msk.v(), P.din["c_rwmask"])
    rst = tile("rw_rst", [128, NCH, 64], BF16)
    k.memset("dve", rst.v(), 1.0); k.memset("dve", rst[:, :, 0:1], 0.0)
    b_r = tile("rw_r", [128, NT]); b_k = tile("rw_k", [128, NT]); b_v = tile("rw_v", [128, NT])
    b_kh = tile("rw_kh", [128, NT]); b_bs = tile("rw_bs", [128, NT])
    b_ys = tile("rw_ys", [128, NT])
    b_lw = tile("rw_lw", [128, NT]); b_a = tile("rw_a", [128, NT]); b_kt = tile("rw_kt", [128, NT])
    b_cl = tile("rw_cl", [128, NT]); b_At = tile("rw_At", [128, NT])
    gC = tile("rw_gC", [128, NCH])
    ST = tile("rw_ST", [128, 64])
    Mm = tile("rw_Mm", [128, 320])
    TT = tile("rw_TT", [128, 192])
    XT = [tile("rw_XT%d" % i, [128, 64]) for i in range(2)]
    Mj = [tile("rw_Mj%d" % i, [128, 64]) for i in range(2)]
    MjT = [tile("rw_MjT%d" % i, [128, 64]) for i in range(2)]
    HP = [slice(0, 64), slice(64, 128)]

    def blocks():
        for tb in range(0, NT, 512):
            yield tb, min(512, NT - tb)

    def bd_sum(dst, src, scale=1.0):
        for i, (tb, n) in enumerate(blocks()):
            pt = pb[i % 2]
            k.mm(pt[:, 0:n], BD2.v(), src[:, tb:tb + n])
            k.act(dst[:, tb:tb + n], pt[:, 0:n], AF.Identity, scale=scale)

    for g in range(NGRP):
        k.dma(b_r.h[:], PRW[g], reads=PRW_res, writes=b_r.res)
        k.dma(b_k.h[:], PRW[3 + g], reads=PRW_res, writes=b_k.res)
        k.dma(b_v.h[:], PRW[6 + g], reads=PRW_res, writes=b_v.res)
        gs = slice(g * 128, (g + 1) * 128)
        k.ts("dve", b_kh.v(), b_k.v(), rwc[:, g, 0:1], OP.mult)
        k.tt("pool", b_At.v(), b_kh.v(), b_kh.v(), OP.mult)
        bd_sum(b_cl, b_At)
        k.ts("dve", b_cl.v(), b_cl.v(), 1e-12, OP.add)
        k.act(b_cl.v(), b_cl.v(), AF.Sqrt)
        k.recip(b_cl.v(), b_cl.v())
        k.tt("dve", b_kh.v(), b_kh.v(), b_cl.v(), OP.mult)
        k.memset("pool", b_ys.v(), 0.0)
        for dr in range(2):
            D = slice(dr * 64, dr * 64 + 64)
            for i, (tb, n) in enumerate(blocks()):
                pt = pb[i % 2]
                k.mm(pt[:, 0:n], wup[D, gs], tw[D, tb:tb + n])
                k.act(b_lw[:, tb:tb + n], pt[:, 0:n], AF.Sigmoid, bias=rwc[:, g, 5 + dr:6 + dr])
            k.ts("dve", b_lw.v(), b_lw.v(), -RW_C, OP.mult)
            for i, (tb, n) in enumerate(blocks()):
                pt = pb[2 + i % 2]
                k.mm(pt[:, 0:n], aup[D, gs], ad[D, tb:tb + n])
                k.act(b_a[:, tb:tb + n], pt[:, 0:n], AF.Sigmoid, bias=rwc[:, g, 7 + dr:8 + dr])
            k.ts("dve", b_kt.v(), b_a.v(), rwc[:, g, 1:2], OP.mult, omka[:, g:g + 1], OP.add)
            k.tt("dve", b_kt.v(), b_kt.v(), b_k.v(), OP.mult)
            k.tt("pool", b_a.v(), b_a.v(), b_kh.v(), OP.mult)
            k.stt("dve", b_At.v(), b_r.v(), rwc[:, g, 2:3], b_kt.v(), OP.mult, OP.mult)
            for i, (tb, n) in enumerate(blocks()):
                pt = pb[i % 2]
                k.mm(pt[:, 0:n], BD2.v(), b_At[:, tb:tb + n])
                if dr == 0:
                    k.tt("dve", b_bs[:, tb:tb + n], pt[:, 0:n], b_v[:, tb:tb + n], OP.mult)
                else:
                    k.tt("dve", b_At[:, tb:tb + n], pt[:, 0:n], b_v[:, tb:tb + n], OP.mult)
            if dr == 1:
                k.tt("pool", b_bs.v(), b_bs.v(), b_At.v(), OP.add)
            k.op("dve", lambda e: e.tensor_tensor_scan(b_cl.h[:], rst.h[:].rearrange("p c s -> p (c s)"), b_lw.h[:], 0.0, OP.mult, OP.add),
                 rst.res + b_lw.res, b_cl.res)
            cl3 = rr(b_cl.v(), "p (c s) -> p c s", s=64)
            k.cp("dve", gC.v(), cl3[:, :, 63])
            if dr == 1:
                k.tt("dve", cl3, bc(V(gC.h[:].unsqueeze(2), gC.res), [128, NCH, 64]), cl3, OP.subtract)
                k.tt("dve", b_cl.v(), b_cl.v(), b_lw.v(), OP.add)
            k.act(gC.v(), gC.v(), AF.Exp)
            k.tt("dve", b_lw.v(), b_cl.v(), b_lw.v(), OP.subtract)
            k.act(b_lw.v(), b_lw.v(), AF.Exp)
            k.stt("dve", b_At.v(), b_kh.v(), -1.0, b_lw.v(), OP.mult, OP.mult)
            k.act(b_lw.v(), b_cl.v(), AF.Exp)
            k.tt("dve", b_lw.v(), b_lw.v(), b_r.v(), OP.mult)
            k.act(b_cl.v(), b_cl.v(), AF.Exp, scale=-1.0)
            k.tt("dve", b_a.v(), b_a.v(), b_cl.v(), OP.mult)
            k.tt("pool", b_kt.v(), b_kt.v(), b_cl.v(), OP.mult)
            b_Rt, b_Bt, b_Kt = b_lw, b_a, b_kt
            k.memset("dve", ST.v(), 0.0)
            order = ORD_F if dr == 0 else ORD_R
            pM, pT, pW, pX, pQ, pQT, pY, pS = pb
            for step in range(NSTEP):
                c = order[step]
                tok = slice(c * 64, c * 64 + 64)
                for hp in HP:
                    k.mm(pM[hp, 0:64], b_Bt[hp, tok], b_At[hp, tok])
                    k.mm(pM[hp, 64:128], b_Bt[hp, tok], b_Rt[hp, tok])
                    k.mm(pM[hp, 128:192], b_Kt[hp, tok], b_At[hp, tok])
                    k.mm(pM[hp, 192:256], b_Kt[hp, tok], b_Rt[hp, tok])
                    k.mm(pM[hp, 256:320], b_At[hp, tok], b_Bt[hp, tok])
                    k.mm(pT[hp, 0:64], b_v[hp, tok], ident[hp, hp])
                    k.mm(pT[hp, 64:128], b_Bt[hp, tok], ident[hp, hp])
                    k.mm(pT[hp, 128:192], b_Kt[hp, tok], ident[hp, hp])
                k.tt("dve", Mm.v(), pM[:, 0:320], msk[:, dr, :], OP.mult)
                k.cp("act", TT.v(), pT[:, 0:192])
                VT, BtT, KtT = TT[:, 0:64], TT[:, 64:128], TT[:, 128:192]
                M1, N1, M2, N2, M1T = [Mm[:, i * 64:(i + 1) * 64] for i in range(5)]
                for hp in HP:
                    k.mm(pW[hp, 0:64], b_At[hp, tok], ST[hp, :], start=True, stop=False)
                    k.mm(pW[hp, 0:64], V(M2.ap[hp], M2.res), V(VT.ap[hp], VT.res), start=False, stop=True)
                x = XT[0]
                k.cp("act", x.v(), pW[:, 0:64])
                mj, mjT = M1, M1T
                for j in range(6):
                    xn = XT[(j + 1) % 2]
                    for hp in HP:
                        k.mm(pX[hp, 0:64], V(mj.ap[hp], mj.res), x[hp, :])
                    k.tt("dve", xn.v(), pX[:, 0:64], x.v(), OP.add)
                    x = xn
                    if j < 5:
                        for hp in HP:
                            k.mm(pQ[hp, 0:64], V(mjT.ap[hp], mjT.res), V(mj.ap[hp], mj.res))
                            k.mm(pQT[hp, 0:64], V(mj.ap[hp], mj.res), V(mjT.ap[hp], mjT.res))
                        nmj = Mj[j % 2]; nmjT = MjT[j % 2]
                        k.cp("act", nmj.v(), pQ[:, 0:64])
                        k.cp("dve", nmjT.v(), pQT[:, 0:64])
                        mj, mjT = nmj.v(), nmjT.v()
                UT = x
                for hp in HP:
                    k.mm(pY[hp, 0:64], ST[hp, :], b_Rt[hp, tok], start=True, stop=False)
                    k.mm(pY[hp, 0:64], UT[hp, :], V(N1.ap[hp], N1.res), start=False, stop=False)
                    k.mm(pY[hp, 0:64], V(VT.ap[hp], VT.res), V(N2.ap[hp], N2.res), start=False, stop=True)
                k.tt("dve", b_ys[:, tok], b_ys[:, tok], pY[:, 0:64], OP.add)
                for hp in HP:
                    k.mm(pS[hp, 0:64], V(BtT.ap[hp], BtT.res), UT[hp, :], start=True, stop=False)
                    k.mm(pS[hp, 0:64], V(KtT.ap[hp], KtT.res), V(VT.ap[hp], VT.res), start=False, stop=True)
                k.tt("dve", ST.v(), ST.v(), pS[:, 0:64], OP.add)
                k.ts("dve", ST.v(), ST.v(), gC[:, c:c + 1], OP.mult)
        if "rw_ys" in P.debug and g == 0:
            P.dump("rw_ys", b_ys.v())
        bd_sum(b_cl, b_ys, 1.0 / 64)
        k.tt("dve", b_ys.v(), b_ys.v(), b_cl.v(), OP.subtract)
        k.tt("pool", b_At.v(), b_ys.v(), b_ys.v(), OP.mult)
        bd_sum(b_cl, b_At, 1.0 / 64)
        k.ts("dve", b_cl.v(), b_cl.v(), 64e-5, OP.add)
        k.act(b_cl.v(), b_cl.v(), AF.Sqrt)
        k.recip(b_cl.v(), b_cl.v())
        k.tt("dve", b_ys.v(), b_ys.v(), b_cl.v(), OP.mult)
        k.ts("dve", b_ys.v(), b_ys.v(), rwc[:, g, 3:4], OP.mult, rwc[:, g, 4:5], OP.add)
        k.tt("dve", b_ys.v(), b_ys.v(), b_bs.v(), OP.add)
        for i, (tb, n) in enumerate(blocks()):
            pt = pb[i % 2]
            k.mm(pt[:, 0:n], gup[:, gs], sg[:, tb:tb + n])
            k.tt("dve", b_ys[:, tb:tb + n], b_ys[:, tb:tb + n], pt[:, 0:n], OP.mult)
        k.dma(MIXT[2 + g], b_ys.h[:], reads=b_ys.res, writes=MIXT_res)
    k.barrier()
    es.close()


MLA_SCALE = 96 ** -0.5


def phase_mla(P, l, hT, w_in, prm, MIXT, MIXT_res, need_ctx=True):
    k = P.k
    nc = P.nc
    es = ExitStack()
    tile = lambda name, shape, dt=F32: T(es.enter_context(usb(nc, "t_" + name, shape, dt)), name)
    ident = P.ident
    pb = P.pb
    wat = tile("wat", [128, 8, 672], BF16)
    load_w_bf16(P, wat, w_in[l, :, 2576:3248], 672)
    qup = tile("mla_qup", [128, 3, 576], BF16)
    kvup = tile("mla_kvup", [128, 2, 768], BF16)
    stg = tile("mla_stg", [128, 3, 768])
    k.ld(stg[:, 0:3, 0:576], prm["mla_q_up"][l].rearrange("(c p) n -> p c n", p=128))
    k.cp("dve", qup.v(), stg[:, 0:3, 0:576])
    k.ld(stg[:, 0:2, :], prm["mla_kv_up"][l].rearrange("(c p) n -> p c n", p=128))
    k.cp("dve", kvup.v(), stg[:, 0:2, :])
    gq = tile("mla_gq", [128, 384]); k.ld(gq.v(), prm["mla_gq"][l])
    gkv = tile("mla_gkv", [128, 256]); k.ld(gkv.v(), prm["mla_gkv"][l])
    QT = tile("mla_QT", [96, 6, NT], BF16)
    KT = tile("mla_KT", [96, 6, NT], BF16)
    Vt = tile("mla_V", [128, NTILE, 6, 65], BF16)
    k.memset("pool", Vt.v(), 1.0)
    pa = tile("mla_pa", [128, 672])
    junk = tile("mla_junk", [128, 384])
    st = tile("mla_st", [128, 8])
    cn = tile("mla_cn", [128, 640])
    cnT = tile("mla_cnT", [128, 5, 128], BF16)
    qt = tile("mla_q", [128, 6, 96])
    kvt = tile("mla_kv", [128, 6, 128])
    kf = tile("mla_kf", [128, 6, 96])
    rope = tile("mla_rope", [128, 32])
    rt = tile("mla_rt", [128, 4, 6, 2, 8])
    for ti in range(NTILE):
        tk = slice(ti * 128, (ti + 1) * 128)
        p0, p1 = pb[0], pb[1]
        for dc in range(8):
            k.mm(p0[:, 0:512], hT[:, dc, tk], wat[:, dc, 0:512], start=(dc == 0), stop=(dc == 7))
        for dc in range(8):
            k.mm(p1[:, 0:160], hT[:, dc, tk], wat[:, dc, 512:672], start=(dc == 0), stop=(dc == 7))
        k.cp("act", pa[:, 0:512], p0[:, 0:512])
        k.cp("dve", pa[:, 512:672], p1[:, 0:160])
        k.act(junk[:, 0:384], pa[:, 0:384], AF.Square, accum=st[:, 0:1])
        k.act(junk[:, 0:256], pa[:, 384:640], AF.Square, accum=st[:, 1:2])
        k.ts("dve", st[:, 2:3], st[:, 0:1], 1.0 / 384, OP.mult, LN_EPS, OP.add)
        k.ts("dve", st[:, 3:4], st[:, 1:2], 1.0 / 256, OP.mult, LN_EPS, OP.add)
        k.act(st[:, 4:6], st[:, 2:4], AF.Sqrt)
        k.recip(st[:, 6:8], st[:, 4:6])
        k.stt("dve", cn[:, 0:384], pa[:, 0:384], st[:, 6:7], gq.v(), OP.mult, OP.mult)
        k.stt("dve", cn[:, 384:640], pa[:, 384:640], st[:, 7:8], gkv.v(), OP.mult, OP.mult)
        p2, p3 = pb[2], pb[3]
        for c in range(4):
            k.mm(p2[:, c * 128:(c + 1) * 128], cn[:, c * 128:(c + 1) * 128], ident.v())
        k.mm(p3[:, 0:128], cn[:, 512:640], ident.v())
        k.cp("act", cnT[:, 0:4, :], rr(p2.v(), "p (c t) -> p c t", c=4))
        k.cp("dve", cnT[:, 4, :], p3[:, 0:128])
        p4, p5, p6, p7 = pb[4], pb[5], pb[6], pb[7]
        for c in range(3):
            k.mm(p4[:, 0:512], cnT[:, c, :], qup[:, c, 0:512], start=(c == 0), stop=(c == 2))
        for c in range(3):
            k.mm(p5[:, 0:64], cnT[:, c, :], qup[:, c, 512:576], start=(c == 0), stop=(c == 2))
        for c in range(2):
            k.mm(p6[:, 0:512], cnT[:, 3 + c, :], kvup[:, c, 0:512], start=(c == 0), stop=(c == 1))
        for c in range(2):
            k.mm(p7[:, 0:256], cnT[:, 3 + c, :], kvup[:, c, 512:768], start=(c == 0), stop=(c == 1))
        qf = rr(qt.v(), "p h e -> p (h e)")
        k.cp("act", qf[:, 0:512], p4[:, 0:512])
        k.cp("dve", qf[:, 512:576], p5[:, 0:64])
        kvf = rr(kvt.v(), "p h e -> p (h e)")
        k.cp("act", kvf[:, 0:512], p6[:, 0:512])
        k.cp("dve", kvf[:, 512:768], p7[:, 0:256])
        k.cp("pool", kf[:, :, 0:64], kvt[:, :, 0:64])
        k.cp("pool", Vt[:, ti, :, 0:64], kvt[:, :, 64:128])
        kr = pa[:, 640:672]
        if ti >= NCTX // 128:
            k.ld(rope.v(), P.din["c_rope"][(ti - NCTX // 128) * 128:(ti - NCTX // 128 + 1) * 128, :])
            cosv = rr(rope[:, 0:16], "p (a f) -> p a f", a=2)
            sinv = rr(rope[:, 16:32], "p (a f) -> p a f", a=2)
            for (xv, H_) in ((rr(qt[:, :, 64:96], "p h (a s f) -> p h a s f", a=2, s=2), 6),
                             (rr(V(kr.ap.unsqueeze(1), kr.res), "p h (a s f) -> p h a s f", a=2, s=2), 1)):
                x1 = xv[:, :, :, 0, :]; x2 = xv[:, :, :, 1, :]
                cb = bc(V(cosv.ap.unsqueeze(1), cosv.res), [128, H_, 2, 8])
                sb_ = bc(V(sinv.ap.unsqueeze(1), sinv.res), [128, H_, 2, 8])
                t1, t2, t3, t4 = [rt[:, i, 0:H_, :, :] for i in range(4)]
                k.tt("dve", t1, x1, cb, OP.mult)
                k.tt("pool", t2, x2, sb_, OP.mult)
                k.tt("dve", t3, x2, cb, OP.mult)
                k.tt("pool", t4, x1, sb_, OP.mult)
                k.tt("dve", x1, t1, t2, OP.subtract)
                k.tt("dve", x2, t3, t4, OP.add)
        k.cp("dve", kf[:, :, 64:96], bc(V(kr.ap.unsqueeze(1), kr.res), [128, 6, 32]))
        for (src, dstT, pp) in ((qt, QT, (pb[0], pb[1])), (kf, KT, (pb[2], pb[3]))):
            for half in range(2):
                pt = pp[half]
                for j in range(3):
                    h = half * 3 + j
                    k.mm(pt[0:96, j * 128:(j + 1) * 128], src[:, h, :], ident.v())
                k.cp("act" if half else "dve", dstT[:, half * 3:half * 3 + 3, tk],
                     rr(pt[0:96, 0:384], "p (j t) -> p j t", j=3))
    if "mla_QT" in P.debug:
        P.dump("mla_QT", QT.v()); P.dump("mla_KT", KT.v())
    E = [tile("mla_E%d" % i, [128, 512], BF16) for i in range(2)]
    P.mla_oT = [tile("mla_oT%d" % i, [128, 3, 128]) for i in range(2)]
    osb = tile("mla_o", [128, 4, 6, 64])
    rs = tile("mla_rs", [128, 4])
    qblocks = [(NCTX + i * 512, 512, list(range(NTILE))) for i in range(4)]
    if need_ctx:
        qblocks.append((0, 256, [0, 1]))
    ei = 0
    for (q0, qn, ktiles) in qblocks:
        nsub = qn // 128
        for h in range(6):
            for kidx, kt_ in enumerate(ktiles):
                ps = pb[4 + ei % 2]
                e_ = E[ei % 2]; ei += 1
                k.mm(ps[:, 0:qn], KT[:, h, kt_ * 128:(kt_ + 1) * 128], QT[:, h, q0:q0 + qn])
                k.act(e_[:, 0:qn], ps[:, 0:qn], AF.Exp, scale=MLA_SCALE)
                for j in range(nsub):
                    k.mm(pb[j][:, 0:65], e_[:, j * 128:(j + 1) * 128], Vt[:, kt_, h, :],
                         start=(kidx == 0), stop=(kidx == len(ktiles) - 1))
            for j in range(nsub):
                k.recip(rs[:, j:j + 1], pb[j][:, 64:65])
                k.ts("dve", osb[:, j, h, :], pb[j][:, 0:64], rs[:, j:j + 1], OP.mult)
        for j in range(nsub):
            pt = pb[6 + j % 2]
            of = rr(osb[:, j, :, :], "p h e -> p (h e)")
            for c in range(3):
                k.mm(pt[:, c * 128:(c + 1) * 128], of[:, c * 128:(c + 1) * 128], ident.v())
            ot = tile("mla_oT_%d_%d" % (q0, j), [128, 3, 128]) if False else P.mla_oT[j % 2]
            k.cp("act", ot.v(), rr(pt[:, 0:384], "p (c t) -> p c t", c=3))
            t0_ = q0 + j * 128
            k.dma(MIXT[5:8, :, t0_:t0_ + 128].rearrange("c p t -> p c t"), ot.h[:], reads=ot.res, writes=MIXT_res)
    k.barrier()
    es.close()


def phase_mixout(P, l, w_out, prm, MIXT, MIXT_res, src_ap, src_res, XD, XD_res, MODD, MOD_res, need_ctx=True):
    k = P.k
    nc = P.nc
    es = ExitStack()
    tile = lambda name, shape, dt=F32: T(es.enter_context(usb(nc, "t_" + name, shape, dt)), name)
    pb = P.pb
    wout = tile("wout", [128, 8, 1024], BF16)
    load_w_bf16(P, wout, w_out[l], 1024)
    ga = [tile("mo_ga%d" % s, [128, D]) for s in range(2)]
    for s in range(2):
        k.dma(ga[s].h[:], MODD[s, MOD_GA], reads=MOD_res, writes=ga[s].res)
    lng = tile("mo_lng", [128, D]); k.ld(lng.v(), prm["ln_mix_g"][l])
    lnb = tile("mo_lnb", [128, D]); k.ld(lnb.v(), prm["ln_mix_b"][l])
    mt = [tile("mo_mt%d" % i, [128, 8, 128]) for i in range(2)]
    mb = [tile("mo_mb%d" % i, [128, 8, 128], BF16) for i in range(2)]
    xt = [tile("mo_x%d" % i, [128, D]) for i in range(2)]
    ut = [tile("mo_u%d" % i, [128, D]) for i in range(2)]
    st = [tile("mo_st%d" % i, [128, 16]) for i in range(2)]
    t0 = 0 if need_ctx else NCTX // 128
    for ti in range(t0, NTILE):
        s = 1 if ti < NCTX // 128 else 0
        m_ = mt[ti % 2]; b_ = mb[ti % 2]; x = xt[ti % 2]; u = ut[ti % 2]; st_ = st[ti % 2]
        k.dma(m_.h[:], MIXT[:, :, ti * 128:(ti + 1) * 128].rearrange("c p t -> p c t"), reads=MIXT_res, writes=m_.res)
        k.dma(x.h[:], src_ap[ti * 128:(ti + 1) * 128, :], reads=src_res[ti], writes=x.res)
        k.cp("pool", b_.v(), m_.v())
        for half in range(2):
            pt = pb[(ti * 2 + half) % 4]
            for c in range(8):
                k.mm(pt.v(), b_[:, c, :], wout[:, c, half * 512:(half + 1) * 512], start=(c == 0), stop=(c == 7))
            k.tt("dve", u[:, half * 512:(half + 1) * 512], pt.v(), ga[s][:, half * 512:(half + 1) * 512], OP.mult)
        k.stt("dve", u.v(), x.v(), ALPHA, u.v(), OP.mult, OP.add)
        mean, rstd, nmr = ln_stats(P, u.v(), st_)
        k.act(u.v(), u.v(), AF.Identity, bias=nmr, scale=rstd)
        k.tt("pool", u.v(), u.v(), lng.v(), OP.mult)
        k.tt("dve", u.v(), u.v(), lnb.v(), OP.add)
        k.dma(XD[ti * 128:(ti + 1) * 128, :], u.h[:], reads=u.res, writes=XD_res[ti])
    k.barrier()
    es.close()


def phase_peer(P, l, prm, peer_w_q, peer_keys, peer_u, peer_v, XD, XD_res, dst_ap, MODD, MOD_res, need_ctx=True):
    import os
    k = P.k
    nc = P.nc
    es = ExitStack()
    tile = lambda name, shape, dt=F32: T(es.enter_context(usb(nc, "t_" + name, shape, dt)), name)
    pb = P.pb
    ident = P.ident
    NSLOT = int(os.environ.get("PEER_SLOTS", 128))
    wq = tile("pe_wq", [128, 8, 2048])
    for c0 in range(0, 2048, 512):
        k.ld(wq[:, :, c0:c0 + 512], peer_w_q[l, :, c0:c0 + 512].rearrange("(c p) n -> p c n", p=128))
    keysT = tile("pe_keysT", [128, 16, 128])
    kst = tile("pe_kst", [128, 4, 128])
    for g4 in range(4):
        k.ld(kst.v(), peer_keys[l].rearrange("h p k d -> k (h p) d")[:, g4 * 4:(g4 + 1) * 4, :])
        pt = pb[g4 % 2]
        for j in range(4):
            k.tr(pt[:, j * 128:(j + 1) * 128], kst[:, j, :], ident.v())
        k.cp("act", keysT[:, g4 * 4:(g4 + 1) * 4, :], rr(pt.v(), "p (j t) -> p j t", j=4))
    mods = {}
    for s in ([0, 1] if need_ctx else [0]):
        for j in (MOD_SHF, MOD_SCF, MOD_GF):
            mods[(s, j)] = tile("pe_mod%d_%d" % (s, j), [128, D])
            k.dma(mods[(s, j)].h[:], MODD[s, j], reads=MOD_res, writes=mods[(s, j)].res)
        k.ts("pool", mods[(s, MOD_SCF)].v(), mods[(s, MOD_SCF)].v(), 1.0, OP.add)
    lng = tile("pe_lng", [128, D]); k.ld(lng.v(), prm["ln_ffn_g"][l])
    lnb = tile("pe_lnb", [128, D]); k.ld(lnb.v(), prm["ln_ffn_b"][l])
    iota16 = tile("pe_iota", [128, 16]); k.ld(iota16.v(), P.din["c_iota16"])
    x = tile("pe_x", [128, D]); h = tile("pe_h", [128, D]); st = tile("pe_st", [128, 16])
    hT = tile("pe_hT", [128, 8, 128])
    qT = tile("pe_qT", [128, 16, 128])
    sc = tile("pe_s", [128, 16, 128])
    wk = tile("pe_wk", [128, 128])
    vals = tile("pe_vals", [128, 16, 16])
    idxu = tile("pe_idxu", [128, 16, 16], U32)
    idxf = tile("pe_idxf", [128, 16, 16])
    cand = tile("pe_cand", [128, 8, 16, 16])
    wk2 = tile("pe_wk2", [128, 256])
    best = tile("pe_best", [128, 8, 16])
    posu = tile("pe_posu", [128, 8, 16], U32)
    posu2 = tile("pe_posu2", [128, 2, 8, 16], U32)
    cu32 = tile("pe_cu32", [128, 2], U32); k.ld(cu32.v(), P.din["c_u32"])
    pa_ = tile("pe_pa", [128, 8, 16]); pb_ = tile("pe_pb", [128, 8, 16])
    oh = tile("pe_oh", [128, 8, 16, 16])
    eidf = tile("pe_eidf", [128, 8, 16]); eid2 = tile("pe_eid2", [128, 8, 16])
    eidu = tile("pe_eidu", [128, 128], U32)
    gate = tile("pe_gate", [128, 8, 16]); gs = tile("pe_gs", [128, 8])
    actv = tile("pe_act", [128, 128]); t1 = tile("pe_t1", [128, 128]); wgt = tile("pe_wgt", [128, 128])
    NB = 4
    ug = [tile("pe_ug%d" % i, [128, D]) for i in range(NB)]
    vg = ug
    junk = tile("pe_junk", [128, D])
    acc = tile("pe_acc", [128, D])
    t0 = 0 if need_ctx else NCTX // 128
    NTL = int(os.environ.get("PEER_TILES", NTILE))
    for ti in range(t0, min(NTILE, t0 + NTL)):
        s = 1 if ti < NCTX // 128 else 0
        k.dma(x.h[:], XD[ti * 128:(ti + 1) * 128, :], reads=XD_res[ti], writes=x.res)
        mean, rstd, nmr = ln_stats(P, x.v(), st)
        k.act(h.v(), x.v(), AF.Identity, bias=nmr, scale=rstd)
        k.tt("dve", h.v(), h.v(), mods[(s, MOD_SCF)].v(), OP.mult)
        k.tt("pool", h.v(), h.v(), mods[(s, MOD_SHF)].v(), OP.add)
        for g4 in range(2):
            pt = pb[g4]
            for j in range(4):
                k.tr(pt[:, j * 128:(j + 1) * 128], h[:, (g4 * 4 + j) * 128:(g4 * 4 + j + 1) * 128], ident.v())
            k.cp("act" if g4 else "dve", hT[:, g4 * 4:(g4 + 1) * 4, :], rr(pt.v(), "p (j t) -> p j t", j=4))
        for g4 in range(4):
            pt = pb[2 + g4 % 2]
            for j in range(4):
                c16 = g4 * 4 + j
                for dc in range(8):
                    k.mm(pt[:, j * 128:(j + 1) * 128], wq[:, dc, c16 * 128:(c16 + 1) * 128], hT[:, dc, :], start=(dc == 0), stop=(dc == 7))
            k.cp("act" if g4 % 2 else "dve", qT[:, g4 * 4:(g4 + 1) * 4, :], rr(pt.v(), "p (j t) -> p j t", j=4))
        for g4 in range(4):
            pt = pb[4 + g4 % 2]
            for j in range(4):
                c16 = g4 * 4 + j
                k.mm(pt[:, j * 128:(j + 1) * 128], qT[:, c16, :], keysT[:, c16, :])
            k.cp("act" if g4 % 2 else "dve", sc[:, g4 * 4:(g4 + 1) * 4, :], rr(pt.v(), "p (j t) -> p j t", j=4))
        for c16 in range(16):
            k.op("dve", lambda e: e.max(vals.h[:, c16, 0:8], sc.h[:, c16, :]), sc.res, vals.res)
            k.op("dve", lambda e: e.max_index(idxu.h[:, c16, 0:8], vals.h[:, c16, 0:8], sc.h[:, c16, :]), sc.res + vals.res, idxu.res)
            k.op("dve", lambda e: e.match_replace(wk.h[:], vals.h[:, c16, 0:8], sc.h[:, c16, :], -1e30), sc.res + vals.res, wk.res)
            k.op("dve", lambda e: e.max(vals.h[:, c16, 8:16], wk.h[:]), wk.res, vals.res)
            k.op("dve", lambda e: e.max_index(idxu.h[:, c16, 8:16], vals.h[:, c16, 8:16], wk.h[:]), wk.res + vals.res, idxu.res)
        k.cp("dve", idxf.v(), idxu.v())
        v4 = rr(vals.v(), "p (h q) a -> p h q a", q=2)
        i4 = rr(idxf.v(), "p (h q) a -> p h q a", q=2)
        v1 = v4[:, :, 0, :]; v2 = v4[:, :, 1, :]
        k.tt("dve", cand.v(), bc(V(v1.ap.unsqueeze(3), v1.res), [128, 8, 16, 16]), bc(V(v2.ap.unsqueeze(2), v2.res), [128, 8, 16, 16]), OP.add)
        for hh in range(8):
            cf = rr(cand[:, hh, :, :], "p a b -> p (a b)")
            k.op("dve", lambda e: e.max(best.h[:, hh, 0:8], cf.ap), cand.res, best.res)
            k.op("dve", lambda e: e.max_index(posu.h[:, hh, 0:8], best.h[:, hh, 0:8], cf.ap), cand.res + best.res, posu.res)
            k.op("dve", lambda e: e.match_replace(wk2.h[:], best.h[:, hh, 0:8], cf.ap, -1e30), cand.res + best.res, wk2.res)
            k.op("dve", lambda e: e.max(best.h[:, hh, 8:16], wk2.h[:]), wk2.res, best.res)
            k.op("dve", lambda e: e.max_index(posu.h[:, hh, 8:16], best.h[:, hh, 8:16], wk2.h[:]), wk2.res + best.res, posu.res)
        k.ts("dve", posu2[:, 0, :, :], posu.v(), cu32[:, 0:1], OP.bitwise_and)
        k.ts("dve", posu2[:, 1, :, :], posu.v(), cu32[:, 1:2], OP.logical_shift_right)
        k.cp("dve", pb_.v(), posu2[:, 0, :, :])
        k.cp("dve", pa_.v(), posu2[:, 1, :, :])
        io4 = bc(V(iota16.h[:].unsqueeze(1).unsqueeze(1), iota16.res), [128, 8, 16, 16])
        for (pp, isrc, dst) in ((pa_, i4[:, :, 0, :], eidf), (pb_, i4[:, :, 1, :], eid2)):
            k.tt("dve", oh.v(), io4, bc(V(pp.h[:].unsqueeze(3), pp.res), [128, 8, 16, 16]), OP.is_equal)
            k.tt("dve", oh.v(), oh.v(), bc(V(isrc.ap.unsqueeze(2), isrc.res), [128, 8, 16, 16]), OP.mult)
            k.red("dve", dst.v(), oh.v(), OP.add)
        k.stt("dve", eidf.v(), eidf.v(), 128.0, eid2.v(), OP.mult, OP.add)
        k.cp("dve", eidu.v(), rr(eidf.v(), "p h k -> p (h k)"))
        k.tt("dve", gate.v(), best.v(), bc(best[:, :, 0:1], [128, 8, 16]), OP.subtract)
        k.act(gate.v(), gate.v(), AF.Exp)
        k.red("dve", gs.v(), gate.v(), OP.add)
        k.recip(gs.v(), gs.v())
        k.tt("dve", gate.v(), gate.v(), bc(V(gs.h[:].unsqueeze(2), gs.res), [128, 8, 16]), OP.mult)
        k.memset("dve", actv.v(), 0.0)
        for sl in range(NSLOT):
            u_ = ug[sl % NB]
            k.dma_custom("pool", lambda e: e.indirect_dma_start(out=u_.h[:], out_offset=None, in_=peer_u.rearrange("l e d -> (l e) d"),
                         in_offset=bass.IndirectOffsetOnAxis(ap=eidu.h[:, sl:sl + 1], axis=0), element_offset=l * 16384 * D), eidu.res, u_.res)
            k.op("dve", lambda e: e.scalar_tensor_tensor(junk.h[:], u_.h[:], 1.0, h.h[:], op0=OP.mult, op1=OP.mult, accum_out=actv.h[:, sl:sl + 1]),
                 u_.res + h.res, junk.res + actv.res)
        k.tt("dve", t1.v(), actv.v(), actv.v(), OP.mult)
        k.ts("dve", t1.v(), t1.v(), 0.044715, OP.mult, 1.0, OP.add)
        k.tt("dve", t1.v(), t1.v(), actv.v(), OP.mult)
        k.act(t1.v(), t1.v(), AF.Sigmoid, scale=1.5957691216057308)
        k.tt("dve", wgt.v(), t1.v(), actv.v(), OP.mult)
        k.tt("dve", wgt.v(), wgt.v(), rr(gate.v(), "p h k -> p (h k)"), OP.mult)
        k.memset("pool", acc.v(), 0.0)
        for sl in range(NSLOT):
            v_ = vg[sl % NB]
            k.dma_custom("pool", lambda e: e.indirect_dma_start(out=v_.h[:], out_offset=None, in_=peer_v.rearrange("l e d -> (l e) d"),
                         in_offset=bass.IndirectOffsetOnAxis(ap=eidu.h[:, sl:sl + 1], axis=0), element_offset=l * 16384 * D), eidu.res, v_.res)
            k.stt("dve", acc.v(), v_.v(), wgt[:, sl:sl + 1], acc.v(), OP.mult, OP.add)
        if "peer_y" in P.debug:
            k.dma(P.dout["peer_y"][ti * 128:(ti + 1) * 128, :], acc.h[:], reads=acc.res)
        k.tt("dve", acc.v(), acc.v(), mods[(s, MOD_GF)].v(), OP.mult)
        k.stt("dve", acc.v(), x.v(), ALPHA, acc.v(), OP.mult, OP.add)
        mean, rstd, nmr = ln_stats(P, acc.v(), st)
        k.act(acc.v(), acc.v(), AF.Identity, bias=nmr, scale=rstd)
        k.tt("pool", acc.v(), acc.v(), lng.v(), OP.mult)
        k.tt("dve", acc.v(), acc.v(), lnb.v(), OP.add)
        d_ap, d_res = dst_ap(ti)
        k.dma(d_ap, acc.h[:], reads=acc.res, writes=d_res)
    k.barrier()
    es.close()


def phase_peer_route(P, l, prm, peer_w_q, peer_keys, XD, XD_res, MODD, MOD_res, RTD, RT_res, HFD, HF_res, need_ctx=True, prep=None):
    import os
    k = P.k
    nc = P.nc
    es = ExitStack()
    tile = lambda name, shape, dt=F32: T(es.enter_context(usb(nc, "t_" + name, shape, dt)), name)
    pb = P.pb
    ident = P.ident
    NSLOT = int(os.environ.get("PEER_SLOTS", 128))
    wq = tile("pe_wq", [128, 8, 2048])
    for c0 in range(0, 2048, 512):
        k.ld(wq[:, :, c0:c0 + 512], peer_w_q[l, :, c0:c0 + 512].rearrange("(c p) n -> p c n", p=128))
    keysT = tile("pe_keysT", [128, 16, 128])
    kst = tile("pe_kst", [128, 4, 128])
    for g4 in range(4):
        k.ld(kst.v(), peer_keys[l].rearrange("h p k d -> k (h p) d")[:, g4 * 4:(g4 + 1) * 4, :])
        pt = pb[g4 % 2]
        for j in range(4):
            k.tr(pt[:, j * 128:(j + 1) * 128], kst[:, j, :], ident.v())
        k.cp("act", keysT[:, g4 * 4:(g4 + 1) * 4, :], rr(pt.v(), "p (j t) -> p j t", j=4))
    mods = {}
    for s in ([0, 1] if need_ctx else [0]):
        for j in (MOD_SHF, MOD_SCF):
            mods[(s, j)] = tile("pe_mod%d_%d" % (s, j), [128, D])
            k.dma(mods[(s, j)].h[:], MODD[s, j], reads=MOD_res, writes=mods[(s, j)].res)
        k.ts("pool", mods[(s, MOD_SCF)].v(), mods[(s, MOD_SCF)].v(), 1.0, OP.add)
    rtT = tile("pe_rtT", [128, 3, 128]); hTb = tile("pe_hTb", [128, 8, 128], BF16)
    iota16 = tile("pe_iota", [128, 16]); k.ld(iota16.v(), P.din["c_iota16"])
    x = tile("pe_x", [128, D]); h = tile("pe_h", [128, D]); st = tile("pe_st", [128, 16])
    hT = tile("pe_hT", [128, 8, 128])
    qT = tile("pe_qT", [128, 16, 128])
    sc = tile("pe_s", [128, 16, 128])
    wk = tile("pe_wk", [128, 128])
    vals = tile("pe_vals", [128, 16, 16])
    idxu = tile("pe_idxu", [128, 16, 16], U32)
    idxf = tile("pe_idxf", [128, 16, 16])
    cand = tile("pe_cand", [128, 8, 16, 16])
    wk2 = tile("pe_wk2", [128, 256])
    best = tile("pe_best", [128, 8, 16])
    posu = tile("pe_posu", [128, 8, 16], U32)
    posu2 = tile("pe_posu2", [128, 2, 8, 16], U32)
    cu32 = tile("pe_cu32", [128, 2], U32); k.ld(cu32.v(), P.din["c_u32"])
    pa_ = tile("pe_pa", [128, 8, 16]); pb_ = tile("pe_pb", [128, 8, 16])
    oh = tile("pe_oh", [128, 8, 16, 16])
    eidf = tile("pe_eidf", [128, 8, 16]); eid2 = tile("pe_eid2", [128, 8, 16])
    eidu = tile("pe_eidu", [128, 128], U32)
    gate = tile("pe_gate", [128, 8, 16]); gs = tile("pe_gs", [128, 8])
    actv = tile("pe_act", [128, 128]); t1 = tile("pe_t1", [128, 128]); wgt = tile("pe_wgt", [128, 128])
    t0 = 0 if need_ctx else NCTX // 128
    NTL = int(os.environ.get("PEER_TILES", NTILE))
    pst = prep_alloc(P, tile) if prep is not None else None
    nprep = 0
    tiles = list(range(t0, min(NTILE, t0 + NTL)))
    for ti in tiles:
        s = 1 if ti < NCTX // 128 else 0
        if prep is not None:
            tgt = 128 if ti == tiles[-1] else min(128, (tiles.index(ti) + 1) * 8)
            while nprep < tgt:
                prep_chunk(P, pst, l, nprep, prep[0], prep[1], prep[2], prep[3], prep[4], [pb[7]])
                nprep += 1
        k.dma(x.h[:], XD[ti * 128:(ti + 1) * 128, :], reads=XD_res[ti], writes=x.res)
        mean, rstd, nmr = ln_stats(P, x.v(), st)
        k.act(h.v(), x.v(), AF.Identity, bias=nmr, scale=rstd)
        k.tt("dve", h.v(), h.v(), mods[(s, MOD_SCF)].v(), OP.mult)
        k.tt("pool", h.v(), h.v(), mods[(s, MOD_SHF)].v(), OP.add)
        for g4 in range(2):
            pt = pb[g4]
            for j in range(4):
                k.tr(pt[:, j * 128:(j + 1) * 128], h[:, (g4 * 4 + j) * 128:(g4 * 4 + j + 1) * 128], ident.v())
            k.cp("act" if g4 else "dve", hT[:, g4 * 4:(g4 + 1) * 4, :], rr(pt.v(), "p (j t) -> p j t", j=4))
        for g4 in range(4):
            pt = pb[2 + g4 % 2]
            for j in range(4):
                c16 = g4 * 4 + j
                for dc in range(8):
                    k.mm(pt[:, j * 128:(j + 1) * 128], wq[:, dc, c16 * 128:(c16 + 1) * 128], hT[:, dc, :], start=(dc == 0), stop=(dc == 7))
            k.cp("act" if g4 % 2 else "dve", qT[:, g4 * 4:(g4 + 1) * 4, :], rr(pt.v(), "p (j t) -> p j t", j=4))
        for g4 in range(4):
            pt = pb[4 + g4 % 2]
            for j in range(4):
                c16 = g4 * 4 + j
                k.mm(pt[:, j * 128:(j + 1) * 128], qT[:, c16, :], keysT[:, c16, :])
            k.cp("act" if g4 % 2 else "dve", sc[:, g4 * 4:(g4 + 1) * 4, :], rr(pt.v(), "p (j t) -> p j t", j=4))
        for c16 in range(16):
            k.op("dve", lambda e: e.max(vals.h[:, c16, 0:8], sc.h[:, c16, :]), sc.res, vals.res)
            k.op("dve", lambda e: e.max_index(idxu.h[:, c16, 0:8], vals.h[:, c16, 0:8], sc.h[:, c16, :]), sc.res + vals.res, idxu.res)
            k.op("dve", lambda e: e.match_replace(wk.h[:], vals.h[:, c16, 0:8], sc.h[:, c16, :], -1e30), sc.res + vals.res, wk.res)
            k.op("dve", lambda e: e.max(vals.h[:, c16, 8:16], wk.h[:]), wk.res, vals.res)
            k.op("dve", lambda e: e.max_index(idxu.h[:, c16, 8:16], vals.h[:, c16, 8:16], wk.h[:]), wk.res + vals.res, idxu.res)
        k.cp("dve", idxf.v(), idxu.v())
        v4 = rr(vals.v(), "p (h q) a -> p h q a", q=2)
        i4 = rr(idxf.v(), "p (h q) a -> p h q a", q=2)
        v1 = v4[:, :, 0, :]; v2 = v4[:, :, 1, :]
        k.tt("dve", cand.v(), bc(V(v1.ap.unsqueeze(3), v1.res), [128, 8, 16, 16]), bc(V(v2.ap.unsqueeze(2), v2.res), [128, 8, 16, 16]), OP.add)
        for hh in range(8):
            cf = rr(cand[:, hh, :, :], "p a b -> p (a b)")
            k.op("dve", lambda e: e.max(best.h[:, hh, 0:8], cf.ap), cand.res, best.res)
            k.op("dve", lambda e: e.max_index(posu.h[:, hh, 0:8], best.h[:, hh, 0:8], cf.ap), cand.res + best.res, posu.res)
            k.op("dve", lambda e: e.match_replace(wk2.h[:], best.h[:, hh, 0:8], cf.ap, -1e30), cand.res + best.res, wk2.res)
            k.op("dve", lambda e: e.max(best.h[:, hh, 8:16], wk2.h[:]), wk2.res, best.res)
            k.op("dve", lambda e: e.max_index(posu.h[:, hh, 8:16], best.h[:, hh, 8:16], wk2.h[:]), wk2.res + best.res, posu.res)
        k.ts("dve", posu2[:, 0, :, :], posu.v(), cu32[:, 0:1], OP.bitwise_and)
        k.ts("dve", posu2[:, 1, :, :], posu.v(), cu32[:, 1:2], OP.logical_shift_right)
        k.cp("dve", pb_.v(), posu2[:, 0, :, :])
        k.cp("dve", pa_.v(), posu2[:, 1, :, :])
        io4 = bc(V(iota16.h[:].unsqueeze(1).unsqueeze(1), iota16.res), [128, 8, 16, 16])
        for (pp, isrc, dst) in ((pa_, i4[:, :, 0, :], eidf), (pb_, i4[:, :, 1, :], eid2)):
            k.tt("dve", oh.v(), io4, bc(V(pp.h[:].unsqueeze(3), pp.res), [128, 8, 16, 16]), OP.is_equal)
            k.tt("dve", oh.v(), oh.v(), bc(V(isrc.ap.unsqueeze(2), isrc.res), [128, 8, 16, 16]), OP.mult)
            k.red("dve", dst.v(), oh.v(), OP.add)
        k.tt("dve", gate.v(), best.v(), bc(best[:, :, 0:1], [128, 8, 16]), OP.subtract)
        k.act(gate.v(), gate.v(), AF.Exp)
        k.red("dve", gs.v(), gate.v(), OP.add)
        k.recip(gs.v(), gs.v())
        k.tt("dve", gate.v(), gate.v(), bc(V(gs.h[:].unsqueeze(2), gs.res), [128, 8, 16]), OP.mult)
        pt = pb[6]
        k.tr(pt[:, 0:128], rr(eidf.v(), "p h k -> p (h k)"), ident.v())
        k.tr(pt[:, 128:256], rr(eid2.v(), "p h k -> p (h k)"), ident.v())
        k.tr(pt[:, 256:384], rr(gate.v(), "p h k -> p (h k)"), ident.v())
        k.cp("act", rtT.v(), rr(pt[:, 0:384], "p (c t) -> p c t", c=3))
        k.dma(RTD[:, :, ti * 128:(ti + 1) * 128].rearrange("c p t -> p c t"), rtT.h[:], reads=rtT.res, writes=RT_res)
        k.cp("pool", hTb.v(), hT.v())
        k.dma(HFD[:, :, ti * 128:(ti + 1) * 128], hTb.h[:], reads=hTb.res, writes=HF_res)
    k.barrier()
    es.close()


class PrepState:
    pass


def prep_alloc(P, tile):
    ps_ = PrepState()
    ps_.uf = [tile("pp_uf%d" % i, [128, D]) for i in range(2)]
    ps_.vf = [tile("pp_vf%d" % i, [128, D]) for i in range(2)]
    ps_.ub = [tile("pp_ub%d" % i, [128, 8, 128], BF16) for i in range(2)]
    ps_.vb = [tile("pp_vb%d" % i, [128, D], BF16) for i in range(2)]
    return ps_


def prep_chunk(P, ps_, l, i, peer_u, peer_v, UTD, VD, PW_res, banks):
    k = P.k
    u_ = ps_.uf[i % 2]; v_ = ps_.vf[i % 2]; ub_ = ps_.ub[i % 2]; vb_ = ps_.vb[i % 2]
    k.ld(u_.v(), peer_u[l, i * 128:(i + 1) * 128, :])
    k.ld(v_.v(), peer_v[l, i * 128:(i + 1) * 128, :])
    for g4 in range(2):
        pt = banks[(i * 2 + g4) % len(banks)]
        for j in range(4):
            dc = g4 * 4 + j
            k.tr(pt[:, j * 128:(j + 1) * 128], u_[:, dc * 128:(dc + 1) * 128], P.ident.v())
        k.cp("act" if g4 else "dve", ub_[:, g4 * 4:(g4 + 1) * 4, :], rr(pt.v(), "p (j e) -> p j e", j=4))
    k.dma(UTD[i], ub_.h[:], reads=ub_.res, writes=PW_res)
    k.cp("pool", vb_.v(), v_.v())
    k.dma(VD[i], vb_.h[:], reads=vb_.res, writes=PW_res)


def phase_peer_prep(P, l, peer_u, peer_v, UTD, VD, PW_res):
    k = P.k
    nc = P.nc
    es = ExitStack()
    tile = lambda name, shape, dt=F32: T(es.enter_context(usb(nc, "t_" + name, shape, dt)), name)
    ps_ = prep_alloc(P, tile)
    for i in range(128):
        prep_chunk(P, ps_, l, i, peer_u, peer_v, UTD, VD, PW_res, P.pb[0:4])
    k.barrier()
    es.close()


def phase_peer_dense(P, l, prm, UTD, VD, PW_res, RTD, RT_res, HFD, HF_res, XD, XD_res, dst_ap, MODD, MOD_res, need_ctx=True):
    import os
    k = P.k
    nc = P.nc
    es = ExitStack()
    tile = lambda name, shape, dt=F32: T(es.enter_context(usb(nc, "t_" + name, shape, dt)), name)
    pb = P.pb
    GT = 256
    mods = {}
    for s in ([0, 1] if need_ctx else [0]):
        mods[s] = tile("pd_gf%d" % s, [128, D])
        k.dma(mods[s].h[:], MODD[s, MOD_GF], reads=MOD_res, writes=mods[s].res)
    lng = tile("pd_lng", [128, D]); k.ld(lng.v(), prm["ln_ffn_g"][l])
    lnb = tile("pd_lnb", [128, D]); k.ld(lnb.v(), prm["ln_ffn_b"][l])
    iota = tile("pd_iota", [128, 128]); k.ld(iota.v(), P.din["c_iota128"])
    G = tile("pd_G", [128, GT, 128], BF16)
    rt = tile("pd_rt", [128, 3, GT])
    hfg = tile("pd_hfg", [128, 8, GT], BF16)
    qtb = tile("pd_qtb", [128, 32, 128], BF16)
    amt = tile("pd_amt", [128, 32, 128], BF16)
    amb = tile("pd_amb", [128, 32, 128], BF16)
    NBUF = 6
    utp = [tile("pd_ut%d" % i, [128, 2, 8, 128], BF16) for i in range(NBUF)]
    vp = [tile("pd_vp%d" % i, [128, 2, D], BF16) for i in range(NBUF)]
    ga = [tile("pd_ga%d" % i, [128, 2, GT]) for i in range(2)]
    GA = [tile("pd_GA%d" % i, [128, 2, GT], BF16) for i in range(2)]
    x = tile("pd_x", [128, D]); acc = tile("pd_acc", [128, D]); st = tile("pd_st", [128, 16])
    g_start = 0 if need_ctx else NCTX
    NG = int(os.environ.get("PEER_GROUPS", 99))
    NCP = int(os.environ.get("PEER_CP", 64))
    groups = list(range(g_start, NT, GT))[:NG]
    io3 = bc(V(iota.h[:].unsqueeze(1), iota.res), [128, 32, 128])
    pA = [pb[0], pb[1]]; pO = [pb[2], pb[3], pb[4], pb[5]]; pG = [pb[6], pb[7]]
    for g0 in groups:
        k.dma(rt.h[:], RTD[:, :, g0:g0 + GT].rearrange("c p t -> p c t"), reads=RT_res, writes=rt.res)
        k.dma(hfg.h[:], HFD[:, :, g0:g0 + GT], reads=HF_res, writes=hfg.res)
        for blk in range(GT // 32):
            ts_ = slice(blk * 32, blk * 32 + 32)
            B3 = lambda v: bc(V(v.ap.unsqueeze(2), v.res), [128, 32, 128])
            k.tt("dve", qtb.v(), io3, B3(rt[:, 1, ts_]), OP.is_equal)
            k.tt("dve", amt.v(), io3, B3(rt[:, 0, ts_]), OP.is_equal)
            k.tt("pool", amb.v(), amt.v(), B3(rt[:, 2, ts_]), OP.mult)
            for q4 in range(8):
                pg = pG[q4 % 2]
                for t4 in range(4):
                    tt_ = q4 * 4 + t4
                    k.mm(pg[:, t4 * 128:(t4 + 1) * 128], qtb[:, tt_, :], amb[:, tt_, :])
                k.cp("act" if q4 % 2 else "dve", G[:, blk * 32 + q4 * 4:blk * 32 + q4 * 4 + 4, :], rr(pg.v(), "p (t i) -> p t i", t=4))

        PF = NBUF - 2

        def emit_dma(cp):
            u_ = utp[cp % NBUF]; v_ = vp[cp % NBUF]
            k.dma(u_.h[:], UTD[2 * cp:2 * cp + 2].rearrange("c d k e -> d c k e"), reads=PW_res, writes=u_.res)
            k.dma(v_.h[:], VD[2 * cp:2 * cp + 2].rearrange("c e n -> e c n"), reads=PW_res, writes=v_.res)

        def emit_A(cp):
            u_ = utp[cp % NBUF]
            pa = pA[cp % 2]
            for cc in range(2):
                for dc in range(8):
                    k.mm(pa[:, cc * GT:(cc + 1) * GT], u_[:, cc, dc, :], hfg[:, dc, :], start=(dc == 0), stop=(dc == 7))

        for cp in range(min(PF + 1, NCP)):
            emit_dma(cp)
        emit_A(0)
        for cp in range(NCP):
            if cp + PF + 1 < NCP:
                emit_dma(cp + PF + 1)
            if cp + 1 < NCP:
                emit_A(cp + 1)
            pa = pA[cp % 2]; ga_ = ga[cp % 2]; GA_ = GA[cp % 2]; v_ = vp[cp % NBUF]
            k.act(rr(ga_.v(), "p c t -> p (c t)"), pa.v(), AF.Gelu_apprx_tanh)
            k.tt("dve", GA_.v(), ga_.v(), V(G.h[:, :, 2 * cp:2 * cp + 2].rearrange("j t i -> j i t"), G.res), OP.mult)
            for tt_ in range(GT // 128):
                for half in range(2):
                    for cc in range(2):
                        k.mm(pO[tt_ * 2 + half].v(), GA_[:, cc, tt_ * 128:(tt_ + 1) * 128], v_[:, cc, half * 512:(half + 1) * 512],
                             start=(cp == 0 and cc == 0), stop=(cp == NCP - 1 and cc == 1))
        for tt_ in range(GT // 128):
            ti = g0 // 128 + tt_
            s = 1 if ti < NCTX // 128 else 0
            k.dma(x.h[:], XD[ti * 128:(ti + 1) * 128, :], reads=XD_res[ti], writes=x.res)
            for half in range(2):
                k.tt("dve", acc[:, half * 512:(half + 1) * 512], pO[tt_ * 2 + half].v(), mods[s][:, half * 512:(half + 1) * 512], OP.mult)
            if "peer_y" in P.debug:
                for half in range(2):
                    k.cp("act", x[:, half * 512:(half + 1) * 512], pO[tt_ * 2 + half].v())
                k.dma(P.dout["peer_y"][ti * 128:(ti + 1) * 128, :], x.h[:], reads=x.res)
                k.dma(x.h[:], XD[ti * 128:(ti + 1) * 128, :], reads=XD_res[ti], writes=x.res)
            k.stt("dve", acc.v(), x.v(), ALPHA, acc.v(), OP.mult, OP.add)
            mean, rstd, nmr = ln_stats(P, acc.v(), st)
            k.act(acc.v(), acc.v(), AF.Identity, bias=nmr, scale=rstd)
            k.tt("pool", acc.v(), acc.v(), lng.v(), OP.mult)
            k.tt("dve", acc.v(), acc.v(), lnb.v(), OP.add)
            d_ap, d_res = dst_ap(ti)
            k.dma(d_ap, acc.h[:], reads=acc.res, writes=d_res)
    k.barrier()
    es.close()


PARAM_SHAPES = {
    "ml_cw": [2, 128, 4, 4], "ml_gb": [2, 128, 16], "ml_ng": [2, 128, 256],
    "rw_muT": [2, 128, 12, 2], "rw_wup": [2, 128, 384], "rw_aup": [2, 128, 384], "rw_gup": [2, 128, 384],
    "rw_cols": [2, 128, 3, 9], "mla_gq": [2, 128, 384], "mla_gkv": [2, 128, 256],
    "ln_mix_g": [2, 128, D], "ln_mix_b": [2, 128, D], "ln_ffn_g": [2, 128, D], "ln_ffn_b": [2, 128, D],
}
WEIGHT_SHAPES = {
    "w_mod": [2, D, 6 * D], "b_mod": [2, 6 * D], "w_in": [2, D, N_IN], "w_out": [2, D, D],
    "peer_w_q": [2, D, 2048], "peer_keys": [2, 8, 2, 128, 128], "peer_u": [2, 16384, D], "peer_v": [2, 16384, D],
    "mla_q_up": [2, 384, 576], "mla_kv_up": [2, 256, 768],
}


def build_program(debug=(), layers=(0, 1), stop_after=None, peer_mode="dense"):
    P = Prog(debug)
    setup_common(P)
    k = P.k
    nc = P.nc
    W = {n: P.inp(n, s) for n, s in WEIGHT_SHAPES.items()}
    prm = {n: P.inp(n, s) for n, s in PARAM_SHAPES.items()}
    prm["mla_q_up"] = W["mla_q_up"]; prm["mla_kv_up"] = W["mla_kv_up"]
    XD = P.scratch("XD1", [NT, D]); XD_res = [[Res("XD%d" % i)] for i in range(NTILE)]
    MODD = P.scratch("MODD", [2, 6, 128, D]); MOD_res = [Res("MODD")]
    PRW = P.scratch("PRW", [12, 128, NT]); PRW_res = [Res("PRW")]
    MIXT = P.scratch("MIXT", [8, 128, NT]); MIXT_res = [Res("MIXT")]
    OUT = P.outp("out", [NT - NCTX, D])
    P.peer_mode = peer_mode
    UTD = P.scratch("UTD", [128, 128, 8, 128], BF16); VD = P.scratch("VD", [128, 128, D], BF16); PW_res = [Res("PW")]
    RTD = P.scratch("RTD", [3, 128, NT]); RT_res = [Res("RT")]
    HFD = P.scratch("HFD", [128, 8, NT], BF16); HF_res = [Res("HF")]
    for l in layers:
        need_ctx = (l == 0)
        first = (l == layers[0])
        src = P.xin if first else XD
        src_res = [[] for _ in range(NTILE)] if first else XD_res
        phase_mod(P, l, W["w_mod"], W["b_mod"], MODD, MOD_res)
        es1 = ExitStack()
        hT = T(es1.enter_context(usb(nc, "t_hT_l%d" % l, [128, 8, NT], BF16)), "hT")
        phase_hT(P, src, src_res, MODD, MOD_res, MOD_SHA, MOD_SCA, hT)
        phase_mlstm(P, l, hT, W["w_in"], prm, MIXT, MIXT_res)
        phase_rwkv_proj(P, l, hT, W["w_in"], prm, PRW, PRW_res)
        phase_mla(P, l, hT, W["w_in"], prm, MIXT, MIXT_res, need_ctx)
        es1.close()
        phase_rwkv(P, l, prm, PRW, PRW_res, MIXT, MIXT_res)
        if stop_after == "mix":
            break
        phase_mixout(P, l, W["w_out"], prm, MIXT, MIXT_res, src, src_res, XD, XD_res, MODD, MOD_res, need_ctx)
        if stop_after == "mixout":
            break
        if l == 1:
            dst = lambda ti: (OUT[(ti - NCTX // 128) * 128:(ti - NCTX // 128 + 1) * 128, :], [])
        else:
            dst = lambda ti: (XD[ti * 128:(ti + 1) * 128, :], XD_res[ti])
        if P.peer_mode == "gather":
            phase_peer(P, l, prm, W["peer_w_q"], W["peer_keys"], W["peer_u"], W["peer_v"], XD, XD_res, dst, MODD, MOD_res, need_ctx)
        else:
            phase_peer_route(P, l, prm, W["peer_w_q"], W["peer_keys"], XD, XD_res, MODD, MOD_res, RTD, RT_res, HFD, HF_res, need_ctx,
                             prep=(W["peer_u"], W["peer_v"], UTD, VD, PW_res))
            phase_peer_dense(P, l, prm, UTD, VD, PW_res, RTD, RT_res, HFD, HF_res, XD, XD_res, dst, MODD, MOD_res, need_ctx)
    k.wait_all("sp")
    P.es.close()
    return P


def make_in_maps(inputs, n_cores=8):
    consts = host_consts()
    hp = host_params(inputs)
    maps = []
    for b in range(n_cores):
        m = dict(consts)
        m.update(hp)
        for n in WEIGHT_SHAPES:
            m[n] = np.ascontiguousarray(inputs[n])
        m["xin"] = np.ascontiguousarray(np.concatenate([inputs["ctx"][b], inputs["x"][b]], 0))
        cv = np.stack([inputs["c"][b], inputs["c_ctx"]], -1)
        m["cvec"] = np.ascontiguousarray(cv.reshape(8, 128, 2).transpose(1, 0, 2))
        maps.append(m)
    return maps


_CACHE = {}


def kernel(**inputs):
    from concourse.bass_utils import run_bass_kernel_spmd
    inputs = {k_: np.asarray(v) for k_, v in inputs.items()}
    if "P" not in _CACHE:
        _CACHE["P"] = build_program()
    P = _CACHE["P"]
    maps = make_in_maps(inputs, 8)
    maps = [{kk: v for kk, v in m.items() if kk in P.din} for m in maps]
    res = run_bass_kernel_spmd(P.nc, maps, core_ids=list(range(8)))
    out = np.stack([np.asarray(res.results[b]["out"]) for b in range(8)], 0).astype(np.float32)
    return out
```

```python
import numpy as np
import concourse.bass as bass
import concourse.mybir as mybir
from contextlib import ExitStack

F32 = mybir.dt.float32
BF16 = mybir.dt.bfloat16
U32 = mybir.dt.uint32
I32 = mybir.dt.int32
AF = mybir.ActivationFunctionType
OP = mybir.AluOpType
AX = mybir.AxisListType


class Res:
    __slots__ = ("name", "w", "r", "pe_row", "excl")

    def __init__(self, name=""):
        self.name = name
        self.pe_row = None
        self.excl = False
        self.w = None
        self.r = {}


class Sched:
    NDMA = 24
    NSW = 8

    def __init__(self, nc, es):
        self.nc = nc
        self.es = es
        self.eng = {"pe": nc.tensor, "dve": nc.vector, "act": nc.scalar,
                    "pool": nc.gpsimd, "sp": nc.sync}
        self.sem = {}
        self.cnt = {}
        for k in self.eng:
            self.sem[k] = es.enter_context(nc.semaphore("s_" + k))
            self.cnt[k] = 0
        self.dsem = [es.enter_context(nc.semaphore("d%d" % i)) for i in range(self.NDMA + self.NSW)]
        self.dcnt = [0] * (self.NDMA + self.NSW)
        self.dnext = 0
        self.swnext = 0
        self.known = {k: {} for k in self.eng}
        self.gen = {k: 0 for k in self.eng}
        self.ekey = {k: k for k in self.eng}
        self.semobj = dict(self.sem)
        for i, s in enumerate(self.dsem):
            self.semobj["d%d" % i] = s
        self.ninst = 0

    def _wait(self, e, tok):
        if tok is None:
            return
        key, val = tok
        kn = self.known[e]
        if kn.get(key, 0) >= val:
            return
        self.eng[e].wait_ge(self.semobj[key], val)
        kn[key] = val

    def _deps(self, e, reads, writes, same_ok=False):
        for r in reads:
            if r.w is not None and not (same_ok and r.w[0].split("#")[0] == e):
                self._wait(e, r.w)
            if r.excl:
                for key, val in r.r.items():
                    if key.split("#")[0] != e:
                        self._wait(e, (key, val))
        for w in writes:
            if w.w is not None and not (same_ok and w.w[0].split("#")[0] == e):
                self._wait(e, w.w)
            for key, val in w.r.items():
                if not (same_ok and key.split("#")[0] == e):
                    self._wait(e, (key, val))

    def _commit(self, tok, reads, writes):
        key, val = tok
        for r in reads:
            if r.r.get(key, 0) < val:
                r.r[key] = val
        for w in writes:
            w.w = tok
            w.r = {}

    SEM_LIMIT = 20000

    def op(self, e, fn, reads=(), writes=(), same_ok=False):
        if getattr(self, "mute", False):
            return None
        if self.cnt[e] >= self.SEM_LIMIT:
            self.gen[e] += 1
            key = "%s#%d" % (e, self.gen[e])
            self.sem[e] = self.es.enter_context(self.nc.semaphore("s_%s_%d" % (e, self.gen[e])))
            self.semobj[key] = self.sem[e]
            self.ekey[e] = key
            self.cnt[e] = 0
        self._deps(e, reads, writes, same_ok)
        ins = fn(self.eng[e])
        self.cnt[e] += 1
        ins.then_inc(self.sem[e], 1)
        self._commit((self.ekey[e], self.cnt[e]), reads, writes)
        self.ninst += 1
        return ins

    def dma(self, out, in_, reads=(), writes=(), q="sp", **kw):
        s = self.dnext
        self.dnext = (self.dnext + 1) % self.NDMA
        key = "d%d" % s
        if self.dcnt[s] > 0:
            self._wait(q, (key, self.dcnt[s]))
        self._deps(q, reads, writes)
        ins = self.eng[q].dma_start(out=out, in_=in_, **kw)
        self.dcnt[s] += 16
        ins.then_inc(self.dsem[s], 16)
        self._commit((key, self.dcnt[s]), reads, writes)
        self.ninst += 1
        return (key, self.dcnt[s])

    def _dma_token(self, ins, q, reads, writes):
        s = self.dnext
        self.dnext = (self.dnext + 1) % self.NDMA
        raise RuntimeError("use dma_custom")

    def dma_custom(self, q, build, reads=(), writes=()):
        if getattr(self, "mute", False):
            return None
        s = self.NDMA + self.swnext
        self.swnext = (self.swnext + 1) % self.NSW
        key = "d%d" % s
        if self.dcnt[s] > 0:
            self._wait(q, (key, self.dcnt[s]))
        self._deps(q, reads, writes)
        ins = build(self.eng[q])
        self.dcnt[s] += 16
        ins.then_inc(self.dsem[s], 16)
        self._commit((key, self.dcnt[s]), reads, writes)
        self.ninst += 1
        return (key, self.dcnt[s])

    def wait_all(self, e="sp"):
        for s in range(self.NDMA + self.NSW):
            if self.dcnt[s] > 0:
                self._wait(e, ("d%d" % s, self.dcnt[s]))
        for k in self.eng:
            if self.cnt[k] > 0:
                self._wait(e, (self.ekey[k], self.cnt[k]))


class V:
    __slots__ = ("ap", "res")

    def __init__(self, ap, res):
        self.ap = ap
        self.res = res

    def __getitem__(self, idx):
        return V(self.ap[idx], self.res)


class T:
    def __init__(self, h, name):
        self.h = h
        self.res = [Res(name)]

    def __getitem__(self, idx):
        return V(self.h[idx], self.res)

    def v(self):
        return V(self.h[:], self.res)


def _rs(*vs):
    out = []
    for v in vs:
        if isinstance(v, V):
            out.extend(v.res)
    return out


def _ap(v):
    return v.ap if isinstance(v, V) else v


class K(Sched):
    def sb(self, name, shape, dt=F32):
        return T(self.es.enter_context(self.nc.sbuf_tensor(name, shape, dt)), name)

    def ps(self, name, shape, dt=F32):
        t = T(self.es.enter_context(self.nc.psum_tensor(name, shape, dt)), name)
        t.res[0].excl = True
        return t

    def tt(self, e, out, in0, in1, op):
        return self.op(e, lambda g: g.tensor_tensor(_ap(out), _ap(in0), _ap(in1), op), _rs(in0, in1), _rs(out))

    def ts(self, e, out, in0, s1, op0, s2=None, op1=None, accum=None):
        def f(g):
            kw = {}
            if op1 is not None:
                kw["op1"] = op1
            if accum is not None:
                kw["accum_out"] = _ap(accum)
            return g.tensor_scalar(_ap(out), _ap(in0), _ap(s1), _ap(s2), op0=op0, **kw)
        return self.op(e, f, _rs(in0, s1, s2), _rs(out, accum))

    def stt(self, e, out, in0, s, in1, op0, op1):
        return self.op(e, lambda g: g.scalar_tensor_tensor(_ap(out), _ap(in0), _ap(s), _ap(in1), op0=op0, op1=op1),
                       _rs(in0, s, in1), _rs(out))

    def cp(self, e, out, in_):
        if e == "act":
            return self.op(e, lambda g: g.copy(_ap(out), _ap(in_)), _rs(in_), _rs(out))
        return self.op(e, lambda g: g.tensor_copy(_ap(out), _ap(in_)), _rs(in_), _rs(out))

    def act(self, out, in_, func, bias=0.0, scale=1.0, accum=None):
        def f(g):
            kw = {}
            if accum is not None:
                kw["accum_out"] = _ap(accum)
            return g.activation(_ap(out), _ap(in_), func, bias=_ap(bias), scale=_ap(scale), **kw)
        return self.op("act", f, _rs(in_, bias, scale), _rs(out, accum))

    def red(self, e, out, in_, op, axis=AX.X):
        return self.op(e, lambda g: g.tensor_reduce(_ap(out), _ap(in_), axis, op), _rs(in_), _rs(out))

    def recip(self, out, in_):
        return self.op("dve", lambda g: g.reciprocal(_ap(out), _ap(in_)), _rs(in_), _rs(out))

    def memset(self, e, out, val):
        return self.op(e, lambda g: g.memset(_ap(out), val), [], _rs(out))

    def mm(self, out, lhsT, rhs, start=True, stop=True):
        if getattr(self, "mute", False):
            return None
        la = _ap(lhsT)
        key = (la.base_partition(), la.partition_size())
        for r in out.res:
            prev = getattr(r, "pe_row", None)
            if prev is not None and prev != key and r.w is not None and r.w[0].split("#")[0] == "pe":
                self._wait("pe", r.w)
        ins = self.op("pe", lambda g: g.matmul(_ap(out), la, _ap(rhs), start=start, stop=stop),
                      _rs(lhsT, rhs), _rs(out), same_ok=True)
        for r in out.res:
            r.pe_row = key
        return ins

    def tr(self, out, in_, ident):
        return self.op("pe", lambda g: g.transpose(_ap(out), _ap(in_), _ap(ident)), _rs(in_, ident), _rs(out),
                       same_ok=True)

    def ld(self, out, in_ap, q="sp", **kw):
        return self.dma(_ap(out), in_ap, reads=[], writes=_rs(out), q=q, **kw)

    def st(self, out_ap, in_, q="sp", dram_res=None, **kw):
        return self.dma(out_ap, _ap(in_), reads=_rs(in_), writes=(dram_res or []), q=q, **kw)

    def barrier(self):
        for e in self.eng:
            self.wait_all(e)


import itertools
_ctr = itertools.count()


def usb(nc, name, shape, dt):
    return nc.sbuf_tensor("%s_u%d" % (name, next(_ctr)), shape, dt)


NT = 2304
NCTX = 256
D = 1024
N_IN = 3248
NTILE = NT // 128
NCH = NT // 64
ALPHA = 4 ** 0.25
LN_EPS = 1e-6
MOD_SHA, MOD_SCA, MOD_GA, MOD_SHF, MOD_SCF, MOD_GF = range(6)


def host_consts():
    c = {}
    c["ident"] = np.eye(128, dtype=np.float32)
    s = np.arange(64)[:, None]; t = np.arange(64)[None, :]
    triF = (s <= t).astype(np.float32); triR = (s >= t).astype(np.float32)
    c["c_tri"] = np.concatenate([triF, triR], 0)
    mF = np.where(t <= s, 0.0, -1e30).astype(np.float32)
    mR = np.where(t >= s, 0.0, -1e30).astype(np.float32)
    c["c_maskD"] = np.concatenate([mF, mR], 0)
    c["c_I2"] = np.concatenate([np.eye(64, dtype=np.float32)] * 2, 0)
    tt_ = np.arange(2048)
    inv = (10000.0 ** (-np.arange(8, dtype=np.float32) / 8)).astype(np.float32)
    ang = np.stack([(tt_ // 64)[:, None].astype(np.float32) * inv, (tt_ % 64)[:, None].astype(np.float32) * inv], 1)
    c["c_rope"] = np.concatenate([np.cos(ang).reshape(2048, 16), np.sin(ang).reshape(2048, 16)], 1).astype(np.float32)
    c["c_u32"] = np.ascontiguousarray(np.broadcast_to(np.array([15, 4], np.uint32)[None, :], (128, 2)))
    c["c_iota128"] = np.ascontiguousarray(np.broadcast_to(np.arange(128, dtype=np.float32)[None, :], (128, 128)))
    c["c_iota16"] = np.ascontiguousarray(np.broadcast_to(np.arange(16, dtype=np.float32)[None, :], (128, 16)))
    bd = np.zeros((128, 128), np.float32); bd[:64, :64] = 1; bd[64:, 64:] = 1
    c["c_BD2"] = bd
    r_ = np.arange(64)[:, None]; c_ = np.arange(64)[None, :]
    su = (r_ < c_).astype(np.float32); iu = (r_ <= c_).astype(np.float32)
    sl = (r_ > c_).astype(np.float32); il = (r_ >= c_).astype(np.float32)
    mf = np.concatenate([su, iu, su, iu, sl], 1); mr = np.concatenate([sl, il, sl, il, su], 1)
    m2 = np.stack([mf, mr], 0)
    c["c_rwmask"] = np.ascontiguousarray(np.concatenate([m2, m2], 1).transpose(1, 0, 2))
    return c


def host_params(inp):
    L = 2
    p = {}
    cw = np.concatenate([inp["ml_conv_w"], inp["ml_conv_b"][:, None, :]], 1)
    p["ml_cw"] = np.ascontiguousarray(cw.reshape(L, 4, 4, 128).transpose(0, 3, 2, 1))
    gb = np.zeros((L, 16), np.float32)
    for d in range(2):
        gb[:, d * 8:d * 8 + 4] = inp["ml_i_bias"][:, d]
        gb[:, d * 8 + 4:d * 8 + 8] = inp["ml_f_bias"][:, d]
    p["ml_gb"] = np.ascontiguousarray(np.broadcast_to(gb[:, None, :], (L, 128, 16)))
    p["ml_ng"] = np.ascontiguousarray(np.broadcast_to(inp["ml_norm_g"][:, None, :], (L, 128, 256)))
    p["mla_gq"] = np.ascontiguousarray(np.broadcast_to(inp["mla_q_norm_g"][:, None, :], (L, 128, 384)))
    p["mla_gkv"] = np.ascontiguousarray(np.broadcast_to(inp["mla_kv_norm_g"][:, None, :], (L, 128, 256)))
    for nm in ("ln_mix_g", "ln_mix_b", "ln_ffn_g", "ln_ffn_b"):
        p[nm] = np.ascontiguousarray(np.broadcast_to(inp[nm][:, None, :], (L, 128, 1024)))
    p["rw_muT"] = np.ascontiguousarray(inp["rw_mu"].reshape(L, 2, 12, 128).transpose(0, 3, 2, 1))
    p["rw_wup"] = np.ascontiguousarray(inp["rw_w_up"].reshape(L, 128, 384))
    p["rw_aup"] = np.ascontiguousarray(inp["rw_a_up"].reshape(L, 128, 384))
    p["rw_gup"] = np.ascontiguousarray(inp["rw_g_up"])
    cols = np.stack([inp["rw_k_k"], inp["rw_k_a"], inp["rw_r_k"], inp["rw_gn_g"], inp["rw_gn_b"],
                     inp["rw_w0"][:, 0], inp["rw_w0"][:, 1], inp["rw_a0"][:, 0], inp["rw_a0"][:, 1]], -1)
    p["rw_cols"] = np.ascontiguousarray(cols.reshape(L, 3, 128, 9).transpose(0, 2, 1, 3))
    return p


class Prog:
    def __init__(self, debug=()):
        self.debug = set(debug)
        self.nc = nc = bass.Bass("TRN2", target_bir_lowering=False)
        self.es = ExitStack()
        self.k = K(nc, self.es)
        self.din = {}
        self.dout = {}

    def inp(self, name, shape, dt=F32):
        self.din[name] = self.nc.dram_tensor(name, list(shape), dt, kind="ExternalInput").ap()
        return self.din[name]

    def outp(self, name, shape, dt=F32):
        self.dout[name] = self.nc.dram_tensor(name, list(shape), dt, kind="ExternalOutput").ap()
        return self.dout[name]

    def dump(self, name, view):
        ap = view.ap
        o = self.outp("o_" + name, list(ap.shape), ap.dtype)
        self.k.dma(o, ap, reads=view.res)

    def scratch(self, name, shape, dt=F32):
        kind = "ExternalOutput" if name in self.debug else "Internal"
        ap = self.nc.dram_tensor(name, list(shape), dt, kind=kind).ap()
        if kind == "ExternalOutput":
            self.dout[name] = ap
        return ap


def setup_common(P):
    k = P.k
    P.ident_d = P.inp("ident", [128, 128])
    P.ident = k.sb("ident_sb", [128, 128])
    k.ld(P.ident.v(), P.ident_d)
    P.identb = k.sb("identb", [128, 128], BF16)
    k.cp("dve", P.identb.v(), P.ident.v())
    P.ones = k.sb("ones", [128, 128])
    k.memset("dve", P.ones.v(), 1.0)
    P.pb = [k.ps("pb%d" % i, [128, 512]) for i in range(8)]
    P.bnst = k.sb("bnst", [128, 12])
    P.wstage = [k.sb("wstage%d" % i, [128, 8, 256]) for i in range(2)]
    P.wsi = 0
    for nm, shp in (("c_tri", [128, 64]), ("c_maskD", [128, 64]), ("c_I2", [128, 64]), ("c_BD2", [128, 128]), ("c_rwmask", [128, 2, 320]), ("c_rope", [2048, 32]), ("c_iota16", [128, 16]), ("c_iota128", [128, 128]), ("c_u32", [128, 2, "u32"])):
        if shp[-1] == "u32":
            P.inp(nm, shp[:-1], U32)
        else:
            P.inp(nm, shp)
    P.xin = P.inp("xin", [NT, D])
    P.cvec = P.inp("cvec", [128, 8, 2])
    P.XD = P.scratch("XD", [NT, D])
    P.XD_res = [Res("XD")]


def phase_mod(P, l, w_mod, b_mod, MODD, MOD_res):
    k = P.k
    nc = P.nc
    es = ExitStack()
    tile = lambda name, shape, dt=F32: T(es.enter_context(usb(nc, "t_" + name, shape, dt)), name)
    csilu = tile("csilu", [128, 8, 2])
    cv = tile("cvec_sb", [128, 8, 2])
    k.ld(cv.v(), P.cvec)
    k.act(csilu.v(), cv.v(), AF.Silu)
    clhs = [tile("clhs%d" % s_, [128, 8, 128]) for s_ in range(2)]
    for s_ in range(2):
        for kc in range(8):
            k.cp("dve", clhs[s_][:, kc, :], V(csilu.h[:, kc, s_:s_ + 1].to_broadcast([128, 128]), csilu.res))
    wbuf = [tile("wmodbuf%d" % i, [128, 8, 512]) for i in range(2)]
    ob = [tile("modout%d" % i, [128, 512]) for i in range(4)]
    bb = tile("bmodrow", [1, 6 * D])
    k.ld(bb.v(), b_mod[l:l + 1, :])
    for n in range(12):
        wb = wbuf[n % 2]
        k.ld(wb.v(), w_mod[l, :, n * 512:(n + 1) * 512].rearrange("(c p) n -> p c n", p=128))
        for s_ in range(2):
            pt = P.pb[(n * 2 + s_) % 4]
            o_ = ob[(n * 2 + s_) % 4]
            for kc in range(8):
                k.mm(pt.v(), clhs[s_][:, kc, :], wb[:, kc, :], start=(kc == 0), stop=False)
            k.mm(pt.v(), P.ones[0:1, :], bb[0:1, n * 512:(n + 1) * 512], start=False, stop=True)
            k.cp("act" if s_ else "dve", o_.v(), pt.v())
            k.dma(MODD[s_, n // 2, :, (n % 2) * 512:(n % 2 + 1) * 512], o_.h[:], reads=o_.res, writes=MOD_res)
    k.barrier()
    es.close()


def ln_stats(P, xt, st, nfeat=D):
    k = P.k
    nchunk = nfeat // 512
    bs = P.bnst
    for c in range(nchunk):
        k.op("dve", lambda g: g.bn_stats(bs.h[:, c * 6:(c + 1) * 6], xt.ap[:, c * 512:(c + 1) * 512]), xt.res, bs.res)
    k.op("dve", lambda g: g.bn_aggr(st.h[:, 0:2], bs.h[:, 0:6 * nchunk]), bs.res, st.res)
    k.ts("dve", st[:, 2:3], st[:, 1:2], LN_EPS, OP.add)
    k.act(st[:, 3:4], st[:, 2:3], AF.Sqrt)
    k.recip(st[:, 4:5], st[:, 3:4])
    k.stt("dve", st[:, 5:6], st[:, 0:1], -1.0, st[:, 4:5], OP.mult, OP.mult)
    return st[:, 0:1], st[:, 4:5], st[:, 5:6]


def phase_hT(P, src_ap, src_res, MODD, MOD_res, shj, scj, hT):
    k = P.k
    nc = P.nc
    es = ExitStack()
    xt = [T(es.enter_context(usb(nc, "ph_x%d" % i, [128, D], F32)), "ph_x%d" % i) for i in range(2)]
    hh = [T(es.enter_context(usb(nc, "ph_h%d" % i, [128, D], F32)), "ph_h%d" % i) for i in range(2)]
    st = [T(es.enter_context(usb(nc, "ph_st%d" % i, [128, 16], F32)), "ph_st%d" % i) for i in range(2)]
    sc1 = [T(es.enter_context(usb(nc, "ph_sc1_%d" % s, [128, D], F32)), "ph_sc1_%d" % s) for s in range(2)]
    sh1 = [T(es.enter_context(usb(nc, "ph_sh1_%d" % s, [128, D], F32)), "ph_sh1_%d" % s) for s in range(2)]
    for s in range(2):
        k.dma(sc1[s].h[:], MODD[s, scj], reads=MOD_res, writes=sc1[s].res)
        k.dma(sh1[s].h[:], MODD[s, shj], reads=MOD_res, writes=sh1[s].res)
        k.ts("pool", sc1[s].v(), sc1[s].v(), 1.0, OP.add)
    for ti in range(NTILE):
        s = 1 if ti < NCTX // 128 else 0
        x = xt[ti % 2]; h = hh[ti % 2]; stt_ = st[ti % 2]
        k.dma(x.h[:], src_ap[ti * 128:(ti + 1) * 128, :], reads=src_res[ti], writes=x.res)
        mean, rstd, nmr = ln_stats(P, x.v(), stt_)
        k.act(h.v(), x.v(), AF.Identity, bias=nmr, scale=rstd)
        k.tt("dve", h.v(), h.v(), sc1[s].v(), OP.mult)
        k.tt("pool", h.v(), h.v(), sh1[s].v(), OP.add)
        for g4 in range(2):
            pt = P.pb[4 + (ti * 2 + g4) % 2]
            for j in range(4):
                dc = g4 * 4 + j
                k.tr(pt[:, j * 128:(j + 1) * 128], h[:, dc * 128:(dc + 1) * 128], P.ident.v())
            k.cp("act", hT[:, g4 * 4:(g4 + 1) * 4, ti * 128:(ti + 1) * 128],
                 V(pt.h[:].rearrange("p (j t) -> p j t", j=4), pt.res))
    k.barrier()
    es.close()


def bc(view, shape):
    return V(view.ap.to_broadcast(shape), view.res)


def rr(view, pat, **kw):
    return V(view.ap.rearrange(pat, **kw), view.res)


def load_w_bf16(P, dst, w_ap, ncols, c_off=0):
    k = P.k
    for c0 in range(0, ncols, 256):
        n = min(256, ncols - c0)
        stg = P.wstage[P.wsi % 2]
        P.wsi += 1
        k.ld(stg[:, :, 0:n], w_ap[:, c0:c0 + n].rearrange("(c p) n -> p c n", p=128))
        k.cp("pool" if (P.wsi % 2) else "dve", dst[:, :, c_off + c0:c_off + c0 + n], stg[:, :, 0:n])


ORD_F = list(range(NCH))
ORD_R = [3, 2, 1, 0] + list(range(NCH - 1, 3, -1))


def phase_mlstm(P, l, hT, w_in, prm, MIXD, MIX_res):
    k = P.k
    nc = P.nc
    es = ExitStack()
    tile = lambda name, shape, dt=F32: T(es.enter_context(usb(nc, "t_" + name, shape, dt)), name)
    ident = P.ident
    wml = tile("wml", [128, 8, 1040], BF16)
    load_w_bf16(P, wml, w_in[l, :, 0:1040], 1040)
    qkT = tile("qkT", [128, 4, NT])
    raw = tile("mlraw", [128, NT])
    cw = tile("mlcw", [128, 4, 4]); k.ld(cw.v(), prm["ml_cw"][l])
    gb = tile("mlgb", [128, 16]); k.ld(gb.v(), prm["ml_gb"][l])
    ng = tile("mlng", [128, 256]); k.ld(ng.v(), prm["ml_ng"][l])
    tri = tile("mltri", [128, 64]); k.ld(tri.v(), P.din["c_tri"])
    maskD = tile("mlmask", [128, 64]); k.ld(maskD.v(), P.din["c_maskD"])
    I2 = tile("mlI2", [128, 64]); k.ld(I2.v(), P.din["c_I2"])
    pb = P.pb
    ib = 0
    for fc in range(4):
        for tb in range(0, NT, 512):
            n = min(512, NT - tb)
            pt = pb[ib % 2]; ib += 1
            for dc in range(8):
                k.mm(pt[:, 0:n], wml[:, dc, fc * 128:(fc + 1) * 128], hT[:, dc, tb:tb + n], start=(dc == 0), stop=(dc == 7))
            k.cp("act", raw[:, tb:tb + n], pt[:, 0:n])
        dst = qkT[:, fc, :]
        k.act(dst, raw.v(), AF.Identity, bias=cw[:, fc, 3:4], scale=cw[:, fc, 1:2])
        for (s0, s1) in ((0, NCTX), (NCTX, NT)):
            k.stt("dve", qkT[:, fc, s0 + 1:s1], raw[:, s0:s1 - 1], cw[:, fc, 0:1], qkT[:, fc, s0 + 1:s1], OP.mult, OP.add)
            k.stt("dve", qkT[:, fc, s0:s1 - 1], raw[:, s0 + 1:s1], cw[:, fc, 2:3], qkT[:, fc, s0:s1 - 1], OP.mult, OP.add)
        k.act(dst, dst, AF.Silu)
        if fc < 2:
            k.ts("dve", dst, dst, 0.125, OP.mult)
    if "ml_qkT" in P.debug:
        P.dump("ml_qkT", qkT.v())
    import os
    if os.environ.get("MLSTOP") == "1":
        k.barrier(); es.close(); return
    NSTEP = int(os.environ.get("MLSTEPS", NCH))
    CUT = int(os.environ.get("MLCUT", 99))
    def sec(n):
        k.mute = CUT < n
    CT = tile("mlCT", [128, 2, 2, 65]); k.memset("dve", CT.v(), 0.0)
    mfull = tile("mlm", [128, 8]); k.memset("dve", mfull.v(), -1e30)
    hsum = tile("mlhsum", [128, NTILE, 256]); k.memset("pool", hsum.v(), 0.0)
    vS = tile("mlvS", [128, 4, 65]); k.memset("dve", vS.v(), 1.0)
    kS = tile("mlkS", [128, 4, 64])
    gS = tile("mlgS", [128, 8])
    cs = tile("mlcs", [128, 12])
    lmb = tile("mllmb", [128, 4])
    D8 = tile("mlD8", [128, 4, 64])
    rmax = tile("mlrmax", [128, 8])
    dm = tile("mldm", [128, 4, 64])
    sm = tile("mlsmall", [128, 64])
    Sm = tile("mlSm", [128, 4, 64])
    ST = tile("mlST", [128, 4, 64])
    tI = tile("mltI", [128, 4, 65])
    nd = tile("mlnd", [128, 4, 65])
    hout = tile("mlhout", [128, 4, 64])
    kw = tile("mlkw", [128, 4, 64])
    mmax = tile("mlmmax", [128, 8])
    dec = tile("mldec", [128, 8])
    c4 = lambda i: sm[:, i * 4:(i + 1) * 4]
    mx, a1, mt, tmp, wint, emt, dabs, rden, wk, mmA = [c4(i) for i in range(10)]
    pA, pR, pS, pST, pN, pI, pU, pX = pb
    B4 = lambda v: bc(V(v.ap.unsqueeze(2), v.res), [128, 4, 64])
    for step in range(NSTEP):
        chunks = (ORD_F[step], ORD_R[step])
        sec(1)
        for dr in range(2):
            c = chunks[dr]
            H = slice(dr * 64, dr * 64 + 64)
            tok = slice(c * 64, c * 64 + 64)
            for dc in range(8):
                k.mm(pX[H, 0:256], hT[:, dc, tok], wml[:, dc, 512:768], start=(dc == 0), stop=(dc == 7))
            for dc in range(8):
                k.mm(pA[H, 16:32], hT[:, dc, tok], wml[:, dc, 1024:1040], start=(dc == 0), stop=(dc == 7))
            for hh in range(2):
                k.mm(pX[H, 256 + hh * 128:256 + (hh + 1) * 128], qkT[:, 2 + hh, tok], ident.v())
            k.tt("dve", gS[H, :], pA[H, 16 + dr * 8:16 + dr * 8 + 8], gb[H, dr * 8:dr * 8 + 8], OP.add)
        k.cp("act", vS[:, :, 0:64], rr(pX[:, 0:256], "p (h e) -> p h e", h=4))
        k.cp("dve", kS.v(), rr(pX[:, 256:512], "p (h e) -> p h e", h=4))
        k.act(gS[:, 4:8], gS[:, 4:8], AF.Exp, scale=-1.0)
        k.act(gS[:, 4:8], gS[:, 4:8], AF.Ln, bias=1.0)
        k.ts("dve", gS[:, 4:8], gS[:, 4:8], -1.0, OP.mult)
        sec(2)
        for dr in range(2):
            H = slice(dr * 64, dr * 64 + 64)
            k.mm(pA[H, 0:4], tri[H, :], gS[H, 4:8])
            k.mm(pA[:, 4 + dr * 4:8 + dr * 4], P.ones[H, :], gS[H, 4:8])
        k.cp("dve", cs.v(), pA[:, 0:12])
        k.tt("dve", lmb.v(), gS[:, 0:4], cs[:, 0:4], OP.subtract)
        k.tt("dve", D8.v(), bc(V(I2.h[:].unsqueeze(1), I2.res), [128, 4, 64]), B4(lmb.v()), OP.mult)
        for dr in range(2):
            H = slice(dr * 64, dr * 64 + 64)
            k.mm(rr(pR[:, dr * 256:(dr + 1) * 256], "p (h s) -> p h s", h=4), P.ones[H, :], D8[H, :, :])
        k.red("dve", rmax.v(), rr(pR.v(), "p (u s) -> p u s", u=8), OP.max)
        for dr in range(2):
            H = slice(dr * 64, dr * 64 + 64)
            k.tt("dve", dm[H, :, :], rr(pR[H, dr * 256:(dr + 1) * 256], "p (h s) -> p h s", h=4),
                 bc(V(maskD.h[H, :].unsqueeze(1), maskD.res), [64, 4, 64]), OP.add)
            k.tt("dve", a1[H, :] if False else V(sm.h[H, 4:8], sm.res), cs[H, 0:4], mfull[H, dr * 4:dr * 4 + 4], OP.add)
        k.tt("dve", dm.v(), dm.v(), B4(cs[:, 0:4]), OP.add)
        k.red("dve", mx, dm.v(), OP.max)
        k.tt("dve", mt, mx, a1, OP.max)
        k.tt("dve", dm.v(), dm.v(), B4(mt), OP.subtract)
        k.act(dm.v(), dm.v(), AF.Exp)
        k.tt("dve", tmp, a1, mt, OP.subtract)
        k.act(wint, tmp, AF.Exp)
        k.act(emt, mt, AF.Exp, scale=-1.0)
        sec(3)
        for dr in range(2):
            c = chunks[dr]
            H = slice(dr * 64, dr * 64 + 64)
            tok = slice(c * 64, c * 64 + 64)
            for h in range(4):
                hp = slice((h % 2) * 64, (h % 2) * 64 + 64)
                k.mm(pS[H, h * 64:(h + 1) * 64], qkT[hp, h // 2, tok], qkT[hp, 2 + h // 2, tok])
        k.tt("dve", Sm.v(), rr(pS[:, 0:256], "p (h s) -> p h s", h=4), dm.v(), OP.mult)
        sec(4)
        for dr in range(2):
            H = slice(dr * 64, dr * 64 + 64)
            for h in range(4):
                k.mm(pST[H, h * 64:(h + 1) * 64], Sm[H, h, :], ident[H, H])
        k.cp("act", ST.v(), rr(pST[:, 0:256], "p (h s) -> p h s", h=4))
        for dr in range(2):
            c = chunks[dr]
            H = slice(dr * 64, dr * 64 + 64)
            tok = slice(c * 64, c * 64 + 64)
            for h in range(4):
                hp = slice((h % 2) * 64, (h % 2) * 64 + 64)
                k.mm(pN[H, h * 65:(h + 1) * 65], ST[H, h, :], vS[H, h, :])
                k.mm(pI[H, h * 65:(h + 1) * 65], qkT[hp, h // 2, tok], CT[hp, dr, h // 2, :])
        sec(5)
        B65 = lambda v: bc(V(v.ap.unsqueeze(2), v.res), [128, 4, 65])
        k.tt("dve", tI.v(), rr(pI[:, 0:260], "p (h e) -> p h e", h=4), B65(wint), OP.mult)
        k.tt("dve", nd.v(), tI.v(), rr(pN[:, 0:260], "p (h e) -> p h e", h=4), OP.add)
        k.act(dabs, nd[:, :, 64], AF.Abs)
        k.tt("dve", dabs, dabs, emt, OP.max)
        k.recip(rden, dabs)
        k.tt("dve", hout.v(), nd[:, :, 0:64], B4(rden), OP.mult)
        sec(6)
        for dr in range(2):
            c = chunks[dr]
            H = slice(dr * 64, dr * 64 + 64)
            Hc = slice((c % 2) * 64, (c % 2) * 64 + 64)
            k.mm(pS[Hc, 256:512], ident[H, H], rr(hout[H, :, :], "p h e -> p (h e)"))
            k.tt("pool" if False else "dve", hsum[Hc, c // 2, :], hsum[Hc, c // 2, :], pS[Hc, 256:512], OP.add)
        sec(7)
        k.tt("dve", mmax.v(), mfull.v(), rmax.v(), OP.max)
        k.tt("dve", dec.v(), mfull.v(), mmax.v(), OP.subtract)
        k.act(dec.v(), dec.v(), AF.Exp)
        for dr in range(2):
            H = slice(dr * 64, dr * 64 + 64)
            k.tt("dve", V(sm.h[H, 36:40], sm.res), lmb[H, :], mmax[H, dr * 4:dr * 4 + 4], OP.subtract)
        k.act(wk, mmA, AF.Exp)
        k.tt("dve", kw.v(), kS.v(), B4(wk), OP.mult)
        for dr in range(2):
            H = slice(dr * 64, dr * 64 + 64)
            for h in range(4):
                hp = slice((h % 2) * 64, (h % 2) * 64 + 64)
                o0 = (dr * 2 + h // 2) * 65
                k.mm(pU[hp, o0:o0 + 65], kw[H, h, :], vS[H, h, :])
        for hpi in range(2):
            hp = slice(hpi * 64, hpi * 64 + 64)
            dview = V(dec.h[hp, :].rearrange("p (d hh hp) -> p d hh hp", d=2, hh=2, hp=2)[:, :, :, hpi].unsqueeze(3).to_broadcast([64, 2, 2, 65]), dec.res)
            k.tt("dve", CT[hp, :, :, :], CT[hp, :, :, :], dview, OP.mult)
        k.tt("dve", CT.v(), CT.v(), rr(pU[:, 0:260], "p (d hh e) -> p d hh e", d=2, hh=2), OP.add)
        k.tt("dve", mfull.v(), cs[:, 4:12], mmax.v(), OP.add)
    k.mute = False
    if "ml_hsum" in P.debug:
        P.dump("ml_hsum", hsum.v())
    if os.environ.get("MLSTOP") == "2":
        k.barrier(); es.close(); return
    osig = tile("mlosig", [128, NTILE, 256])
    for ti in range(NTILE):
        pt = pb[ti % 2]
        for dc in range(8):
            k.mm(pt[:, 0:256], hT[:, dc, ti * 128:(ti + 1) * 128], wml[:, dc, 768:1024], start=(dc == 0), stop=(dc == 7))
        k.act(osig[:, ti, :], pt[:, 0:256], AF.Sigmoid)
    NG = NTILE * 4
    h3 = rr(hsum.v(), "p t (h e) -> p (t h) e", h=4)
    st = tile("mlst", [128, 4, NG])
    sq = tile("mlsq", [128, NG, 64])
    BG = lambda v: bc(V(v.ap.unsqueeze(2), v.res), [128, NG, 64])
    k.red("dve", st[:, 0, :], h3, OP.add)
    k.ts("dve", st[:, 0, :], st[:, 0, :], 1.0 / 64, OP.mult)
    k.tt("dve", h3, h3, BG(st[:, 0, :]), OP.subtract)
    k.tt("pool", sq.v(), h3, h3, OP.mult)
    k.red("dve", st[:, 1, :], sq.v(), OP.add)
    k.ts("dve", st[:, 1, :], st[:, 1, :], 1.0 / 64, OP.mult, LN_EPS, OP.add)
    k.act(st[:, 2, :], st[:, 1, :], AF.Sqrt)
    k.recip(st[:, 3, :], st[:, 2, :])
    k.tt("dve", h3, h3, BG(st[:, 3, :]), OP.mult)
    k.tt("pool", hsum.v(), hsum.v(), bc(V(ng.h[:].unsqueeze(1), ng.res), [128, NTILE, 256]), OP.mult)
    k.tt("dve", hsum.v(), hsum.v(), osig.v(), OP.mult)
    oT = [tile("mloT%d" % i, [128, 2, 128]) for i in range(2)]
    for ti in range(NTILE):
        pt = pb[ti % 2]
        for c in range(2):
            k.mm(pt[:, c * 128:(c + 1) * 128], hsum[:, ti, c * 128:(c + 1) * 128], ident.v())
        o_ = oT[ti % 2]
        k.cp("act", o_.v(), rr(pt[:, 0:256], "p (c t) -> p c t", c=2))
        k.dma(MIXD[0:2, :, ti * 128:(ti + 1) * 128].rearrange("c p t -> p c t"), o_.h[:], reads=o_.res, writes=MIX_res)
    k.barrier()
    es.close()


RW_C = 0.6065306597126334


def phase_rwkv_proj(P, l, hT, w_in, prm, PRW, PRW_res):
    k = P.k
    nc = P.nc
    es = ExitStack()
    tile = lambda name, shape, dt=F32: T(es.enter_context(usb(nc, "t_" + name, shape, dt)), name)
    wrw = tile("wrw", [128, 8, 1536], BF16)
    load_w_bf16(P, wrw, w_in[l, :, 1040:2576], 1536)
    mu = tile("rwmu", [128, 12, 2]); k.ld(mu.v(), prm["rw_muT"][l])
    c0 = tile("rwc0", [128, 12])
    k.tt("dve", c0.v(), mu[:, :, 0], mu[:, :, 1], OP.add)
    k.ts("dve", c0.v(), c0.v(), -1.0, OP.mult, 1.0, OP.add)
    raw = [tile("rwraw%d" % i, [128, NT]) for i in range(2)]
    outb = [tile("rwout%d" % i, [128, NT]) for i in range(2)]
    ib = 0
    for ch in range(12):
        rw_ = raw[ch % 2]; ob = outb[ch % 2]
        for tb in range(0, NT, 512):
            n = min(512, NT - tb)
            pt = P.pb[ib % 2]; ib += 1
            for dc in range(8):
                k.mm(pt[:, 0:n], wrw[:, dc, ch * 128:(ch + 1) * 128], hT[:, dc, tb:tb + n], start=(dc == 0), stop=(dc == 7))
            k.cp("act", rw_[:, tb:tb + n], pt[:, 0:n])
        k.act(ob.v(), rw_.v(), AF.Identity, scale=c0[:, ch:ch + 1])
        for (s0, s1) in ((0, NCTX), (NCTX, NT)):
            k.stt("dve", ob[:, s0 + 1:s1], rw_[:, s0:s1 - 1], mu[:, ch, 0:1], ob[:, s0 + 1:s1], OP.mult, OP.add)
            k.stt("dve", ob[:, s0:s1 - 1], rw_[:, s0 + 1:s1], mu[:, ch, 1:2], ob[:, s0:s1 - 1], OP.mult, OP.add)
        k.dma(PRW[ch], ob.h[:], reads=ob.res, writes=PRW_res)
    k.barrier()
    es.close()


def phase_rwkv(P, l, prm, PRW, PRW_res, MIXT, MIXT_res):
    k = P.k
    nc = P.nc
    es = ExitStack()
    tile = lambda name, shape, dt=F32: T(es.enter_context(usb(nc, "t_" + name, shape, dt)), name)
    ident = P.ident
    pb = P.pb
    import os
    NSTEP = int(os.environ.get("RWSTEPS", NCH))
    NGRP = int(os.environ.get("RWGRPS", 3))
    tw = tile("rw_tw", [128, NT]); ad = tile("rw_ad", [128, NT]); sg = tile("rw_sg", [128, NT])
    k.dma(tw.h[:], PRW[9], reads=PRW_res, writes=tw.res)
    k.dma(ad.h[:], PRW[10], reads=PRW_res, writes=ad.res)
    k.dma(sg.h[:], PRW[11], reads=PRW_res, writes=sg.res)
    k.act(tw.v(), tw.v(), AF.Tanh)
    k.act(sg.v(), sg.v(), AF.Sigmoid)
    wup = tile("rw_wup_sb", [128, 384]); k.ld(wup.v(), prm["rw_wup"][l])
    aup = tile("rw_aup_sb", [128, 384]); k.ld(aup.v(), prm["rw_aup"][l])
    gup = tile("rw_gup_sb", [128, 384]); k.ld(gup.v(), prm["rw_gup"][l])
    rwc = tile("rw_cols_sb", [128, 3, 9]); k.ld(rwc.v(), prm["rw_cols"][l])
    omka = tile("rw_omka", [128, 3])
    k.ts("dve", omka.v(), rwc[:, :, 1], -1.0, OP.mult, 1.0, OP.add)
    BD2 = tile("rw_bd2", [128, 128]); k.ld(BD2.v(), P.din["c_BD2"])
    msk = tile("rw_mask", [128, 2, 320]); k.ld(msk.v(), P.din["c_rwmask"])
    rst = tile("rw_rst", [128, NCH, 64], BF16)
    k.memset("dve", rst.v(), 1.0); k.memset("dve", rst[:, :, 0:1], 0.0)
    b_r = tile("rw_r", [128, NT]); b_k = tile("rw_k", [128, NT]); b_v = tile("rw_v", [128, NT])
    b_kh = tile("rw_kh", [128, NT]); b_bs = tile("rw_bs", [128, NT])
    b_ys = tile("rw_ys", [128, NT])
    b_lw = tile("rw_lw", [128, NT]); b_a = tile("rw_a", [128, NT]); b_kt = tile("rw_kt", [128, NT])
    b_cl = tile("rw_cl", [128, NT]); b_At = tile("rw_At", [128, NT])
    gC = tile("rw_gC", [128, NCH])
    ST = tile("rw_ST", [128, 64])
    STb = tile("rw_STb", [128, 64], BF16)
    Mm = tile("rw_Mm", [128, 320], BF16)
    TT = tile("rw_TT", [128, 192], BF16)
    XT = [tile("rw_XT%d" % i, [128, 64], BF16) for i in range(2)]
    Mj = [tile("rw_Mj%d" % i, [128, 64], BF16) for i in range(2)]
    MjT = [tile("rw_MjT%d" % i, [128, 64], BF16) for i in range(2)]
    c_At = tile("rw_cAt", [128, NT], BF16); c_Rt = tile("rw_cRt", [128, NT], BF16)
    c_Bt = tile("rw_cBt", [128, NT], BF16); c_Kt = tile("rw_cKt", [128, NT], BF16)
    c_v = tile("rw_cv", [128, NT], BF16)
    identb = P.identb
    HP = [slice(0, 64), slice(64, 128)]

    def blocks():
        for tb in range(0, NT, 512):
            yield tb, min(512, NT - tb)

    def bd_sum(dst, src, scale=1.0):
        for i, (tb, n) in enumerate(blocks()):
            pt = pb[i % 2]
            k.mm(pt[:, 0:n], BD2.v(), src[:, tb:tb + n])
            k.act(dst[:, tb:tb + n], pt[:, 0:n], AF.Identity, scale=scale)

    for g in range(NGRP):
        k.dma(b_r.h[:], PRW[g], reads=PRW_res, writes=b_r.res)
        k.dma(b_k.h[:], PRW[3 + g], reads=PRW_res, writes=b_k.res)
        k.dma(b_v.h[:], PRW[6 + g], reads=PRW_res, writes=b_v.res)
        gs = slice(g * 128, (g + 1) * 128)
        k.ts("dve", b_kh.v(), b_k.v(), rwc[:, g, 0:1], OP.mult)
        k.tt("pool", b_At.v(), b_kh.v(), b_kh.v(), OP.mult)
        bd_sum(b_cl, b_At)
        k.ts("dve", b_cl.v(), b_cl.v(), 1e-12, OP.add)
        k.act(b_cl.v(), b_cl.v(), AF.Sqrt)
        k.recip(b_cl.v(), b_cl.v())
        k.tt("dve", b_kh.v(), b_kh.v(), b_cl.v(), OP.mult)
        k.memset("pool", b_ys.v(), 0.0)
        for dr in range(2):
            D = slice(dr * 64, dr * 64 + 64)
            for i, (tb, n) in enumerate(blocks()):
                pt = pb[i % 2]
                k.mm(pt[:, 0:n], wup[D, gs], tw[D, tb:tb + n])
                k.act(b_lw[:, tb:tb + n], pt[:, 0:n], AF.Sigmoid, bias=rwc[:, g, 5 + dr:6 + dr])
            k.ts("dve", b_lw.v(), b_lw.v(), -RW_C, OP.mult)
            for i, (tb, n) in enumerate(blocks()):
                pt = pb[2 + i % 2]
                k.mm(pt[:, 0:n], aup[D, gs], ad[D, tb:tb + n])
                k.act(b_a[:, tb:tb + n], pt[:, 0:n], AF.Sigmoid, bias=rwc[:, g, 7 + dr:8 + dr])
            k.ts("dve", b_kt.v(), b_a.v(), rwc[:, g, 1:2], OP.mult, omka[:, g:g + 1], OP.add)
            k.tt("dve", b_kt.v(), b_kt.v(), b_k.v(), OP.mult)
            k.tt("pool", b_a.v(), b_a.v(), b_kh.v(), OP.mult)
            k.stt("dve", b_At.v(), b_r.v(), rwc[:, g, 2:3], b_kt.v(), OP.mult, OP.mult)
            for i, (tb, n) in enumerate(blocks()):
                pt = pb[i % 2]
                k.mm(pt[:, 0:n], BD2.v(), b_At[:, tb:tb + n])
                if dr == 0:
                    k.tt("dve", b_bs[:, tb:tb + n], pt[:, 0:n], b_v[:, tb:tb + n], OP.mult)
                else:
                    k.tt("dve", b_At[:, tb:tb + n], pt[:, 0:n], b_v[:, tb:tb + n], OP.mult)
            if dr == 1:
                k.tt("pool", b_bs.v(), b_bs.v(), b_At.v(), OP.add)
            k.op("dve", lambda e: e.tensor_tensor_scan(b_cl.h[:], rst.h[:].rearrange("p c s -> p (c s)"), b_lw.h[:], 0.0, OP.mult, OP.add),
                 rst.res + b_lw.res, b_cl.res)
            cl3 = rr(b_cl.v(), "p (c s) -> p c s", s=64)
            k.cp("dve", gC.v(), cl3[:, :, 63])
            if dr == 1:
                k.tt("dve", cl3, bc(V(gC.h[:].unsqueeze(2), gC.res), [128, NCH, 64]), cl3, OP.subtract)
                k.tt("dve", b_cl.v(), b_cl.v(), b_lw.v(), OP.add)
            k.act(gC.v(), gC.v(), AF.Exp)
            k.tt("dve", b_lw.v(), b_cl.v(), b_lw.v(), OP.subtract)
            k.act(b_lw.v(), b_lw.v(), AF.Exp)
            k.stt("dve", b_At.v(), b_kh.v(), -1.0, b_lw.v(), OP.mult, OP.mult)
            k.act(b_lw.v(), b_cl.v(), AF.Exp)
            k.tt("dve", b_lw.v(), b_lw.v(), b_r.v(), OP.mult)
            k.act(b_cl.v(), b_cl.v(), AF.Exp, scale=-1.0)
            k.tt("dve", b_a.v(), b_a.v(), b_cl.v(), OP.mult)
            k.tt("pool", b_kt.v(), b_kt.v(), b_cl.v(), OP.mult)
            k.cp("pool", c_At.v(), b_At.v()); k.cp("act", c_Rt.v(), b_lw.v())
            k.cp("pool", c_Bt.v(), b_a.v()); k.cp("act", c_Kt.v(), b_kt.v())
            if dr == 0:
                k.cp("pool", c_v.v(), b_v.v())
            b_Rt, b_Bt, b_Kt = c_Rt, c_Bt, c_Kt
            k.memset("dve", ST.v(), 0.0)
            k.memset("dve", STb.v(), 0.0)
            order = ORD_F if dr == 0 else ORD_R
            pM, pT, pW, pX, pQ, pQT, pY, pS = pb
            for step in range(NSTEP):
                c = order[step]
                tok = slice(c * 64, c * 64 + 64)
                for hp in HP:
                    k.mm(pM[hp, 0:64], b_Bt[hp, tok], c_At[hp, tok])
                    k.mm(pM[hp, 64:128], b_Bt[hp, tok], b_Rt[hp, tok])
                    k.mm(pM[hp, 128:192], b_Kt[hp, tok], c_At[hp, tok])
                    k.mm(pM[hp, 192:256], b_Kt[hp, tok], b_Rt[hp, tok])
                    k.mm(pM[hp, 256:320], c_At[hp, tok], b_Bt[hp, tok])
                    k.mm(pT[hp, 0:64], c_v[hp, tok], identb[hp, hp])
                    k.mm(pT[hp, 64:128], b_Bt[hp, tok], identb[hp, hp])
                    k.mm(pT[hp, 128:192], b_Kt[hp, tok], identb[hp, hp])
                k.tt("dve", Mm.v(), pM[:, 0:320], msk[:, dr, :], OP.mult)
                k.cp("act", TT.v(), pT[:, 0:192])
                VT, BtT, KtT = TT[:, 0:64], TT[:, 64:128], TT[:, 128:192]
                M1, N1, M2, N2, M1T = [Mm[:, i * 64:(i + 1) * 64] for i in range(5)]
                for hp in HP:
                    k.mm(pW[hp, 0:64], c_At[hp, tok], STb[hp, :], start=True, stop=False)
                    k.mm(pW[hp, 0:64], V(M2.ap[hp], M2.res), V(VT.ap[hp], VT.res), start=False, stop=True)
                x = XT[0]
                k.cp("act", x.v(), pW[:, 0:64])
                mj, mjT = M1, M1T
                for j in range(6):
                    xn = XT[(j + 1) % 2]
                    for hp in HP:
                        k.mm(pX[hp, 0:64], V(mj.ap[hp], mj.res), x[hp, :])
                    k.tt("dve", xn.v(), pX[:, 0:64], x.v(), OP.add)
                    x = xn
                    if j < 5:
                        for hp in HP:
                            k.mm(pQ[hp, 0:64], V(mjT.ap[hp], mjT.res), V(mj.ap[hp], mj.res))
                            k.mm(pQT[hp, 0:64], V(mj.ap[hp], mj.res), V(mjT.ap[hp], mjT.res))
                        nmj = Mj[j % 2]; nmjT = MjT[j % 2]
                        k.cp("act", nmj.v(), pQ[:, 0:64])
                        k.cp("dve", nmjT.v(), pQT[:, 0:64])
                        mj, mjT = nmj.v(), nmjT.v()
                UT = x
                for hp in HP:
                    k.mm(pY[hp, 0:64], STb[hp, :], b_Rt[hp, tok], start=True, stop=False)
                    k.mm(pY[hp, 0:64], UT[hp, :], V(N1.ap[hp], N1.res), start=False, stop=False)
                    k.mm(pY[hp, 0:64], V(VT.ap[hp], VT.res), V(N2.ap[hp], N2.res), start=False, stop=True)
                k.tt("dve", b_ys[:, tok], b_ys[:, tok], pY[:, 0:64], OP.add)
                for hp in HP:
                    k.mm(pS[hp, 0:64], V(BtT.ap[hp], BtT.res), UT[hp, :], start=True, stop=False)
                    k.mm(pS[hp, 0:64], V(KtT.ap[hp], KtT.res), V(VT.ap[hp], VT.res), start=False, stop=True)
                k.tt("dve", ST.v(), ST.v(), pS[:, 0:64], OP.add)
                k.ts("dve", ST.v(), ST.v(), gC[:, c:c + 1], OP.mult)
                k.cp("act", STb.v(), ST.v())
        if "rw_ys" in P.debug and g == 0:
            P.dump("rw_ys", b_ys.v())
        bd_sum(b_cl, b_ys, 1.0 / 64)
        k.tt("dve", b_ys.v(), b_ys.v(), b_cl.v(), OP.subtract)
        k.tt("pool", b_At.v(), b_ys.v(), b_ys.v(), OP.mult)
        bd_sum(b_cl, b_At, 1.0 / 64)
        k.ts("dve", b_cl.v(), b_cl.v(), 64e-5, OP.add)
        k.act(b_cl.v(), b_cl.v(), AF.Sqrt)
        k.recip(b_cl.v(), b_cl.v())
        k.tt("dve", b_ys.v(), b_ys.v(), b_cl.v(), OP.mult)
        k.ts("dve", b_ys.v(), b_ys.v(), rwc[:, g, 3:4], OP.mult, rwc[:, g, 4:5], OP.add)
        k.tt("dve", b_ys.v(), b_ys.v(), b_bs.v(), OP.add)
        for i, (tb, n) in enumerate(blocks()):
            pt = pb[i % 2]
            k.mm(pt[:, 0:n], gup[:, gs], sg[:, tb:tb + n])
            k.tt("dve", b_ys[:, tb:tb + n], b_ys[:, tb:tb + n], pt[:, 0:n], OP.mult)
        k.dma(MIXT[2 + g], b_ys.h[:], reads=b_ys.res, writes=MIXT_res)
    k.barrier()
    es.close()


MLA_SCALE = 96 ** -0.5


def phase_mla(P, l, hT, w_in, prm, MIXT, MIXT_res, need_ctx=True):
    k = P.k
    nc = P.nc
    es = ExitStack()
    tile = lambda name, shape, dt=F32: T(es.enter_context(usb(nc, "t_" + name, shape, dt)), name)
    ident = P.ident
    pb = P.pb
    wat = tile("wat", [128, 8, 672], BF16)
    load_w_bf16(P, wat, w_in[l, :, 2576:3248], 672)
    qup = tile("mla_qup", [128, 3, 576], BF16)
    kvup = tile("mla_kvup", [128, 2, 768], BF16)
    stg = tile("mla_stg", [128, 3, 768])
    k.ld(stg[:, 0:3, 0:576], prm["mla_q_up"][l].rearrange("(c p) n -> p c n", p=128))
    k.cp("dve", qup.v(), stg[:, 0:3, 0:576])
    k.ld(stg[:, 0:2, :], prm["mla_kv_up"][l].rearrange("(c p) n -> p c n", p=128))
    k.cp("dve", kvup.v(), stg[:, 0:2, :])
    gq = tile("mla_gq", [128, 384]); k.ld(gq.v(), prm["mla_gq"][l])
    gkv = tile("mla_gkv", [128, 256]); k.ld(gkv.v(), prm["mla_gkv"][l])
    QT = tile("mla_QT", [96, 6, NT], BF16)
    KT = tile("mla_KT", [96, 6, NT], BF16)
    Vt = tile("mla_V", [128, NTILE, 6, 65], BF16)
    k.memset("pool", Vt.v(), 1.0)
    pa = tile("mla_pa", [128, 672])
    junk = tile("mla_junk", [128, 384])
    st = tile("mla_st", [128, 8])
    cn = tile("mla_cn", [128, 640])
    cnT = tile("mla_cnT", [128, 5, 128], BF16)
    qt = tile("mla_q", [128, 6, 96])
    kvt = tile("mla_kv", [128, 6, 128])
    kf = tile("mla_kf", [128, 6, 96])
    rope = tile("mla_rope", [128, 32])
    rt = tile("mla_rt", [128, 4, 6, 2, 8])
    for ti in range(NTILE):
        tk = slice(ti * 128, (ti + 1) * 128)
        p0, p1 = pb[0], pb[1]
        for dc in range(8):
            k.mm(p0[:, 0:512], hT[:, dc, tk], wat[:, dc, 0:512], start=(dc == 0), stop=(dc == 7))
        for dc in range(8):
            k.mm(p1[:, 0:160], hT[:, dc, tk], wat[:, dc, 512:672], start=(dc == 0), stop=(dc == 7))
        k.cp("act", pa[:, 0:512], p0[:, 0:512])
        k.cp("dve", pa[:, 512:672], p1[:, 0:160])
        k.act(junk[:, 0:384], pa[:, 0:384], AF.Square, accum=st[:, 0:1])
        k.act(junk[:, 0:256], pa[:, 384:640], AF.Square, accum=st[:, 1:2])
        k.ts("dve", st[:, 2:3], st[:, 0:1], 1.0 / 384, OP.mult, LN_EPS, OP.add)
        k.ts("dve", st[:, 3:4], st[:, 1:2], 1.0 / 256, OP.mult, LN_EPS, OP.add)
        k.act(st[:, 4:6], st[:, 2:4], AF.Sqrt)
        k.recip(st[:, 6:8], st[:, 4:6])
        k.stt("dve", cn[:, 0:384], pa[:, 0:384], st[:, 6:7], gq.v(), OP.mult, OP.mult)
        k.stt("dve", cn[:, 384:640], pa[:, 384:640], st[:, 7:8], gkv.v(), OP.mult, OP.mult)
        p2, p3 = pb[2], pb[3]
        for c in range(4):
            k.mm(p2[:, c * 128:(c + 1) * 128], cn[:, c * 128:(c + 1) * 128], ident.v())
        k.mm(p3[:, 0:128], cn[:, 512:640], ident.v())
        k.cp("act", cnT[:, 0:4, :], rr(p2.v(), "p (c t) -> p c t", c=4))
        k.cp("dve", cnT[:, 4, :], p3[:, 0:128])
        p4, p5, p6, p7 = pb[4], pb[5], pb[6], pb[7]
        for c in range(3):
            k.mm(p4[:, 0:512], cnT[:, c, :], qup[:, c, 0:512], start=(c == 0), stop=(c == 2))
        for c in range(3):
            k.mm(p5[:, 0:64], cnT[:, c, :], qup[:, c, 512:576], start=(c == 0), stop=(c == 2))
        for c in range(2):
            k.mm(p6[:, 0:512], cnT[:, 3 + c, :], kvup[:, c, 0:512], start=(c == 0), stop=(c == 1))
        for c in range(2):
            k.mm(p7[:, 0:256], cnT[:, 3 + c, :], kvup[:, c, 512:768], start=(c == 0), stop=(c == 1))
        qf = rr(qt.v(), "p h e -> p (h e)")
        k.cp("act", qf[:, 0:512], p4[:, 0:512])
        k.cp("dve", qf[:, 512:576], p5[:, 0:64])
        kvf = rr(kvt.v(), "p h e -> p (h e)")
        k.cp("act", kvf[:, 0:512], p6[:, 0:512])
        k.cp("dve", kvf[:, 512:768], p7[:, 0:256])
        k.cp("pool", kf[:, :, 0:64], kvt[:, :, 0:64])
        k.cp("pool", Vt[:, ti, :, 0:64], kvt[:, :, 64:128])
        kr = pa[:, 640:672]
        if ti >= NCTX // 128:
            k.ld(rope.v(), P.din["c_rope"][(ti - NCTX // 128) * 128:(ti - NCTX // 128 + 1) * 128, :])
            cosv = rr(rope[:, 0:16], "p (a f) -> p a f", a=2)
            sinv = rr(rope[:, 16:32], "p (a f) -> p a f", a=2)
            for (xv, H_) in ((rr(qt[:, :, 64:96], "p h (a s f) -> p h a s f", a=2, s=2), 6),
                             (rr(V(kr.ap.unsqueeze(1), kr.res), "p h (a s f) -> p h a s f", a=2, s=2), 1)):
                x1 = xv[:, :, :, 0, :]; x2 = xv[:, :, :, 1, :]
                cb = bc(V(cosv.ap.unsqueeze(1), cosv.res), [128, H_, 2, 8])
                sb_ = bc(V(sinv.ap.unsqueeze(1), sinv.res), [128, H_, 2, 8])
                t1, t2, t3, t4 = [rt[:, i, 0:H_, :, :] for i in range(4)]
                k.tt("dve", t1, x1, cb, OP.mult)
                k.tt("pool", t2, x2, sb_, OP.mult)
                k.tt("dve", t3, x2, cb, OP.mult)
                k.tt("pool", t4, x1, sb_, OP.mult)
                k.tt("dve", x1, t1, t2, OP.subtract)
                k.tt("dve", x2, t3, t4, OP.add)
        k.cp("dve", kf[:, :, 64:96], bc(V(kr.ap.unsqueeze(1), kr.res), [128, 6, 32]))
        for (src, dstT, pp) in ((qt, QT, (pb[0], pb[1])), (kf, KT, (pb[2], pb[3]))):
            for half in range(2):
                pt = pp[half]
                for j in range(3):
                    h = half * 3 + j
                    k.mm(pt[0:96, j * 128:(j + 1) * 128], src[:, h, :], ident.v())
                k.cp("act" if half else "dve", dstT[:, half * 3:half * 3 + 3, tk],
                     rr(pt[0:96, 0:384], "p (j t) -> p j t", j=3))
    if "mla_QT" in P.debug:
        P.dump("mla_QT", QT.v()); P.dump("mla_KT", KT.v())
    E = [tile("mla_E%d" % i, [128, 512], BF16) for i in range(2)]
    P.mla_oT = [tile("mla_oT%d" % i, [128, 3, 128]) for i in range(2)]
    osb = tile("mla_o", [128, 4, 6, 64])
    rs = tile("mla_rs", [128, 4])
    qblocks = [(NCTX + i * 512, 512, list(range(NTILE))) for i in range(4)]
    if need_ctx:
        qblocks.append((0, 256, [0, 1]))
    ei = 0
    for (q0, qn, ktiles) in qblocks:
        nsub = qn // 128
        for h in range(6):
            for kidx, kt_ in enumerate(ktiles):
                ps = pb[4 + ei % 2]
                e_ = E[ei % 2]; ei += 1
                k.mm(ps[:, 0:qn], KT[:, h, kt_ * 128:(kt_ + 1) * 128], QT[:, h, q0:q0 + qn])
                k.act(e_[:, 0:qn], ps[:, 0:qn], AF.Exp, scale=MLA_SCALE)
                for j in range(nsub):
                    k.mm(pb[j][:, 0:65], e_[:, j * 128:(j + 1) * 128], Vt[:, kt_, h, :],
                         start=(kidx == 0), stop=(kidx == len(ktiles) - 1))
            for j in range(nsub):
                k.recip(rs[:, j:j + 1], pb[j][:, 64:65])
                k.ts("dve", osb[:, j, h, :], pb[j][:, 0:64], rs[:, j:j + 1], OP.mult)
        for j in range(nsub):
            pt = pb[6 + j % 2]
            of = rr(osb[:, j, :, :], "p h e -> p (h e)")
            for c in range(3):
                k.mm(pt[:, c * 128:(c + 1) * 128], of[:, c * 128:(c + 1) * 128], ident.v())
            ot = tile("mla_oT_%d_%d" % (q0, j), [128, 3, 128]) if False else P.mla_oT[j % 2]
            k.cp("act", ot.v(), rr(pt[:, 0:384], "p (c t) -> p c t", c=3))
            t0_ = q0 + j * 128
            k.dma(MIXT[5:8, :, t0_:t0_ + 128].rearrange("c p t -> p c t"), ot.h[:], reads=ot.res, writes=MIXT_res)
    k.barrier()
    es.close()


def phase_mixout(P, l, w_out, prm, MIXT, MIXT_res, src_ap, src_res, XD, XD_res, MODD, MOD_res, need_ctx=True):
    k = P.k
    nc = P.nc
    es = ExitStack()
    tile = lambda name, shape, dt=F32: T(es.enter_context(usb(nc, "t_" + name, shape, dt)), name)
    pb = P.pb
    wout = tile("wout", [128, 8, 1024], BF16)
    load_w_bf16(P, wout, w_out[l], 1024)
    ga = [tile("mo_ga%d" % s, [128, D]) for s in range(2)]
    for s in range(2):
        k.dma(ga[s].h[:], MODD[s, MOD_GA], reads=MOD_res, writes=ga[s].res)
    lng = tile("mo_lng", [128, D]); k.ld(lng.v(), prm["ln_mix_g"][l])
    lnb = tile("mo_lnb", [128, D]); k.ld(lnb.v(), prm["ln_mix_b"][l])
    mt = [tile("mo_mt%d" % i, [128, 8, 128]) for i in range(2)]
    mb = [tile("mo_mb%d" % i, [128, 8, 128], BF16) for i in range(2)]
    xt = [tile("mo_x%d" % i, [128, D]) for i in range(2)]
    ut = [tile("mo_u%d" % i, [128, D]) for i in range(2)]
    st = [tile("mo_st%d" % i, [128, 16]) for i in range(2)]
    t0 = 0 if need_ctx else NCTX // 128
    for ti in range(t0, NTILE):
        s = 1 if ti < NCTX // 128 else 0
        m_ = mt[ti % 2]; b_ = mb[ti % 2]; x = xt[ti % 2]; u = ut[ti % 2]; st_ = st[ti % 2]
        k.dma(m_.h[:], MIXT[:, :, ti * 128:(ti + 1) * 128].rearrange("c p t -> p c t"), reads=MIXT_res, writes=m_.res)
        k.dma(x.h[:], src_ap[ti * 128:(ti + 1) * 128, :], reads=src_res[ti], writes=x.res)
        k.cp("pool", b_.v(), m_.v())
        for half in range(2):
            pt = pb[(ti * 2 + half) % 4]
            for c in range(8):
                k.mm(pt.v(), b_[:, c, :], wout[:, c, half * 512:(half + 1) * 512], start=(c == 0), stop=(c == 7))
            k.tt("dve", u[:, half * 512:(half + 1) * 512], pt.v(), ga[s][:, half * 512:(half + 1) * 512], OP.mult)
        k.stt("dve", u.v(), x.v(), ALPHA, u.v(), OP.mult, OP.add)
        mean, rstd, nmr = ln_stats(P, u.v(), st_)
        k.act(u.v(), u.v(), AF.Identity, bias=nmr, scale=rstd)
        k.tt("pool", u.v(), u.v(), lng.v(), OP.mult)
        k.tt("dve", u.v(), u.v(), lnb.v(), OP.add)
        k.dma(XD[ti * 128:(ti + 1) * 128, :], u.h[:], reads=u.res, writes=XD_res[ti])
    k.barrier()
    es.close()


def phase_peer(P, l, prm, peer_w_q, peer_keys, peer_u, peer_v, XD, XD_res, dst_ap, MODD, MOD_res, need_ctx=True):
    import os
    k = P.k
    nc = P.nc
    es = ExitStack()
    tile = lambda name, shape, dt=F32: T(es.enter_context(usb(nc, "t_" + name, shape, dt)), name)
    pb = P.pb
    ident = P.ident
    NSLOT = int(os.environ.get("PEER_SLOTS", 128))
    wq = tile("pe_wq", [128, 8, 2048])
    for c0 in range(0, 2048, 512):
        k.ld(wq[:, :, c0:c0 + 512], peer_w_q[l, :, c0:c0 + 512].rearrange("(c p) n -> p c n", p=128))
    keysT = tile("pe_keysT", [128, 16, 128])
    kst = tile("pe_kst", [128, 4, 128])
    for g4 in range(4):
        k.ld(kst.v(), peer_keys[l].rearrange("h p k d -> k (h p) d")[:, g4 * 4:(g4 + 1) * 4, :])
        pt = pb[g4 % 2]
        for j in range(4):
            k.tr(pt[:, j * 128:(j + 1) * 128], kst[:, j, :], ident.v())
        k.cp("act", keysT[:, g4 * 4:(g4 + 1) * 4, :], rr(pt.v(), "p (j t) -> p j t", j=4))
    mods = {}
    for s in ([0, 1] if need_ctx else [0]):
        for j in (MOD_SHF, MOD_SCF, MOD_GF):
            mods[(s, j)] = tile("pe_mod%d_%d" % (s, j), [128, D])
            k.dma(mods[(s, j)].h[:], MODD[s, j], reads=MOD_res, writes=mods[(s, j)].res)
        k.ts("pool", mods[(s, MOD_SCF)].v(), mods[(s, MOD_SCF)].v(), 1.0, OP.add)
    lng = tile("pe_lng", [128, D]); k.ld(lng.v(), prm["ln_ffn_g"][l])
    lnb = tile("pe_lnb", [128, D]); k.ld(lnb.v(), prm["ln_ffn_b"][l])
    iota16 = tile("pe_iota", [128, 16]); k.ld(iota16.v(), P.din["c_iota16"])
    x = tile("pe_x", [128, D]); h = tile("pe_h", [128, D]); st = tile("pe_st", [128, 16])
    hT = tile("pe_hT", [128, 8, 128])
    qT = tile("pe_qT", [128, 16, 128])
    sc = tile("pe_s", [128, 16, 128])
    wk = tile("pe_wk", [128, 128])
    vals = tile("pe_vals", [128, 16, 16])
    idxu = tile("pe_idxu", [128, 16, 16], U32)
    idxf = tile("pe_idxf", [128, 16, 16])
    cand = tile("pe_cand", [128, 8, 16, 16])
    wk2 = tile("pe_wk2", [128, 256])
    best = tile("pe_best", [128, 8, 16])
    posu = tile("pe_posu", [128, 8, 16], U32)
    posu2 = tile("pe_posu2", [128, 2, 8, 16], U32)
    cu32 = tile("pe_cu32", [128, 2], U32); k.ld(cu32.v(), P.din["c_u32"])
    pa_ = tile("pe_pa", [128, 8, 16]); pb_ = tile("pe_pb", [128, 8, 16])
    oh = tile("pe_oh", [128, 8, 16, 16])
    eidf = tile("pe_eidf", [128, 8, 16]); eid2 = tile("pe_eid2", [128, 8, 16])
    eidu = tile("pe_eidu", [128, 128], U32)
    gate = tile("pe_gate", [128, 8, 16]); gs = tile("pe_gs", [128, 8])
    actv = tile("pe_act", [128, 128]); t1 = tile("pe_t1", [128, 128]); wgt = tile("pe_wgt", [128, 128])
    NB = 4
    ug = [tile("pe_ug%d" % i, [128, D]) for i in range(NB)]
    vg = ug
    junk = tile("pe_junk", [128, D])
    acc = tile("pe_acc", [128, D])
    t0 = 0 if need_ctx else NCTX // 128
    NTL = int(os.environ.get("PEER_TILES", NTILE))
    for ti in range(t0, min(NTILE, t0 + NTL)):
        s = 1 if ti < NCTX // 128 else 0
        k.dma(x.h[:], XD[ti * 128:(ti + 1) * 128, :], reads=XD_res[ti], writes=x.res)
        mean, rstd, nmr = ln_stats(P, x.v(), st)
        k.act(h.v(), x.v(), AF.Identity, bias=nmr, scale=rstd)
        k.tt("dve", h.v(), h.v(), mods[(s, MOD_SCF)].v(), OP.mult)
        k.tt("pool", h.v(), h.v(), mods[(s, MOD_SHF)].v(), OP.add)
        for g4 in range(2):
            pt = pb[g4]
            for j in range(4):
                k.tr(pt[:, j * 128:(j + 1) * 128], h[:, (g4 * 4 + j) * 128:(g4 * 4 + j + 1) * 128], ident.v())
            k.cp("act" if g4 else "dve", hT[:, g4 * 4:(g4 + 1) * 4, :], rr(pt.v(), "p (j t) -> p j t", j=4))
        for g4 in range(4):
            pt = pb[2 + g4 % 2]
            for j in range(4):
                c16 = g4 * 4 + j
                for dc in range(8):
                    k.mm(pt[:, j * 128:(j + 1) * 128], wq[:, dc, c16 * 128:(c16 + 1) * 128], hT[:, dc, :], start=(dc == 0), stop=(dc == 7))
            k.cp("act" if g4 % 2 else "dve", qT[:, g4 * 4:(g4 + 1) * 4, :], rr(pt.v(), "p (j t) -> p j t", j=4))
        for g4 in range(4):
            pt = pb[4 + g4 % 2]
            for j in range(4):
                c16 = g4 * 4 + j
                k.mm(pt[:, j * 128:(j + 1) * 128], qT[:, c16, :], keysT[:, c16, :])
            k.cp("act" if g4 % 2 else "dve", sc[:, g4 * 4:(g4 + 1) * 4, :], rr(pt.v(), "p (j t) -> p j t", j=4))
        for c16 in range(16):
            k.op("dve", lambda e: e.max(vals.h[:, c16, 0:8], sc.h[:, c16, :]), sc.res, vals.res)
            k.op("dve", lambda e: e.max_index(idxu.h[:, c16, 0:8], vals.h[:, c16, 0:8], sc.h[:, c16, :]), sc.res + vals.res, idxu.res)
            k.op("dve", lambda e: e.match_replace(wk.h[:], vals.h[:, c16, 0:8], sc.h[:, c16, :], -1e30), sc.res + vals.res, wk.res)
            k.op("dve", lambda e: e.max(vals.h[:, c16, 8:16], wk.h[:]), wk.res, vals.res)
            k.op("dve", lambda e: e.max_index(idxu.h[:, c16, 8:16], vals.h[:, c16, 8:16], wk.h[:]), wk.res + vals.res, idxu.res)
        k.cp("dve", idxf.v(), idxu.v())
        v4 = rr(vals.v(), "p (h q) a -> p h q a", q=2)
        i4 = rr(idxf.v(), "p (h q) a -> p h q a", q=2)
        v1 = v4[:, :, 0, :]; v2 = v4[:, :, 1, :]
        k.tt("dve", cand.v(), bc(V(v1.ap.unsqueeze(3), v1.res), [128, 8, 16, 16]), bc(V(v2.ap.unsqueeze(2), v2.res), [128, 8, 16, 16]), OP.add)
        for hh in range(8):
            cf = rr(cand[:, hh, :, :], "p a b -> p (a b)")
            k.op("dve", lambda e: e.max(best.h[:, hh, 0:8], cf.ap), cand.res, best.res)
            k.op("dve", lambda e: e.max_index(posu.h[:, hh, 0:8], best.h[:, hh, 0:8], cf.ap), cand.res + best.res, posu.res)
            k.op("dve", lambda e: e.match_replace(wk2.h[:], best.h[:, hh, 0:8], cf.ap, -1e30), cand.res + best.res, wk2.res)
            k.op("dve", lambda e: e.max(best.h[:, hh, 8:16], wk2.h[:]), wk2.res, best.res)
            k.op("dve", lambda e: e.max_index(posu.h[:, hh, 8:16], best.h[:, hh, 8:16], wk2.h[:]), wk2.res + best.res, posu.res)
        k.ts("dve", posu2[:, 0, :, :], posu.v(), cu32[:, 0:1], OP.bitwise_and)
        k.ts("dve", posu2[:, 1, :, :], posu.v(), cu32[:, 1:2], OP.logical_shift_right)
        k.cp("dve", pb_.v(), posu2[:, 0, :, :])
        k.cp("dve", pa_.v(), posu2[:, 1, :, :])
        io4 = bc(V(iota16.h[:].unsqueeze(1).unsqueeze(1), iota16.res), [128, 8, 16, 16])
        for (pp, isrc, dst) in ((pa_, i4[:, :, 0, :], eidf), (pb_, i4[:, :, 1, :], eid2)):
            k.tt("dve", oh.v(), io4, bc(V(pp.h[:].unsqueeze(3), pp.res), [128, 8, 16, 16]), OP.is_equal)
            k.tt("dve", oh.v(), oh.v(), bc(V(isrc.ap.unsqueeze(2), isrc.res), [128, 8, 16, 16]), OP.mult)
            k.red("dve", dst.v(), oh.v(), OP.add)
        k.stt("dve", eidf.v(), eidf.v(), 128.0, eid2.v(), OP.mult, OP.add)
        k.cp("dve", eidu.v(), rr(eidf.v(), "p h k -> p (h k)"))
        k.tt("dve", gate.v(), best.v(), bc(best[:, :, 0:1], [128, 8, 16]), OP.subtract)
        k.act(gate.v(), gate.v(), AF.Exp)
        k.red("dve", gs.v(), gate.v(), OP.add)
        k.recip(gs.v(), gs.v())
        k.tt("dve", gate.v(), gate.v(), bc(V(gs.h[:].unsqueeze(2), gs.res), [128, 8, 16]), OP.mult)
        k.memset("dve", actv.v(), 0.0)
        for sl in range(NSLOT):
            u_ = ug[sl % NB]
            k.dma_custom("pool", lambda e: e.indirect_dma_start(out=u_.h[:], out_offset=None, in_=peer_u.rearrange("l e d -> (l e) d"),
                         in_offset=bass.IndirectOffsetOnAxis(ap=eidu.h[:, sl:sl + 1], axis=0), element_offset=l * 16384 * D), eidu.res, u_.res)
            k.op("dve", lambda e: e.scalar_tensor_tensor(junk.h[:], u_.h[:], 1.0, h.h[:], op0=OP.mult, op1=OP.mult, accum_out=actv.h[:, sl:sl + 1]),
                 u_.res + h.res, junk.res + actv.res)
        k.tt("dve", t1.v(), actv.v(), actv.v(), OP.mult)
        k.ts("dve", t1.v(), t1.v(), 0.044715, OP.mult, 1.0, OP.add)
        k.tt("dve", t1.v(), t1.v(), actv.v(), OP.mult)
        k.act(t1.v(), t1.v(), AF.Sigmoid, scale=1.5957691216057308)
        k.tt("dve", wgt.v(), t1.v(), actv.v(), OP.mult)
        k.tt("dve", wgt.v(), wgt.v(), rr(gate.v(), "p h k -> p (h k)"), OP.mult)
        k.memset("pool", acc.v(), 0.0)
        for sl in range(NSLOT):
            v_ = vg[sl % NB]
            k.dma_custom("pool", lambda e: e.indirect_dma_start(out=v_.h[:], out_offset=None, in_=peer_v.rearrange("l e d -> (l e) d"),
                         in_offset=bass.IndirectOffsetOnAxis(ap=eidu.h[:, sl:sl + 1], axis=0), element_offset=l * 16384 * D), eidu.res, v_.res)
            k.stt("dve", acc.v(), v_.v(), wgt[:, sl:sl + 1], acc.v(), OP.mult, OP.add)
        if "peer_y" in P.debug:
            k.dma(P.dout["peer_y"][ti * 128:(ti + 1) * 128, :], acc.h[:], reads=acc.res)
        k.tt("dve", acc.v(), acc.v(), mods[(s, MOD_GF)].v(), OP.mult)
        k.stt("dve", acc.v(), x.v(), ALPHA, acc.v(), OP.mult, OP.add)
        mean, rstd, nmr = ln_stats(P, acc.v(), st)
        k.act(acc.v(), acc.v(), AF.Identity, bias=nmr, scale=rstd)
        k.tt("pool", acc.v(), acc.v(), lng.v(), OP.mult)
        k.tt("dve", acc.v(), acc.v(), lnb.v(), OP.add)
        d_ap, d_res = dst_ap(ti)
        k.dma(d_ap, acc.h[:], reads=acc.res, writes=d_res)
    k.barrier()
    es.close()


def phase_peer_route(P, l, prm, peer_w_q, peer_keys, XD, XD_res, MODD, MOD_res, RTD, RT_res, HFD, HF_res, need_ctx=True, prep=None):
    import os
    k = P.k
    nc = P.nc
    es = ExitStack()
    tile = lambda name, shape, dt=F32: T(es.enter_context(usb(nc, "t_" + name, shape, dt)), name)
    pb = P.pb
    ident = P.ident
    NSLOT = int(os.environ.get("PEER_SLOTS", 128))
    wq = tile("pe_wq", [128, 8, 2048])
    for c0 in range(0, 2048, 512):
        k.ld(wq[:, :, c0:c0 + 512], peer_w_q[l, :, c0:c0 + 512].rearrange("(c p) n -> p c n", p=128))
    keysT = tile("pe_keysT", [128, 16, 128])
    kst = tile("pe_kst", [128, 4, 128])
    for g4 in range(4):
        k.ld(kst.v(), peer_keys[l].rearrange("h p k d -> k (h p) d")[:, g4 * 4:(g4 + 1) * 4, :])
        pt = pb[g4 % 2]
        for j in range(4):
            k.tr(pt[:, j * 128:(j + 1) * 128], kst[:, j, :], ident.v())
        k.cp("act", keysT[:, g4 * 4:(g4 + 1) * 4, :], rr(pt.v(), "p (j t) -> p j t", j=4))
    mods = {}
    for s in ([0, 1] if need_ctx else [0]):
        for j in (MOD_SHF, MOD_SCF):
            mods[(s, j)] = tile("pe_mod%d_%d" % (s, j), [128, D])
            k.dma(mods[(s, j)].h[:], MODD[s, j], reads=MOD_res, writes=mods[(s, j)].res)
        k.ts("pool", mods[(s, MOD_SCF)].v(), mods[(s, MOD_SCF)].v(), 1.0, OP.add)
    rtT = tile("pe_rtT", [128, 3, 128]); hTb = tile("pe_hTb", [128, 8, 128], BF16)
    iota16 = tile("pe_iota", [128, 16]); k.ld(iota16.v(), P.din["c_iota16"])
    x = tile("pe_x", [128, D]); h = tile("pe_h", [128, D]); st = tile("pe_st", [128, 16])
    hT = tile("pe_hT", [128, 8, 128])
    qT = tile("pe_qT", [128, 16, 128])
    sc = tile("pe_s", [128, 16, 128])
    wk = tile("pe_wk", [128, 128])
    vals = tile("pe_vals", [128, 16, 16])
    idxu = tile("pe_idxu", [128, 16, 16], U32)
    idxf = tile("pe_idxf", [128, 16, 16])
    cand = tile("pe_cand", [128, 8, 16, 16])
    wk2 = tile("pe_wk2", [128, 256])
    best = tile("pe_best", [128, 8, 16])
    posu = tile("pe_posu", [128, 8, 16], U32)
    posu2 = tile("pe_posu2", [128, 2, 8, 16], U32)
    cu32 = tile("pe_cu32", [128, 2], U32); k.ld(cu32.v(), P.din["c_u32"])
    pa_ = tile("pe_pa", [128, 8, 16]); pb_ = tile("pe_pb", [128, 8, 16])
    oh = tile("pe_oh", [128, 8, 16, 16])
    eidf = tile("pe_eidf", [128, 8, 16]); eid2 = tile("pe_eid2", [128, 8, 16])
    eidu = tile("pe_eidu", [128, 128], U32)
    gate = tile("pe_gate", [128, 8, 16]); gs = tile("pe_gs", [128, 8])
    actv = tile("pe_act", [128, 128]); t1 = tile("pe_t1", [128, 128]); wgt = tile("pe_wgt", [128, 128])
    t0 = 0 if need_ctx else NCTX // 128
    NTL = int(os.environ.get("PEER_TILES", NTILE))
    pst = prep_alloc(P, tile) if prep is not None else None
    nprep = 0
    tiles = list(range(t0, min(NTILE, t0 + NTL)))
    for ti in tiles:
        s = 1 if ti < NCTX // 128 else 0
        if prep is not None:
            tgt = 128 if ti == tiles[-1] else min(128, (tiles.index(ti) + 1) * 8)
            while nprep < tgt:
                prep_chunk(P, pst, l, nprep, prep[0], prep[1], prep[2], prep[3], prep[4], [pb[7]])
                nprep += 1
        k.dma(x.h[:], XD[ti * 128:(ti + 1) * 128, :], reads=XD_res[ti], writes=x.res)
        mean, rstd, nmr = ln_stats(P, x.v(), st)
        k.act(h.v(), x.v(), AF.Identity, bias=nmr, scale=rstd)
        k.tt("dve", h.v(), h.v(), mods[(s, MOD_SCF)].v(), OP.mult)
        k.tt("pool", h.v(), h.v(), mods[(s, MOD_SHF)].v(), OP.add)
        for g4 in range(2):
            pt = pb[g4]
            for j in range(4):
                k.tr(pt[:, j * 128:(j + 1) * 128], h[:, (g4 * 4 + j) * 128:(g4 * 4 + j + 1) * 128], ident.v())
            k.cp("act" if g4 else "dve", hT[:, g4 * 4:(g4 + 1) * 4, :], rr(pt.v(), "p (j t) -> p j t", j=4))
        for g4 in range(4):
            pt = pb[2 + g4 % 2]
            for j in range(4):
                c16 = g4 * 4 + j
                for dc in range(8):
                    k.mm(pt[:, j * 128:(j + 1) * 128], wq[:, dc, c16 * 128:(c16 + 1) * 128], hT[:, dc, :], start=(dc == 0), stop=(dc == 7))
            k.cp("act" if g4 % 2 else "dve", qT[:, g4 * 4:(g4 + 1) * 4, :], rr(pt.v(), "p (j t) -> p j t", j=4))
        for g4 in range(4):
            pt = pb[4 + g4 % 2]
            for j in range(4):
                c16 = g4 * 4 + j
                k.mm(pt[:, j * 128:(j + 1) * 128], qT[:, c16, :], keysT[:, c16, :])
            k.cp("act" if g4 % 2 else "dve", sc[:, g4 * 4:(g4 + 1) * 4, :], rr(pt.v(), "p (j t) -> p j t", j=4))
        for c16 in range(16):
            k.op("dve", lambda e: e.max(vals.h[:, c16, 0:8], sc.h[:, c16, :]), sc.res, vals.res)
            k.op("dve", lambda e: e.max_index(idxu.h[:, c16, 0:8], vals.h[:, c16, 0:8], sc.h[:, c16, :]), sc.res + vals.res, idxu.res)
            k.op("dve", lambda e: e.match_replace(wk.h[:], vals.h[:, c16, 0:8], sc.h[:, c16, :], -1e30), sc.res + vals.res, wk.res)
            k.op("dve", lambda e: e.max(vals.h[:, c16, 8:16], wk.h[:]), wk.res, vals.res)
            k.op("dve", lambda e: e.max_index(idxu.h[:, c16, 8:16], vals.h[:, c16, 8:16], wk.h[:]), wk.res + vals.res, idxu.res)
        k.cp("dve", idxf.v(), idxu.v())
        v4 = rr(vals.v(), "p (h q) a -> p h q a", q=2)
        i4 = rr(idxf.v(), "p (h q) a -> p h q a", q=2)
        v1 = v4[:, :, 0, :]; v2 = v4[:, :, 1, :]
        k.tt("dve", cand.v(), bc(V(v1.ap.unsqueeze(3), v1.res), [128, 8, 16, 16]), bc(V(v2.ap.unsqueeze(2), v2.res), [128, 8, 16, 16]), OP.add)
        for hh in range(8):
            cf = rr(cand[:, hh, :, :], "p a b -> p (a b)")
            k.op("dve", lambda e: e.max(best.h[:, hh, 0:8], cf.ap), cand.res, best.res)
            k.op("dve", lambda e: e.max_index(posu.h[:, hh, 0:8], best.h[:, hh, 0:8], cf.ap), cand.res + best.res, posu.res)
            k.op("dve", lambda e: e.match_replace(wk2.h[:], best.h[:, hh, 0:8], cf.ap, -1e30), cand.res + best.res, wk2.res)
            k.op("dve", lambda e: e.max(best.h[:, hh, 8:16], wk2.h[:]), wk2.res, best.res)
            k.op("dve", lambda e: e.max_index(posu.h[:, hh, 8:16], best.h[:, hh, 8:16], wk2.h[:]), wk2.res + best.res, posu.res)
        k.ts("dve", posu2[:, 0, :, :], posu.v(), cu32[:, 0:1], OP.bitwise_and)
        k.ts("dve", posu2[:, 1, :, :], posu.v(), cu32[:, 1:2], OP.logical_shift_right)
        k.cp("dve", pb_.v(), posu2[:, 0, :, :])
        k.cp("dve", pa_.v(), posu2[:, 1, :, :])
        io4 = bc(V(iota16.h[:].unsqueeze(1).unsqueeze(1), iota16.res), [128, 8, 16, 16])
        for (pp, isrc, dst) in ((pa_, i4[:, :, 0, :], eidf), (pb_, i4[:, :, 1, :], eid2)):
            k.tt("dve", oh.v(), io4, bc(V(pp.h[:].unsqueeze(3), pp.res), [128, 8, 16, 16]), OP.is_equal)
            k.tt("dve", oh.v(), oh.v(), bc(V(isrc.ap.unsqueeze(2), isrc.res), [128, 8, 16, 16]), OP.mult)
            k.red("dve", dst.v(), oh.v(), OP.add)
        k.tt("dve", gate.v(), best.v(), bc(best[:, :, 0:1], [128, 8, 16]), OP.subtract)
        k.act(gate.v(), gate.v(), AF.Exp)
        k.red("dve", gs.v(), gate.v(), OP.add)
        k.recip(gs.v(), gs.v())
        k.tt("dve", gate.v(), gate.v(), bc(V(gs.h[:].unsqueeze(2), gs.res), [128, 8, 16]), OP.mult)
        pt = pb[6]
        k.tr(pt[:, 0:128], rr(eidf.v(), "p h k -> p (h k)"), ident.v())
        k.tr(pt[:, 128:256], rr(eid2.v(), "p h k -> p (h k)"), ident.v())
        k.tr(pt[:, 256:384], rr(gate.v(), "p h k -> p (h k)"), ident.v())
        k.cp("act", rtT.v(), rr(pt[:, 0:384], "p (c t) -> p c t", c=3))
        k.dma(RTD[:, :, ti * 128:(ti + 1) * 128].rearrange("c p t -> p c t"), rtT.h[:], reads=rtT.res, writes=RT_res)
        k.cp("pool", hTb.v(), hT.v())
        k.dma(HFD[:, :, ti * 128:(ti + 1) * 128], hTb.h[:], reads=hTb.res, writes=HF_res)
    k.barrier()
    es.close()


class PrepState:
    pass


def prep_alloc(P, tile):
    ps_ = PrepState()
    ps_.uf = [tile("pp_uf%d" % i, [128, D]) for i in range(2)]
    ps_.vf = [tile("pp_vf%d" % i, [128, D]) for i in range(2)]
    ps_.ub = [tile("pp_ub%d" % i, [128, 8, 128], BF16) for i in range(2)]
    ps_.vb = [tile("pp_vb%d" % i, [128, D], BF16) for i in range(2)]
    return ps_


def prep_chunk(P, ps_, l, i, peer_u, peer_v, UTD, VD, PW_res, banks):
    k = P.k
    u_ = ps_.uf[i % 2]; v_ = ps_.vf[i % 2]; ub_ = ps_.ub[i % 2]; vb_ = ps_.vb[i % 2]
    k.ld(u_.v(), peer_u[l, i * 128:(i + 1) * 128, :])
    k.ld(v_.v(), peer_v[l, i * 128:(i + 1) * 128, :])
    for g4 in range(2):
        pt = banks[(i * 2 + g4) % len(banks)]
        for j in range(4):
            dc = g4 * 4 + j
            k.tr(pt[:, j * 128:(j + 1) * 128], u_[:, dc * 128:(dc + 1) * 128], P.ident.v())
        k.cp("act" if g4 else "dve", ub_[:, g4 * 4:(g4 + 1) * 4, :], rr(pt.v(), "p (j e) -> p j e", j=4))
    k.dma(UTD[i], ub_.h[:], reads=ub_.res, writes=PW_res)
    k.cp("pool", vb_.v(), v_.v())
    k.dma(VD[i], vb_.h[:], reads=vb_.res, writes=PW_res)


def phase_peer_prep(P, l, peer_u, peer_v, UTD, VD, PW_res):
    k = P.k
    nc = P.nc
    es = ExitStack()
    tile = lambda name, shape, dt=F32: T(es.enter_context(usb(nc, "t_" + name, shape, dt)), name)
    ps_ = prep_alloc(P, tile)
    for i in range(128):
        prep_chunk(P, ps_, l, i, peer_u, peer_v, UTD, VD, PW_res, P.pb[0:4])
    k.barrier()
    es.close()


def phase_peer_dense(P, l, prm, UTD, VD, PW_res, RTD, RT_res, HFD, HF_res, XD, XD_res, dst_ap, MODD, MOD_res, need_ctx=True):
    import os
    k = P.k
    nc = P.nc
    es = ExitStack()
    tile = lambda name, shape, dt=F32: T(es.enter_context(usb(nc, "t_" + name, shape, dt)), name)
    pb = P.pb
    GT = 256
    mods = {}
    for s in ([0, 1] if need_ctx else [0]):
        mods[s] = tile("pd_gf%d" % s, [128, D])
        k.dma(mods[s].h[:], MODD[s, MOD_GF], reads=MOD_res, writes=mods[s].res)
    lng = tile("pd_lng", [128, D]); k.ld(lng.v(), prm["ln_ffn_g"][l])
    lnb = tile("pd_lnb", [128, D]); k.ld(lnb.v(), prm["ln_ffn_b"][l])
    iota = tile("pd_iota", [128, 128]); k.ld(iota.v(), P.din["c_iota128"])
    G = tile("pd_G", [128, GT, 128], BF16)
    rt = tile("pd_rt", [128, 3, GT])
    hfg = tile("pd_hfg", [128, 8, GT], BF16)
    qtb = tile("pd_qtb", [128, 32, 128], BF16)
    amt = tile("pd_amt", [128, 32, 128], BF16)
    amb = tile("pd_amb", [128, 32, 128], BF16)
    NBUF = 6
    utp = [tile("pd_ut%d" % i, [128, 2, 8, 128], BF16) for i in range(NBUF)]
    vp = [tile("pd_vp%d" % i, [128, 2, D], BF16) for i in range(NBUF)]
    ga = [tile("pd_ga%d" % i, [128, 2, GT]) for i in range(2)]
    GA = [tile("pd_GA%d" % i, [128, 2, GT], BF16) for i in range(2)]
    x = tile("pd_x", [128, D]); acc = tile("pd_acc", [128, D]); st = tile("pd_st", [128, 16])
    g_start = 0 if need_ctx else NCTX
    NG = int(os.environ.get("PEER_GROUPS", 99))
    NCP = int(os.environ.get("PEER_CP", 64))
    groups = list(range(g_start, NT, GT))[:NG]
    io3 = bc(V(iota.h[:].unsqueeze(1), iota.res), [128, 32, 128])
    pA = [pb[0], pb[1]]; pO = [pb[2], pb[3], pb[4], pb[5]]; pG = [pb[6], pb[7]]
    for g0 in groups:
        k.dma(rt.h[:], RTD[:, :, g0:g0 + GT].rearrange("c p t -> p c t"), reads=RT_res, writes=rt.res)
        k.dma(hfg.h[:], HFD[:, :, g0:g0 + GT], reads=HF_res, writes=hfg.res)
        for blk in range(GT // 32):
            ts_ = slice(blk * 32, blk * 32 + 32)
            B3 = lambda v: bc(V(v.ap.unsqueeze(2), v.res), [128, 32, 128])
            k.tt("dve", qtb.v(), io3, B3(rt[:, 1, ts_]), OP.is_equal)
            k.tt("dve", amt.v(), io3, B3(rt[:, 0, ts_]), OP.is_equal)
            k.tt("pool", amb.v(), amt.v(), B3(rt[:, 2, ts_]), OP.mult)
            for q4 in range(8):
                pg = pG[q4 % 2]
                for t4 in range(4):
                    tt_ = q4 * 4 + t4
                    k.mm(pg[:, t4 * 128:(t4 + 1) * 128], qtb[:, tt_, :], amb[:, tt_, :])
                k.cp("act" if q4 % 2 else "dve", G[:, blk * 32 + q4 * 4:blk * 32 + q4 * 4 + 4, :], rr(pg.v(), "p (t i) -> p t i", t=4))

        PF = NBUF - 2

        def emit_dma(cp):
            u_ = utp[cp % NBUF]; v_ = vp[cp % NBUF]
            k.dma(u_.h[:], UTD[2 * cp:2 * cp + 2].rearrange("c d k e -> d c k e"), reads=PW_res, writes=u_.res)
            k.dma(v_.h[:], VD[2 * cp:2 * cp + 2].rearrange("c e n -> e c n"), reads=PW_res, writes=v_.res)

        def emit_A(cp):
            u_ = utp[cp % NBUF]
            pa = pA[cp % 2]
            for cc in range(2):
                for dc in range(8):
                    k.mm(pa[:, cc * GT:(cc + 1) * GT], u_[:, cc, dc, :], hfg[:, dc, :], start=(dc == 0), stop=(dc == 7))

        for cp in range(min(PF + 1, NCP)):
            emit_dma(cp)
        emit_A(0)
        for cp in range(NCP):
            if cp + PF + 1 < NCP:
                emit_dma(cp + PF + 1)
            if cp + 1 < NCP:
                emit_A(cp + 1)
            pa = pA[cp % 2]; ga_ = ga[cp % 2]; GA_ = GA[cp % 2]; v_ = vp[cp % NBUF]
            k.act(rr(ga_.v(), "p c t -> p (c t)"), pa.v(), AF.Gelu_apprx_tanh)
            k.tt("dve", GA_.v(), ga_.v(), V(G.h[:, :, 2 * cp:2 * cp + 2].rearrange("j t i -> j i t"), G.res), OP.mult)
            for tt_ in range(GT // 128):
                for half in range(2):
                    for cc in range(2):
                        k.mm(pO[tt_ * 2 + half].v(), GA_[:, cc, tt_ * 128:(tt_ + 1) * 128], v_[:, cc, half * 512:(half + 1) * 512],
                             start=(cp == 0 and cc == 0), stop=(cp == NCP - 1 and cc == 1))
        for tt_ in range(GT // 128):
            ti = g0 // 128 + tt_
            s = 1 if ti < NCTX // 128 else 0
            k.dma(x.h[:], XD[ti * 128:(ti + 1) * 128, :], reads=XD_res[ti], writes=x.res)
            for half in range(2):
                k.tt("dve", acc[:, half * 512:(half + 1) * 512], pO[tt_ * 2 + half].v(), mods[s][:, half * 512:(half + 1) * 512], OP.mult)
            if "peer_y" in P.debug:
                for half in range(2):
                    k.cp("act", x[:, half * 512:(half + 1) * 512], pO[tt_ * 2 + half].v())
                k.dma(P.dout["peer_y"][ti * 128:(ti + 1) * 128, :], x.h[:], reads=x.res)
                k.dma(x.h[:], XD[ti * 128:(ti + 1) * 128, :], reads=XD_res[ti], writes=x.res)
            k.stt("dve", acc.v(), x.v(), ALPHA, acc.v(), OP.mult, OP.add)
            mean, rstd, nmr = ln_stats(P, acc.v(), st)
            k.act(acc.v(), acc.v(), AF.Identity, bias=nmr, scale=rstd)
            k.tt("pool", acc.v(), acc.v(), lng.v(), OP.mult)
            k.tt("dve", acc.v(), acc.v(), lnb.v(), OP.add)
            d_ap, d_res = dst_ap(ti)
            k.dma(d_ap, acc.h[:], reads=acc.res, writes=d_res)
    k.barrier()
    es.close()


PARAM_SHAPES = {
    "ml_cw": [2, 128, 4, 4], "ml_gb": [2, 128, 16], "ml_ng": [2, 128, 256],
    "rw_muT": [2, 128, 12, 2], "rw_wup": [2, 128, 384], "rw_aup": [2, 128, 384], "rw_gup": [2, 128, 384],
    "rw_cols": [2, 128, 3, 9], "mla_gq": [2, 128, 384], "mla_gkv": [2, 128, 256],
    "ln_mix_g": [2, 128, D], "ln_mix_b": [2, 128, D], "ln_ffn_g": [2, 128, D], "ln_ffn_b": [2, 128, D],
}
WEIGHT_SHAPES = {
    "w_mod": [2, D, 6 * D], "b_mod": [2, 6 * D], "w_in": [2, D, N_IN], "w_out": [2, D, D],
    "peer_w_q": [2, D, 2048], "peer_keys": [2, 8, 2, 128, 128], "peer_u": [2, 16384, D], "peer_v": [2, 16384, D],
    "mla_q_up": [2, 384, 576], "mla_kv_up": [2, 256, 768],
}


def build_program(debug=(), layers=(0, 1), stop_after=None, peer_mode="dense"):
    P = Prog(debug)
    setup_common(P)
    k = P.k
    nc = P.nc
    W = {n: P.inp(n, s) for n, s in WEIGHT_SHAPES.items()}
    prm = {n: P.inp(n, s) for n, s in PARAM_SHAPES.items()}
    prm["mla_q_up"] = W["mla_q_up"]; prm["mla_kv_up"] = W["mla_kv_up"]
    XD = P.scratch("XD1", [NT, D]); XD_res = [[Res("XD%d" % i)] for i in range(NTILE)]
    MODD = P.scratch("MODD", [2, 6, 128, D]); MOD_res = [Res("MODD")]
    PRW = P.scratch("PRW", [12, 128, NT]); PRW_res = [Res("PRW")]
    MIXT = P.scratch("MIXT", [8, 128, NT]); MIXT_res = [Res("MIXT")]
    OUT = P.outp("out", [NT - NCTX, D])
    P.peer_mode = peer_mode
    UTD = P.scratch("UTD", [128, 128, 8, 128], BF16); VD = P.scratch("VD", [128, 128, D], BF16); PW_res = [Res("PW")]
    RTD = P.scratch("RTD", [3, 128, NT]); RT_res = [Res("RT")]
    HFD = P.scratch("HFD", [128, 8, NT], BF16); HF_res = [Res("HF")]
    for l in layers:
        need_ctx = (l == 0)
        first = (l == layers[0])
        src = P.xin if first else XD
        src_res = [[] for _ in range(NTILE)] if first else XD_res
        phase_mod(P, l, W["w_mod"], W["b_mod"], MODD, MOD_res)
        es1 = ExitStack()
        hT = T(es1.enter_context(usb(nc, "t_hT_l%d" % l, [128, 8, NT], BF16)), "hT")
        phase_hT(P, src, src_res, MODD, MOD_res, MOD_SHA, MOD_SCA, hT)
        phase_mlstm(P, l, hT, W["w_in"], prm, MIXT, MIXT_res)
        phase_rwkv_proj(P, l, hT, W["w_in"], prm, PRW, PRW_res)
        phase_mla(P, l, hT, W["w_in"], prm, MIXT, MIXT_res, need_ctx)
        es1.close()
        phase_rwkv(P, l, prm, PRW, PRW_res, MIXT, MIXT_res)
        if stop_after == "mix":
            break
        phase_mixout(P, l, W["w_out"], prm, MIXT, MIXT_res, src, src_res, XD, XD_res, MODD, MOD_res, need_ctx)
        if stop_after == "mixout":
            break
        if l == 1:
            dst = lambda ti: (OUT[(ti - NCTX // 128) * 128:(ti - NCTX // 128 + 1) * 128, :], [])
        else:
            dst = lambda ti: (XD[ti * 128:(ti + 1) * 128, :], XD_res[ti])
        if P.peer_mode == "gather":
            phase_peer(P, l, prm, W["peer_w_q"], W["peer_keys"], W["peer_u"], W["peer_v"], XD, XD_res, dst, MODD, MOD_res, need_ctx)
        else:
            phase_peer_route(P, l, prm, W["peer_w_q"], W["peer_keys"], XD, XD_res, MODD, MOD_res, RTD, RT_res, HFD, HF_res, need_ctx,
                             prep=(W["peer_u"], W["peer_v"], UTD, VD, PW_res))
            phase_peer_dense(P, l, prm, UTD, VD, PW_res, RTD, RT_res, HFD, HF_res, XD, XD_res, dst, MODD, MOD_res, need_ctx)
    k.wait_all("sp")
    P.es.close()
    return P


def make_in_maps(inputs, n_cores=8):
    consts = host_consts()
    hp = host_params(inputs)
    maps = []
    for b in range(n_cores):
        m = dict(consts)
        m.update(hp)
        for n in WEIGHT_SHAPES:
            m[n] = np.ascontiguousarray(inputs[n])
        m["xin"] = np.ascontiguousarray(np.concatenate([inputs["ctx"][b], inputs["x"][b]], 0))
        cv = np.stack([inputs["c"][b], inputs["c_ctx"]], -1)
        m["cvec"] = np.ascontiguousarray(cv.reshape(8, 128, 2).transpose(1, 0, 2))
        maps.append(m)
    return maps


_CACHE = {}


def kernel(**inputs):
    from concourse.bass_utils import run_bass_kernel_spmd
    inputs = {k_: np.asarray(v) for k_, v in inputs.items()}
    if "P" not in _CACHE:
        _CACHE["P"] = build_program()
    P = _CACHE["P"]
    maps = make_in_maps(inputs, 8)
    maps = [{kk: v for kk, v in m.items() if kk in P.din} for m in maps]
    res = run_bass_kernel_spmd(P.nc, maps, core_ids=list(range(8)))
    out = np.stack([np.asarray(res.results[b]["out"]) for b in range(8)], 0).astype(np.float32)
    return out
```

```python
import numpy as np
import concourse.bass as bass
import concourse.mybir as mybir
from contextlib import ExitStack

F32 = mybir.dt.float32
BF16 = mybir.dt.bfloat16
U32 = mybir.dt.uint32
I32 = mybir.dt.int32
AF = mybir.ActivationFunctionType
OP = mybir.AluOpType
AX = mybir.AxisListType


class Res:
    __slots__ = ("name", "w", "r", "pe_row", "excl")

    def __init__(self, name=""):
        self.name = name
        self.pe_row = None
        self.excl = False
        self.w = None
        self.r = {}


class Sched:
    NDMA = 24
    NSW = 8

    def __init__(self, nc, es):
        self.nc = nc
        self.es = es
        self.eng = {"pe": nc.tensor, "dve": nc.vector, "act": nc.scalar,
                    "pool": nc.gpsimd, "sp": nc.sync}
        self.sem = {}
        self.cnt = {}
        for k in self.eng:
            self.sem[k] = es.enter_context(nc.semaphore("s_" + k))
            self.cnt[k] = 0
        self.dsem = [es.enter_context(nc.semaphore("d%d" % i)) for i in range(self.NDMA + self.NSW)]
        self.dcnt = [0] * (self.NDMA + self.NSW)
        self.dnext = 0
        self.swnext = 0
        self.known = {k: {} for k in self.eng}
        self.gen = {k: 0 for k in self.eng}
        self.ekey = {k: k for k in self.eng}
        self.semobj = dict(self.sem)
        for i, s in enumerate(self.dsem):
            self.semobj["d%d" % i] = s
        self.ninst = 0

    def _wait(self, e, tok):
        if tok is None:
            return
        key, val = tok
        kn = self.known[e]
        if kn.get(key, 0) >= val:
            return
        self.eng[e].wait_ge(self.semobj[key], val)
        kn[key] = val

    def _deps(self, e, reads, writes, same_ok=False):
        for r in reads:
            if r.w is not None and not (same_ok and r.w[0].split("#")[0] == e):
                self._wait(e, r.w)
            if r.excl:
                for key, val in r.r.items():
                    if key.split("#")[0] != e:
                        self._wait(e, (key, val))
        for w in writes:
            if w.w is not None and not (same_ok and w.w[0].split("#")[0] == e):
                self._wait(e, w.w)
            for key, val in w.r.items():
                if not (same_ok and key.split("#")[0] == e):
                    self._wait(e, (key, val))

    def _commit(self, tok, reads, writes):
        key, val = tok
        for r in reads:
            if r.r.get(key, 0) < val:
                r.r[key] = val
        for w in writes:
            w.w = tok
            w.r = {}

    SEM_LIMIT = 20000

    def op(self, e, fn, reads=(), writes=(), same_ok=False):
        if getattr(self, "mute", False):
            return None
        if self.cnt[e] >= self.SEM_LIMIT:
            self.gen[e] += 1
            key = "%s#%d" % (e, self.gen[e])
            self.sem[e] = self.es.enter_context(self.nc.semaphore("s_%s_%d" % (e, self.gen[e])))
            self.semobj[key] = self.sem[e]
            self.ekey[e] = key
            self.cnt[e] = 0
        self._deps(e, reads, writes, same_ok)
        ins = fn(self.eng[e])
        self.cnt[e] += 1
        ins.then_inc(self.sem[e], 1)
        self._commit((self.ekey[e], self.cnt[e]), reads, writes)
        self.ninst += 1
        return ins

    def dma(self, out, in_, reads=(), writes=(), q="sp", **kw):
        s = self.dnext
        self.dnext = (self.dnext + 1) % self.NDMA
        key = "d%d" % s
        if self.dcnt[s] > 0:
            self._wait(q, (key, self.dcnt[s]))
        self._deps(q, reads, writes)
        ins = self.eng[q].dma_start(out=out, in_=in_, **kw)
        self.dcnt[s] += 16
        ins.then_inc(self.dsem[s], 16)
        self._commit((key, self.dcnt[s]), reads, writes)
        self.ninst += 1
        return (key, self.dcnt[s])

    def _dma_token(self, ins, q, reads, writes):
        s = self.dnext
        self.dnext = (self.dnext + 1) % self.NDMA
        raise RuntimeError("use dma_custom")

    def dma_custom(self, q, build, reads=(), writes=()):
        if getattr(self, "mute", False):
            return None
        s = self.NDMA + self.swnext
        self.swnext = (self.swnext + 1) % self.NSW
        key = "d%d" % s
        if self.dcnt[s] > 0:
            self._wait(q, (key, self.dcnt[s]))
        self._deps(q, reads, writes)
        ins = build(self.eng[q])
        self.dcnt[s] += 16
        ins.then_inc(self.dsem[s], 16)
        self._commit((key, self.dcnt[s]), reads, writes)
        self.ninst += 1
        return (key, self.dcnt[s])

    def wait_all(self, e="sp"):
        for s in range(self.NDMA + self.NSW):
            if self.dcnt[s] > 0:
                self._wait(e, ("d%d" % s, self.dcnt[s]))
        for k in self.eng:
            if self.cnt[k] > 0:
                self._wait(e, (self.ekey[k], self.cnt[k]))


class V:
    __slots__ = ("ap", "res")

    def __init__(self, ap, res):
        self.ap = ap
        self.res = res

    def __getitem__(self, idx):
        return V(self.ap[idx], self.res)


class T:
    def __init__(self, h, name):
        self.h = h
        self.res = [Res(name)]

    def __getitem__(self, idx):
        return V(self.h[idx], self.res)

    def v(self):
        return V(self.h[:], self.res)


def _rs(*vs):
    out = []
    for v in vs:
        if isinstance(v, V):
            out.extend(v.res)
    return out


def _ap(v):
    return v.ap if isinstance(v, V) else v


class K(Sched):
    def sb(self, name, shape, dt=F32):
        return T(self.es.enter_context(self.nc.sbuf_tensor(name, shape, dt)), name)

    def ps(self, name, shape, dt=F32):
        t = T(self.es.enter_context(self.nc.psum_tensor(name, shape, dt)), name)
        t.res[0].excl = True
        return t

    def tt(self, e, out, in0, in1, op):
        return self.op(e, lambda g: g.tensor_tensor(_ap(out), _ap(in0), _ap(in1), op), _rs(in0, in1), _rs(out))

    def ts(self, e, out, in0, s1, op0, s2=None, op1=None, accum=None):
        def f(g):
            kw = {}
            if op1 is not None:
                kw["op1"] = op1
            if accum is not None:
                kw["accum_out"] = _ap(accum)
            return g.tensor_scalar(_ap(out), _ap(in0), _ap(s1), _ap(s2), op0=op0, **kw)
        return self.op(e, f, _rs(in0, s1, s2), _rs(out, accum))

    def stt(self, e, out, in0, s, in1, op0, op1):
        return self.op(e, lambda g: g.scalar_tensor_tensor(_ap(out), _ap(in0), _ap(s), _ap(in1), op0=op0, op1=op1),
                       _rs(in0, s, in1), _rs(out))

    def cp(self, e, out, in_):
        if e == "act":
            return self.op(e, lambda g: g.copy(_ap(out), _ap(in_)), _rs(in_), _rs(out))
        return self.op(e, lambda g: g.tensor_copy(_ap(out), _ap(in_)), _rs(in_), _rs(out))

    def act(self, out, in_, func, bias=0.0, scale=1.0, accum=None):
        def f(g):
            kw = {}
            if accum is not None:
                kw["accum_out"] = _ap(accum)
            return g.activation(_ap(out), _ap(in_), func, bias=_ap(bias), scale=_ap(scale), **kw)
        return self.op("act", f, _rs(in_, bias, scale), _rs(out, accum))

    def red(self, e, out, in_, op, axis=AX.X):
        return self.op(e, lambda g: g.tensor_reduce(_ap(out), _ap(in_), axis, op), _rs(in_), _rs(out))

    def recip(self, out, in_):
        return self.op("dve", lambda g: g.reciprocal(_ap(out), _ap(in_)), _rs(in_), _rs(out))

    def memset(self, e, out, val):
        return self.op(e, lambda g: g.memset(_ap(out), val), [], _rs(out))

    def mm(self, out, lhsT, rhs, start=True, stop=True):
        if getattr(self, "mute", False):
            return None
        la = _ap(lhsT)
        key = (la.base_partition(), la.partition_size())
        for r in out.res:
            prev = getattr(r, "pe_row", None)
            if prev is not None and prev != key and r.w is not None and r.w[0].split("#")[0] == "pe":
                self._wait("pe", r.w)
        ins = self.op("pe", lambda g: g.matmul(_ap(out), la, _ap(rhs), start=start, stop=stop),
                      _rs(lhsT, rhs), _rs(out), same_ok=True)
        for r in out.res:
            r.pe_row = key
        return ins

    def tr(self, out, in_, ident):
        return self.op("pe", lambda g: g.transpose(_ap(out), _ap(in_), _ap(ident)), _rs(in_, ident), _rs(out),
                       same_ok=True)

    def ld(self, out, in_ap, q="sp", **kw):
        return self.dma(_ap(out), in_ap, reads=[], writes=_rs(out), q=q, **kw)

    def st(self, out_ap, in_, q="sp", dram_res=None, **kw):
        return self.dma(out_ap, _ap(in_), reads=_rs(in_), writes=(dram_res or []), q=q, **kw)

    def barrier(self):
        for e in self.eng:
            self.wait_all(e)


import itertools
_ctr = itertools.count()


def usb(nc, name, shape, dt):
    return nc.sbuf_tensor("%s_u%d" % (name, next(_ctr)), shape, dt)


NT = 2304
NCTX = 256
D = 1024
N_IN = 3248
NTILE = NT // 128
NCH = NT // 64
ALPHA = 4 ** 0.25
LN_EPS = 1e-6
MOD_SHA, MOD_SCA, MOD_GA, MOD_SHF, MOD_SCF, MOD_GF = range(6)


def host_consts():
    c = {}
    c["ident"] = np.eye(128, dtype=np.float32)
    s = np.arange(64)[:, None]; t = np.arange(64)[None, :]
    triF = (s <= t).astype(np.float32); triR = (s >= t).astype(np.float32)
    c["c_tri"] = np.concatenate([triF, triR], 0)
    mF = np.where(t <= s, 0.0, -1e30).astype(np.float32)
    mR = np.where(t >= s, 0.0, -1e30).astype(np.float32)
    c["c_maskD"] = np.concatenate([mF, mR], 0)
    c["c_I2"] = np.concatenate([np.eye(64, dtype=np.float32)] * 2, 0)
    tt_ = np.arange(2048)
    inv = (10000.0 ** (-np.arange(8, dtype=np.float32) / 8)).astype(np.float32)
    ang = np.stack([(tt_ // 64)[:, None].astype(np.float32) * inv, (tt_ % 64)[:, None].astype(np.float32) * inv], 1)
    c["c_rope"] = np.concatenate([np.cos(ang).reshape(2048, 16), np.sin(ang).reshape(2048, 16)], 1).astype(np.float32)
    c["c_u32"] = np.ascontiguousarray(np.broadcast_to(np.array([15, 4], np.uint32)[None, :], (128, 2)))
    c["c_iota128"] = np.ascontiguousarray(np.broadcast_to(np.arange(128, dtype=np.float32)[None, :], (128, 128)))
    c["c_iota16"] = np.ascontiguousarray(np.broadcast_to(np.arange(16, dtype=np.float32)[None, :], (128, 16)))
    bd = np.zeros((128, 128), np.float32); bd[:64, :64] = 1; bd[64:, 64:] = 1
    c["c_BD2"] = bd
    r_ = np.arange(64)[:, None]; c_ = np.arange(64)[None, :]
    su = (r_ < c_).astype(np.float32); iu = (r_ <= c_).astype(np.float32)
    sl = (r_ > c_).astype(np.float32); il = (r_ >= c_).astype(np.float32)
    mf = np.concatenate([su, iu, su, iu, sl], 1); mr = np.concatenate([sl, il, sl, il, su], 1)
    m2 = np.stack([mf, mr], 0)
    c["c_rwmask"] = np.ascontiguousarray(np.concatenate([m2, m2], 1).transpose(1, 0, 2))
    return c


def host_params(inp):
    L = 2
    p = {}
    cw = np.concatenate([inp["ml_conv_w"], inp["ml_conv_b"][:, None, :]], 1)
    p["ml_cw"] = np.ascontiguousarray(cw.reshape(L, 4, 4, 128).transpose(0, 3, 2, 1))
    gb = np.zeros((L, 16), np.float32)
    for d in range(2):
        gb[:, d * 8:d * 8 + 4] = inp["ml_i_bias"][:, d]
        gb[:, d * 8 + 4:d * 8 + 8] = inp["ml_f_bias"][:, d]
    p["ml_gb"] = np.ascontiguousarray(np.broadcast_to(gb[:, None, :], (L, 128, 16)))
    p["ml_ng"] = np.ascontiguousarray(np.broadcast_to(inp["ml_norm_g"][:, None, :], (L, 128, 256)))
    p["mla_gq"] = np.ascontiguousarray(np.broadcast_to(inp["mla_q_norm_g"][:, None, :], (L, 128, 384)))
    p["mla_gkv"] = np.ascontiguousarray(np.broadcast_to(inp["mla_kv_norm_g"][:, None, :], (L, 128, 256)))
    for nm in ("ln_mix_g", "ln_mix_b", "ln_ffn_g", "ln_ffn_b"):
        p[nm] = np.ascontiguousarray(np.broadcast_to(inp[nm][:, None, :], (L, 128, 1024)))
    p["rw_muT"] = np.ascontiguousarray(inp["rw_mu"].reshape(L, 2, 12, 128).transpose(0, 3, 2, 1))
    p["rw_wup"] = np.ascontiguousarray(inp["rw_w_up"].reshape(L, 128, 384))
    p["rw_aup"] = np.ascontiguousarray(inp["rw_a_up"].reshape(L, 128, 384))
    p["rw_gup"] = np.ascontiguousarray(inp["rw_g_up"])
    cols = np.stack([inp["rw_k_k"], inp["rw_k_a"], inp["rw_r_k"], inp["rw_gn_g"], inp["rw_gn_b"],
                     inp["rw_w0"][:, 0], inp["rw_w0"][:, 1], inp["rw_a0"][:, 0], inp["rw_a0"][:, 1]], -1)
    p["rw_cols"] = np.ascontiguousarray(cols.reshape(L, 3, 128, 9).transpose(0, 2, 1, 3))
    return p


class Prog:
    def __init__(self, debug=()):
        self.debug = set(debug)
        self.nc = nc = bass.Bass("TRN2", target_bir_lowering=False)
        self.es = ExitStack()
        self.k = K(nc, self.es)
        self.din = {}
        self.dout = {}

    def inp(self, name, shape, dt=F32):
        self.din[name] = self.nc.dram_tensor(name, list(shape), dt, kind="ExternalInput").ap()
        return self.din[name]

    def outp(self, name, shape, dt=F32):
        self.dout[name] = self.nc.dram_tensor(name, list(shape), dt, kind="ExternalOutput").ap()
        return self.dout[name]

    def dump(self, name, view):
        ap = view.ap
        o = self.outp("o_" + name, list(ap.shape), ap.dtype)
        self.k.dma(o, ap, reads=view.res)

    def scratch(self, name, shape, dt=F32):
        kind = "ExternalOutput" if name in self.debug else "Internal"
        ap = self.nc.dram_tensor(name, list(shape), dt, kind=kind).ap()
        if kind == "ExternalOutput":
            self.dout[name] = ap
        return ap


def setup_common(P):
    k = P.k
    P.ident_d = P.inp("ident", [128, 128])
    P.ident = k.sb("ident_sb", [128, 128])
    k.ld(P.ident.v(), P.ident_d)
    P.identb = k.sb("identb", [128, 128], BF16)
    k.cp("dve", P.identb.v(), P.ident.v())
    P.ones = k.sb("ones", [128, 128])
    k.memset("dve", P.ones.v(), 1.0)
    P.pb = [k.ps("pb%d" % i, [128, 512]) for i in range(8)]
    P.bnst = k.sb("bnst", [128, 12])
    P.wstage = [k.sb("wstage%d" % i, [128, 8, 256]) for i in range(2)]
    P.wsi = 0
    for nm, shp in (("c_tri", [128, 64]), ("c_maskD", [128, 64]), ("c_I2", [128, 64]), ("c_BD2", [128, 128]), ("c_rwmask", [128, 2, 320]), ("c_rope", [2048, 32]), ("c_iota16", [128, 16]), ("c_iota128", [128, 128]), ("c_u32", [128, 2, "u32"])):
        if shp[-1] == "u32":
            P.inp(nm, shp[:-1], U32)
        else:
            P.inp(nm, shp)
    P.xin = P.inp("xin", [NT, D])
    P.cvec = P.inp("cvec", [128, 8, 2])
    P.XD = P.scratch("XD", [NT, D])
    P.XD_res = [Res("XD")]


def phase_mod(P, l, w_mod, b_mod, MODD, MOD_res):
    k = P.k
    nc = P.nc
    es = ExitStack()
    tile = lambda name, shape, dt=F32: T(es.enter_context(usb(nc, "t_" + name, shape, dt)), name)
    csilu = tile("csilu", [128, 8, 2])
    cv = tile("cvec_sb", [128, 8, 2])
    k.ld(cv.v(), P.cvec)
    k.act(csilu.v(), cv.v(), AF.Silu)
    clhs = [tile("clhs%d" % s_, [128, 8, 128]) for s_ in range(2)]
    for s_ in range(2):
        for kc in range(8):
            k.cp("dve", clhs[s_][:, kc, :], V(csilu.h[:, kc, s_:s_ + 1].to_broadcast([128, 128]), csilu.res))
    wbuf = [tile("wmodbuf%d" % i, [128, 8, 512]) for i in range(2)]
    ob = [tile("modout%d" % i, [128, 512]) for i in range(4)]
    bb = tile("bmodrow", [1, 6 * D])
    k.ld(bb.v(), b_mod[l:l + 1, :])
    for n in range(12):
        wb = wbuf[n % 2]
        k.ld(wb.v(), w_mod[l, :, n * 512:(n + 1) * 512].rearrange("(c p) n -> p c n", p=128))
        for s_ in range(2):
            pt = P.pb[(n * 2 + s_) % 4]
            o_ = ob[(n * 2 + s_) % 4]
            for kc in range(8):
                k.mm(pt.v(), clhs[s_][:, kc, :], wb[:, kc, :], start=(kc == 0), stop=False)
            k.mm(pt.v(), P.ones[0:1, :], bb[0:1, n * 512:(n + 1) * 512], start=False, stop=True)
            k.cp("act" if s_ else "dve", o_.v(), pt.v())
            k.dma(MODD[s_, n // 2, :, (n % 2) * 512:(n % 2 + 1) * 512], o_.h[:], reads=o_.res, writes=MOD_res)
    k.barrier()
    es.close()


def ln_stats(P, xt, st, nfeat=D):
    k = P.k
    nchunk = nfeat // 512
    bs = P.bnst
    for c in range(nchunk):
        k.op("dve", lambda g: g.bn_stats(bs.h[:, c * 6:(c + 1) * 6], xt.ap[:, c * 512:(c + 1) * 512]), xt.res, bs.res)
    k.op("dve", lambda g: g.bn_aggr(st.h[:, 0:2], bs.h[:, 0:6 * nchunk]), bs.res, st.res)
    k.ts("dve", st[:, 2:3], st[:, 1:2], LN_EPS, OP.add)
    k.act(st[:, 3:4], st[:, 2:3], AF.Sqrt)
    k.recip(st[:, 4:5], st[:, 3:4])
    k.stt("dve", st[:, 5:6], st[:, 0:1], -1.0, st[:, 4:5], OP.mult, OP.mult)
    return st[:, 0:1], st[:, 4:5], st[:, 5:6]


def phase_hT(P, src_ap, src_res, MODD, MOD_res, shj, scj, hT):
    k = P.k
    nc = P.nc
    es = ExitStack()
    xt = [T(es.enter_context(usb(nc, "ph_x%d" % i, [128, D], F32)), "ph_x%d" % i) for i in range(2)]
    hh = [T(es.enter_context(usb(nc, "ph_h%d" % i, [128, D], F32)), "ph_h%d" % i) for i in range(2)]
    st = [T(es.enter_context(usb(nc, "ph_st%d" % i, [128, 16], F32)), "ph_st%d" % i) for i in range(2)]
    sc1 = [T(es.enter_context(usb(nc, "ph_sc1_%d" % s, [128, D], F32)), "ph_sc1_%d" % s) for s in range(2)]
    sh1 = [T(es.enter_context(usb(nc, "ph_sh1_%d" % s, [128, D], F32)), "ph_sh1_%d" % s) for s in range(2)]
    for s in range(2):
        k.dma(sc1[s].h[:], MODD[s, scj], reads=MOD_res, writes=sc1[s].res)
        k.dma(sh1[s].h[:], MODD[s, shj], reads=MOD_res, writes=sh1[s].res)
        k.ts("pool", sc1[s].v(), sc1[s].v(), 1.0, OP.add)
    for ti in range(NTILE):
        s = 1 if ti < NCTX // 128 else 0
        x = xt[ti % 2]; h = hh[ti % 2]; stt_ = st[ti % 2]
        k.dma(x.h[:], src_ap[ti * 128:(ti + 1) * 128, :], reads=src_res[ti], writes=x.res)
        mean, rstd, nmr = ln_stats(P, x.v(), stt_)
        k.act(h.v(), x.v(), AF.Identity, bias=nmr, scale=rstd)
        k.tt("dve", h.v(), h.v(), sc1[s].v(), OP.mult)
        k.tt("pool", h.v(), h.v(), sh1[s].v(), OP.add)
        for g4 in range(2):
            pt = P.pb[4 + (ti * 2 + g4) % 2]
            for j in range(4):
                dc = g4 * 4 + j
                k.tr(pt[:, j * 128:(j + 1) * 128], h[:, dc * 128:(dc + 1) * 128], P.ident.v())
            k.cp("act", hT[:, g4 * 4:(g4 + 1) * 4, ti * 128:(ti + 1) * 128],
                 V(pt.h[:].rearrange("p (j t) -> p j t", j=4), pt.res))
    k.barrier()
    es.close()


def bc(view, shape):
    return V(view.ap.to_broadcast(shape), view.res)


def rr(view, pat, **kw):
    return V(view.ap.rearrange(pat, **kw), view.res)


def load_w_bf16(P, dst, w_ap, ncols, c_off=0):
    k = P.k
    for c0 in range(0, ncols, 256):
        n = min(256, ncols - c0)
        stg = P.wstage[P.wsi % 2]
        P.wsi += 1
        k.ld(stg[:, :, 0:n], w_ap[:, c0:c0 + n].rearrange("(c p) n -> p c n", p=128))
        k.cp("pool" if (P.wsi % 2) else "dve", dst[:, :, c_off + c0:c_off + c0 + n], stg[:, :, 0:n])


ORD_F = list(range(NCH))
ORD_R = [3, 2, 1, 0] + list(range(NCH - 1, 3, -1))


def phase_mlstm(P, l, hT, w_in, prm, MIXD, MIX_res):
    k = P.k
    nc = P.nc
    es = ExitStack()
    tile = lambda name, shape, dt=F32: T(es.enter_context(usb(nc, "t_" + name, shape, dt)), name)
    ident = P.ident
    wml = tile("wml", [128, 8, 1040], BF16)
    load_w_bf16(P, wml, w_in[l, :, 0:1040], 1040)
    qkT = tile("qkT", [128, 4, NT])
    raw = tile("mlraw", [128, NT])
    cw = tile("mlcw", [128, 4, 4]); k.ld(cw.v(), prm["ml_cw"][l])
    gb = tile("mlgb", [128, 16]); k.ld(gb.v(), prm["ml_gb"][l])
    ng = tile("mlng", [128, 256]); k.ld(ng.v(), prm["ml_ng"][l])
    tri = tile("mltri", [128, 64]); k.ld(tri.v(), P.din["c_tri"])
    maskD = tile("mlmask", [128, 64]); k.ld(maskD.v(), P.din["c_maskD"])
    I2 = tile("mlI2", [128, 64]); k.ld(I2.v(), P.din["c_I2"])
    pb = P.pb
    ib = 0
    for fc in range(4):
        for tb in range(0, NT, 512):
            n = min(512, NT - tb)
            pt = pb[ib % 2]; ib += 1
            for dc in range(8):
                k.mm(pt[:, 0:n], wml[:, dc, fc * 128:(fc + 1) * 128], hT[:, dc, tb:tb + n], start=(dc == 0), stop=(dc == 7))
            k.cp("act", raw[:, tb:tb + n], pt[:, 0:n])
        dst = qkT[:, fc, :]
        k.act(dst, raw.v(), AF.Identity, bias=cw[:, fc, 3:4], scale=cw[:, fc, 1:2])
        for (s0, s1) in ((0, NCTX), (NCTX, NT)):
            k.stt("dve", qkT[:, fc, s0 + 1:s1], raw[:, s0:s1 - 1], cw[:, fc, 0:1], qkT[:, fc, s0 + 1:s1], OP.mult, OP.add)
            k.stt("dve", qkT[:, fc, s0:s1 - 1], raw[:, s0 + 1:s1], cw[:, fc, 2:3], qkT[:, fc, s0:s1 - 1], OP.mult, OP.add)
        k.act(dst, dst, AF.Silu)
        if fc < 2:
            k.ts("dve", dst, dst, 0.125, OP.mult)
    if "ml_qkT" in P.debug:
        P.dump("ml_qkT", qkT.v())
    import os
    if os.environ.get("MLSTOP") == "1":
        k.barrier(); es.close(); return
    NSTEP = int(os.environ.get("MLSTEPS", NCH))
    CUT = int(os.environ.get("MLCUT", 99))
    def sec(n):
        k.mute = CUT < n
    CT = tile("mlCT", [128, 2, 2, 65]); k.memset("dve", CT.v(), 0.0)
    mfull = tile("mlm", [128, 8]); k.memset("dve", mfull.v(), -1e30)
    hsum = tile("mlhsum", [128, NTILE, 256]); k.memset("pool", hsum.v(), 0.0)
    vS = tile("mlvS", [128, 4, 65]); k.memset("dve", vS.v(), 1.0)
    kS = tile("mlkS", [128, 4, 64])
    gS = tile("mlgS", [128, 8])
    cs = tile("mlcs", [128, 12])
    lmb = tile("mllmb", [128, 4])
    D8 = tile("mlD8", [128, 4, 64])
    rmax = tile("mlrmax", [128, 8])
    dm = tile("mldm", [128, 4, 64])
    sm = tile("mlsmall", [128, 64])
    Sm = tile("mlSm", [128, 4, 64])
    ST = tile("mlST", [128, 4, 64])
    tI = tile("mltI", [128, 4, 65])
    nd = tile("mlnd", [128, 4, 65])
    hout = tile("mlhout", [128, 4, 64])
    kw = tile("mlkw", [128, 4, 64])
    mmax = tile("mlmmax", [128, 8])
    dec = tile("mldec", [128, 8])
    c4 = lambda i: sm[:, i * 4:(i + 1) * 4]
    mx, a1, mt, tmp, wint, emt, dabs, rden, wk, mmA = [c4(i) for i in range(10)]
    pA, pR, pS, pST, pN, pI, pU, pX = pb
    B4 = lambda v: bc(V(v.ap.unsqueeze(2), v.res), [128, 4, 64])
    for step in range(NSTEP):
        chunks = (ORD_F[step], ORD_R[step])
        sec(1)
        for dr in range(2):
            c = chunks[dr]
            H = slice(dr * 64, dr * 64 + 64)
            tok = slice(c * 64, c * 64 + 64)
            for dc in range(8):
                k.mm(pX[H, 0:256], hT[:, dc, tok], wml[:, dc, 512:768], start=(dc == 0), stop=(dc == 7))
            for dc in range(8):
                k.mm(pA[H, 16:32], hT[:, dc, tok], wml[:, dc, 1024:1040], start=(dc == 0), stop=(dc == 7))
            for hh in range(2):
                k.mm(pX[H, 256 + hh * 128:256 + (hh + 1) * 128], qkT[:, 2 + hh, tok], ident.v())
            k.tt("dve", gS[H, :], pA[H, 16 + dr * 8:16 + dr * 8 + 8], gb[H, dr * 8:dr * 8 + 8], OP.add)
        k.cp("act", vS[:, :, 0:64], rr(pX[:, 0:256], "p (h e) -> p h e", h=4))
        k.cp("dve", kS.v(), rr(pX[:, 256:512], "p (h e) -> p h e", h=4))
        k.act(gS[:, 4:8], gS[:, 4:8], AF.Exp, scale=-1.0)
        k.act(gS[:, 4:8], gS[:, 4:8], AF.Ln, bias=1.0)
        k.ts("dve", gS[:, 4:8], gS[:, 4:8], -1.0, OP.mult)
        sec(2)
        for dr in range(2):
            H = slice(dr * 64, dr * 64 + 64)
            k.mm(pA[H, 0:4], tri[H, :], gS[H, 4:8])
            k.mm(pA[:, 4 + dr * 4:8 + dr * 4], P.ones[H, :], gS[H, 4:8])
        k.cp("dve", cs.v(), pA[:, 0:12])
        k.tt("dve", lmb.v(), gS[:, 0:4], cs[:, 0:4], OP.subtract)
        k.tt("dve", D8.v(), bc(V(I2.h[:].unsqueeze(1), I2.res), [128, 4, 64]), B4(lmb.v()), OP.mult)
        for dr in range(2):
            H = slice(dr * 64, dr * 64 + 64)
            k.mm(rr(pR[:, dr * 256:(dr + 1) * 256], "p (h s) -> p h s", h=4), P.ones[H, :], D8[H, :, :])
        k.red("dve", rmax.v(), rr(pR.v(), "p (u s) -> p u s", u=8), OP.max)
        for dr in range(2):
            H = slice(dr * 64, dr * 64 + 64)
            k.tt("dve", dm[H, :, :], rr(pR[H, dr * 256:(dr + 1) * 256], "p (h s) -> p h s", h=4),
                 bc(V(maskD.h[H, :].unsqueeze(1), maskD.res), [64, 4, 64]), OP.add)
            k.tt("dve", a1[H, :] if False else V(sm.h[H, 4:8], sm.res), cs[H, 0:4], mfull[H, dr * 4:dr * 4 + 4], OP.add)
        k.tt("dve", dm.v(), dm.v(), B4(cs[:, 0:4]), OP.add)
        k.red("dve", mx, dm.v(), OP.max)
        k.tt("dve", mt, mx, a1, OP.max)
        k.tt("dve", dm.v(), dm.v(), B4(mt), OP.subtract)
        k.act(dm.v(), dm.v(), AF.Exp)
        k.tt("dve", tmp, a1, mt, OP.subtract)
        k.act(wint, tmp, AF.Exp)
        k.act(emt, mt, AF.Exp, scale=-1.0)
        sec(3)
        for dr in range(2):
            c = chunks[dr]
            H = slice(dr * 64, dr * 64 + 64)
            tok = slice(c * 64, c * 64 + 64)
            for h in range(4):
                hp = slice((h % 2) * 64, (h % 2) * 64 + 64)
                k.mm(pS[H, h * 64:(h + 1) * 64], qkT[hp, h // 2, tok], qkT[hp, 2 + h // 2, tok])
        k.tt("dve", Sm.v(), rr(pS[:, 0:256], "p (h s) -> p h s", h=4), dm.v(), OP.mult)
        sec(4)
        for dr in range(2):
            H = slice(dr * 64, dr * 64 + 64)
            for h in range(4):
                k.mm(pST[H, h * 64:(h + 1) * 64], Sm[H, h, :], ident[H, H])
        k.cp("act", ST.v(), rr(pST[:, 0:256], "p (h s) -> p h s", h=4))
        for dr in range(2):
            c = chunks[dr]
            H = slice(dr * 64, dr * 64 + 64)
            tok = slice(c * 64, c * 64 + 64)
            for h in range(4):
                hp = slice((h % 2) * 64, (h % 2) * 64 + 64)
                k.mm(pN[H, h * 65:(h + 1) * 65], ST[H, h, :], vS[H, h, :])
                k.mm(pI[H, h * 65:(h + 1) * 65], qkT[hp, h // 2, tok], CT[hp, dr, h // 2, :])
        sec(5)
        B65 = lambda v: bc(V(v.ap.unsqueeze(2), v.res), [128, 4, 65])
        k.tt("dve", tI.v(), rr(pI[:, 0:260], "p (h e) -> p h e", h=4), B65(wint), OP.mult)
        k.tt("dve", nd.v(), tI.v(), rr(pN[:, 0:260], "p (h e) -> p h e", h=4), OP.add)
        k.act(dabs, nd[:, :, 64], AF.Abs)
        k.tt("dve", dabs, dabs, emt, OP.max)
        k.recip(rden, dabs)
        k.tt("dve", hout.v(), nd[:, :, 0:64], B4(rden), OP.mult)
        sec(6)
        for dr in range(2):
            c = chunks[dr]
            H = slice(dr * 64, dr * 64 + 64)
            Hc = slice((c % 2) * 64, (c % 2) * 64 + 64)
            k.mm(pS[Hc, 256:512], ident[H, H], rr(hout[H, :, :], "p h e -> p (h e)"))
            k.tt("pool" if False else "dve", hsum[Hc, c // 2, :], hsum[Hc, c // 2, :], pS[Hc, 256:512], OP.add)
        sec(7)
        k.tt("dve", mmax.v(), mfull.v(), rmax.v(), OP.max)
        k.tt("dve", dec.v(), mfull.v(), mmax.v(), OP.subtract)
        k.act(dec.v(), dec.v(), AF.Exp)
        for dr in range(2):
            H = slice(dr * 64, dr * 64 + 64)
            k.tt("dve", V(sm.h[H, 36:40], sm.res), lmb[H, :], mmax[H, dr * 4:dr * 4 + 4], OP.subtract)
        k.act(wk, mmA, AF.Exp)
        k.tt("dve", kw.v(), kS.v(), B4(wk), OP.mult)
        for dr in range(2):
            H = slice(dr * 64, dr * 64 + 64)
            for h in range(4):
                hp = slice((h % 2) * 64, (h % 2) * 64 + 64)
                o0 = (dr * 2 + h // 2) * 65
                k.mm(pU[hp, o0:o0 + 65], kw[H, h, :], vS[H, h, :])
        for hpi in range(2):
            hp = slice(hpi * 64, hpi * 64 + 64)
            dview = V(dec.h[hp, :].rearrange("p (d hh hp) -> p d hh hp", d=2, hh=2, hp=2)[:, :, :, hpi].unsqueeze(3).to_broadcast([64, 2, 2, 65]), dec.res)
            k.tt("dve", CT[hp, :, :, :], CT[hp, :, :, :], dview, OP.mult)
        k.tt("dve", CT.v(), CT.v(), rr(pU[:, 0:260], "p (d hh e) -> p d hh e", d=2, hh=2), OP.add)
        k.tt("dve", mfull.v(), cs[:, 4:12], mmax.v(), OP.add)
    k.mute = False
    if "ml_hsum" in P.debug:
        P.dump("ml_hsum", hsum.v())
    if os.environ.get("MLSTOP") == "2":
        k.barrier(); es.close(); return
    osig = tile("mlosig", [128, NTILE, 256])
    for ti in range(NTILE):
        pt = pb[ti % 2]
        for dc in range(8):
            k.mm(pt[:, 0:256], hT[:, dc, ti * 128:(ti + 1) * 128], wml[:, dc, 768:1024], start=(dc == 0), stop=(dc == 7))
        k.act(osig[:, ti, :], pt[:, 0:256], AF.Sigmoid)
    NG = NTILE * 4
    h3 = rr(hsum.v(), "p t (h e) -> p (t h) e", h=4)
    st = tile("mlst", [128, 4, NG])
    sq = tile("mlsq", [128, NG, 64])
    BG = lambda v: bc(V(v.ap.unsqueeze(2), v.res), [128, NG, 64])
    k.red("dve", st[:, 0, :], h3, OP.add)
    k.ts("dve", st[:, 0, :], st[:, 0, :], 1.0 / 64, OP.mult)
    k.tt("dve", h3, h3, BG(st[:, 0, :]), OP.subtract)
    k.tt("pool", sq.v(), h3, h3, OP.mult)
    k.red("dve", st[:, 1, :], sq.v(), OP.add)
    k.ts("dve", st[:, 1, :], st[:, 1, :], 1.0 / 64, OP.mult, LN_EPS, OP.add)
    k.act(st[:, 2, :], st[:, 1, :], AF.Sqrt)
    k.recip(st[:, 3, :], st[:, 2, :])
    k.tt("dve", h3, h3, BG(st[:, 3, :]), OP.mult)
    k.tt("pool", hsum.v(), hsum.v(), bc(V(ng.h[:].unsqueeze(1), ng.res), [128, NTILE, 256]), OP.mult)
    k.tt("dve", hsum.v(), hsum.v(), osig.v(), OP.mult)
    oT = [tile("mloT%d" % i, [128, 2, 128]) for i in range(2)]
    for ti in range(NTILE):
        pt = pb[ti % 2]
        for c in range(2):
            k.mm(pt[:, c * 128:(c + 1) * 128], hsum[:, ti, c * 128:(c + 1) * 128], ident.v())
        o_ = oT[ti % 2]
        k.cp("act", o_.v(), rr(pt[:, 0:256], "p (c t) -> p c t", c=2))
        k.dma(MIXD[0:2, :, ti * 128:(ti + 1) * 128].rearrange("c p t -> p c t"), o_.h[:], reads=o_.res, writes=MIX_res)
    k.barrier()
    es.close()


RW_C = 0.6065306597126334


def phase_rwkv_proj(P, l, hT, w_in, prm, PRW, PRW_res):
    k = P.k
    nc = P.nc
    es = ExitStack()
    tile = lambda name, shape, dt=F32: T(es.enter_context(usb(nc, "t_" + name, shape, dt)), name)
    wrw = tile("wrw", [128, 8, 1536], BF16)
    load_w_bf16(P, wrw, w_in[l, :, 1040:2576], 1536)
    mu = tile("rwmu", [128, 12, 2]); k.ld(mu.v(), prm["rw_muT"][l])
    c0 = tile("rwc0", [128, 12])
    k.tt("dve", c0.v(), mu[:, :, 0], mu[:, :, 1], OP.add)
    k.ts("dve", c0.v(), c0.v(), -1.0, OP.mult, 1.0, OP.add)
    raw = [tile("rwraw%d" % i, [128, NT]) for i in range(2)]
    outb = [tile("rwout%d" % i, [128, NT]) for i in range(2)]
    ib = 0
    for ch in range(12):
        rw_ = raw[ch % 2]; ob = outb[ch % 2]
        for tb in range(0, NT, 512):
            n = min(512, NT - tb)
            pt = P.pb[ib % 2]; ib += 1
            for dc in range(8):
                k.mm(pt[:, 0:n], wrw[:, dc, ch * 128:(ch + 1) * 128], hT[:, dc, tb:tb + n], start=(dc == 0), stop=(dc == 7))
            k.cp("act", rw_[:, tb:tb + n], pt[:, 0:n])
        k.act(ob.v(), rw_.v(), AF.Identity, scale=c0[:, ch:ch + 1])
        for (s0, s1) in ((0, NCTX), (NCTX, NT)):
            k.stt("dve", ob[:, s0 + 1:s1], rw_[:, s0:s1 - 1], mu[:, ch, 0:1], ob[:, s0 + 1:s1], OP.mult, OP.add)
            k.stt("dve", ob[:, s0:s1 - 1], rw_[:, s0 + 1:s1], mu[:, ch, 1:2], ob[:, s0:s1 - 1], OP.mult, OP.add)
        k.dma(PRW[ch], ob.h[:], reads=ob.res, writes=PRW_res)
    k.barrier()
    es.close()


def phase_rwkv(P, l, prm, PRW, PRW_res, MIXT, MIXT_res):
    k = P.k
    nc = P.nc
    es = ExitStack()
    tile = lambda name, shape, dt=F32: T(es.enter_context(usb(nc, "t_" + name, shape, dt)), name)
    ident = P.ident
    pb = P.pb
    import os
    NSTEP = int(os.environ.get("RWSTEPS", NCH))
    NGRP = int(os.environ.get("RWGRPS", 3))
    tw = tile("rw_tw", [128, NT]); ad = tile("rw_ad", [128, NT]); sg = tile("rw_sg", [128, NT])
    k.dma(tw.h[:], PRW[9], reads=PRW_res, writes=tw.res)
    k.dma(ad.h[:], PRW[10], reads=PRW_res, writes=ad.res)
    k.dma(sg.h[:], PRW[11], reads=PRW_res, writes=sg.res)
    k.act(tw.v(), tw.v(), AF.Tanh)
    k.act(sg.v(), sg.v(), AF.Sigmoid)
    wup = tile("rw_wup_sb", [128, 384]); k.ld(wup.v(), prm["rw_wup"][l])
    aup = tile("rw_aup_sb", [128, 384]); k.ld(aup.v(), prm["rw_aup"][l])
    gup = tile("rw_gup_sb", [128, 384]); k.ld(gup.v(), prm["rw_gup"][l])
    rwc = tile("rw_cols_sb", [128, 3, 9]); k.ld(rwc.v(), prm["rw_cols"][l])
    omka = tile("rw_omka", [128, 3])
    k.ts("dve", omka.v(), rwc[:, :, 1], -1.0, OP.mult, 1.0, OP.add)
    BD2 = tile("rw_bd2", [128, 128]); k.ld(BD2.v(), P.din["c_BD2"])
    msk = tile("rw_mask", [128, 2, 320]); k.ld(msk.v(), P.din["c_rwmask"])
    rst = tile("rw_rst", [128, NCH, 64], BF16)
    k.memset("dve", rst.v(), 1.0); k.memset("dve", rst[:, :, 0:1], 0.0)
    b_r = tile("rw_r", [128, NT]); b_k = tile("rw_k", [128, NT]); b_v = tile("rw_v", [128, NT])
    b_kh = tile("rw_kh", [128, NT]); b_bs = tile("rw_bs", [128, NT])
    b_ys = tile("rw_ys", [128, NT])
    b_lw = tile("rw_lw", [128, NT]); b_a = tile("rw_a", [128, NT]); b_kt = tile("rw_kt", [128, NT])
    b_cl = tile("rw_cl", [128, NT]); b_At = tile("rw_At", [128, NT])
    gC = tile("rw_gC", [128, NCH])
    ST = tile("rw_ST", [128, 64])
    STb = tile("rw_STb", [128, 64], BF16)
    Mm = tile("rw_Mm", [128, 320], BF16)
    TT = tile("rw_TT", [128, 192], BF16)
    XT = [tile("rw_XT%d" % i, [128, 64], BF16) for i in range(2)]
    Mj = [tile("rw_Mj%d" % i, [128, 128], BF16) for i in range(2)]
    MjT = [tile("rw_MjT%d" % i, [128, 128], BF16) for i in range(2)]
    M1bd = tile("rw_M1bd", [128, 128], BF16); M1Tbd = tile("rw_M1Tbd", [128, 128], BF16)
    k.memset("dve", M1bd.v(), 0.0); k.memset("dve", M1Tbd.v(), 0.0)
    c_At = tile("rw_cAt", [128, NT], BF16); c_Rt = tile("rw_cRt", [128, NT], BF16)
    c_Bt = tile("rw_cBt", [128, NT], BF16); c_Kt = tile("rw_cKt", [128, NT], BF16)
    c_v = tile("rw_cv", [128, NT], BF16)
    identb = P.identb
    HP = [slice(0, 64), slice(64, 128)]

    def blocks():
        for tb in range(0, NT, 512):
            yield tb, min(512, NT - tb)

    def bd_sum(dst, src, scale=1.0):
        for i, (tb, n) in enumerate(blocks()):
            pt = pb[i % 2]
            k.mm(pt[:, 0:n], BD2.v(), src[:, tb:tb + n])
            k.act(dst[:, tb:tb + n], pt[:, 0:n], AF.Identity, scale=scale)

    for g in range(NGRP):
        k.dma(b_r.h[:], PRW[g], reads=PRW_res, writes=b_r.res)
        k.dma(b_k.h[:], PRW[3 + g], reads=PRW_res, writes=b_k.res)
        k.dma(b_v.h[:], PRW[6 + g], reads=PRW_res, writes=b_v.res)
        gs = slice(g * 128, (g + 1) * 128)
        k.ts("dve", b_kh.v(), b_k.v(), rwc[:, g, 0:1], OP.mult)
        k.tt("pool", b_At.v(), b_kh.v(), b_kh.v(), OP.mult)
        bd_sum(b_cl, b_At)
        k.ts("dve", b_cl.v(), b_cl.v(), 1e-12, OP.add)
        k.act(b_cl.v(), b_cl.v(), AF.Sqrt)
        k.recip(b_cl.v(), b_cl.v())
        k.tt("dve", b_kh.v(), b_kh.v(), b_cl.v(), OP.mult)
        k.memset("pool", b_ys.v(), 0.0)
        for dr in range(2):
            D = slice(dr * 64, dr * 64 + 64)
            for i, (tb, n) in enumerate(blocks()):
                pt = pb[i % 2]
                k.mm(pt[:, 0:n], wup[D, gs], tw[D, tb:tb + n])
                k.act(b_lw[:, tb:tb + n], pt[:, 0:n], AF.Sigmoid, bias=rwc[:, g, 5 + dr:6 + dr])
            k.ts("dve", b_lw.v(), b_lw.v(), -RW_C, OP.mult)
            for i, (tb, n) in enumerate(blocks()):
                pt = pb[2 + i % 2]
                k.mm(pt[:, 0:n], aup[D, gs], ad[D, tb:tb + n])
                k.act(b_a[:, tb:tb + n], pt[:, 0:n], AF.Sigmoid, bias=rwc[:, g, 7 + dr:8 + dr])
            k.ts("dve", b_kt.v(), b_a.v(), rwc[:, g, 1:2], OP.mult, omka[:, g:g + 1], OP.add)
            k.tt("dve", b_kt.v(), b_kt.v(), b_k.v(), OP.mult)
            k.tt("pool", b_a.v(), b_a.v(), b_kh.v(), OP.mult)
            k.stt("dve", b_At.v(), b_r.v(), rwc[:, g, 2:3], b_kt.v(), OP.mult, OP.mult)
            for i, (tb, n) in enumerate(blocks()):
                pt = pb[i % 2]
                k.mm(pt[:, 0:n], BD2.v(), b_At[:, tb:tb + n])
                if dr == 0:
                    k.tt("dve", b_bs[:, tb:tb + n], pt[:, 0:n], b_v[:, tb:tb + n], OP.mult)
                else:
                    k.tt("dve", b_At[:, tb:tb + n], pt[:, 0:n], b_v[:, tb:tb + n], OP.mult)
            if dr == 1:
                k.tt("pool", b_bs.v(), b_bs.v(), b_At.v(), OP.add)
            k.op("dve", lambda e: e.tensor_tensor_scan(b_cl.h[:], rst.h[:].rearrange("p c s -> p (c s)"), b_lw.h[:], 0.0, OP.mult, OP.add),
                 rst.res + b_lw.res, b_cl.res)
            cl3 = rr(b_cl.v(), "p (c s) -> p c s", s=64)
            k.cp("dve", gC.v(), cl3[:, :, 63])
            if dr == 1:
                k.tt("dve", cl3, bc(V(gC.h[:].unsqueeze(2), gC.res), [128, NCH, 64]), cl3, OP.subtract)
                k.tt("dve", b_cl.v(), b_cl.v(), b_lw.v(), OP.add)
            k.act(gC.v(), gC.v(), AF.Exp)
            k.tt("dve", b_lw.v(), b_cl.v(), b_lw.v(), OP.subtract)
            k.act(b_lw.v(), b_lw.v(), AF.Exp)
            k.stt("dve", b_At.v(), b_kh.v(), -1.0, b_lw.v(), OP.mult, OP.mult)
            k.act(b_lw.v(), b_cl.v(), AF.Exp)
            k.tt("dve", b_lw.v(), b_lw.v(), b_r.v(), OP.mult)
            k.act(b_cl.v(), b_cl.v(), AF.Exp, scale=-1.0)
            k.tt("dve", b_a.v(), b_a.v(), b_cl.v(), OP.mult)
            k.tt("pool", b_kt.v(), b_kt.v(), b_cl.v(), OP.mult)
            k.cp("pool", c_At.v(), b_At.v()); k.cp("act", c_Rt.v(), b_lw.v())
            k.cp("pool", c_Bt.v(), b_a.v()); k.cp("act", c_Kt.v(), b_kt.v())
            if dr == 0:
                k.cp("pool", c_v.v(), b_v.v())
            b_Rt, b_Bt, b_Kt = c_Rt, c_Bt, c_Kt
            k.memset("dve", ST.v(), 0.0)
            k.memset("dve", STb.v(), 0.0)
            order = ORD_F if dr == 0 else ORD_R
            pM, pT, pW, pX, pQ, pQT, pY, pS = pb
            for step in range(NSTEP):
                c = order[step]
                tok = slice(c * 64, c * 64 + 64)
                for hp in HP:
                    k.mm(pM[hp, 0:64], b_Bt[hp, tok], c_At[hp, tok])
                    k.mm(pM[hp, 64:128], b_Bt[hp, tok], b_Rt[hp, tok])
                    k.mm(pM[hp, 128:192], b_Kt[hp, tok], c_At[hp, tok])
                    k.mm(pM[hp, 192:256], b_Kt[hp, tok], b_Rt[hp, tok])
                    k.mm(pM[hp, 256:320], c_At[hp, tok], b_Bt[hp, tok])
                    k.mm(pT[hp, 0:64], c_v[hp, tok], identb[hp, hp])
                    k.mm(pT[hp, 64:128], b_Bt[hp, tok], identb[hp, hp])
                    k.mm(pT[hp, 128:192], b_Kt[hp, tok], identb[hp, hp])
                k.tt("dve", Mm.v(), pM[:, 0:320], msk[:, dr, :], OP.mult)
                k.cp("act", TT.v(), pT[:, 0:192])
                VT, BtT, KtT = TT[:, 0:64], TT[:, 64:128], TT[:, 128:192]
                M1, N1, M2, N2, M1T = [Mm[:, i * 64:(i + 1) * 64] for i in range(5)]
                for hp in HP:
                    k.mm(pW[hp, 0:64], c_At[hp, tok], STb[hp, :], start=True, stop=False)
                    k.mm(pW[hp, 0:64], V(M2.ap[hp], M2.res), V(VT.ap[hp], VT.res), start=False, stop=True)
                x = XT[0]
                k.cp("act", x.v(), pW[:, 0:64])
                for hpi, hp in enumerate(HP):
                    k.cp("act", M1bd[hp, hpi * 64:(hpi + 1) * 64], V(M1.ap[hp], M1.res))
                    k.cp("pool", M1Tbd[hp, hpi * 64:(hpi + 1) * 64], V(M1T.ap[hp], M1T.res))
                mj, mjT = M1bd.v(), M1Tbd.v()
                for j in range(6):
                    xn = XT[(j + 1) % 2]
                    k.mm(pX[:, 0:64], mj, x.v())
                    k.tt("dve", xn.v(), pX[:, 0:64], x.v(), OP.add)
                    x = xn
                    if j < 5:
                        k.mm(pQ[:, 0:128], mjT, mj)
                        k.mm(pQT[:, 0:128], mj, mjT)
                        nmj = Mj[j % 2]; nmjT = MjT[j % 2]
                        k.cp("act", nmj.v(), pQ[:, 0:128])
                        k.cp("dve", nmjT.v(), pQT[:, 0:128])
                        mj, mjT = nmj.v(), nmjT.v()
                UT = x
                for hp in HP:
                    k.mm(pY[hp, 0:64], STb[hp, :], b_Rt[hp, tok], start=True, stop=False)
                    k.mm(pY[hp, 0:64], UT[hp, :], V(N1.ap[hp], N1.res), start=False, stop=False)
                    k.mm(pY[hp, 0:64], V(VT.ap[hp], VT.res), V(N2.ap[hp], N2.res), start=False, stop=True)
                k.tt("dve", b_ys[:, tok], b_ys[:, tok], pY[:, 0:64], OP.add)
                for hp in HP:
                    k.mm(pS[hp, 0:64], V(BtT.ap[hp], BtT.res), UT[hp, :], start=True, stop=False)
                    k.mm(pS[hp, 0:64], V(KtT.ap[hp], KtT.res), V(VT.ap[hp], VT.res), start=False, stop=True)
                k.tt("dve", ST.v(), ST.v(), pS[:, 0:64], OP.add)
                k.ts("dve", ST.v(), ST.v(), gC[:, c:c + 1], OP.mult)
                k.cp("act", STb.v(), ST.v())
        if "rw_ys" in P.debug and g == 0:
            P.dump("rw_ys", b_ys.v())
        bd_sum(b_cl, b_ys, 1.0 / 64)
        k.tt("dve", b_ys.v(), b_ys.v(), b_cl.v(), OP.subtract)
        k.tt("pool", b_At.v(), b_ys.v(), b_ys.v(), OP.mult)
        bd_sum(b_cl, b_At, 1.0 / 64)
        k.ts("dve", b_cl.v(), b_cl.v(), 64e-5, OP.add)
        k.act(b_cl.v(), b_cl.v(), AF.Sqrt)
        k.recip(b_cl.v(), b_cl.v())
        k.tt("dve", b_ys.v(), b_ys.v(), b_cl.v(), OP.mult)
        k.ts("dve", b_ys.v(), b_ys.v(), rwc[:, g, 3:4], OP.mult, rwc[:, g, 4:5], OP.add)
        k.tt("dve", b_ys.v(), b_ys.v(), b_bs.v(), OP.add)
        for i, (tb, n) in enumerate(blocks()):
            pt = pb[i % 2]
            k.mm(pt[:, 0:n], gup[:, gs], sg[:, tb:tb + n])
            k.tt("dve", b_ys[:, tb:tb + n], b_ys[:, tb:tb + n], pt[:, 0:n], OP.mult)
        k.dma(MIXT[2 + g], b_ys.h[:], reads=b_ys.res, writes=MIXT_res)
    k.barrier()
    es.close()


MLA_SCALE = 96 ** -0.5


def phase_mla(P, l, hT, w_in, prm, MIXT, MIXT_res, need_ctx=True):
    k = P.k
    nc = P.nc
    es = ExitStack()
    tile = lambda name, shape, dt=F32: T(es.enter_context(usb(nc, "t_" + name, shape, dt)), name)
    ident = P.ident
    pb = P.pb
    wat = tile("wat", [128, 8, 672], BF16)
    load_w_bf16(P, wat, w_in[l, :, 2576:3248], 672)
    qup = tile("mla_qup", [128, 3, 576], BF16)
    kvup = tile("mla_kvup", [128, 2, 768], BF16)
    stg = tile("mla_stg", [128, 3, 768])
    k.ld(stg[:, 0:3, 0:576], prm["mla_q_up"][l].rearrange("(c p) n -> p c n", p=128))
    k.cp("dve", qup.v(), stg[:, 0:3, 0:576])
    k.ld(stg[:, 0:2, :], prm["mla_kv_up"][l].rearrange("(c p) n -> p c n", p=128))
    k.cp("dve", kvup.v(), stg[:, 0:2, :])
    gq = tile("mla_gq", [128, 384]); k.ld(gq.v(), prm["mla_gq"][l])
    gkv = tile("mla_gkv", [128, 256]); k.ld(gkv.v(), prm["mla_gkv"][l])
    QT = tile("mla_QT", [96, 6, NT], BF16)
    KT = tile("mla_KT", [96, 6, NT], BF16)
    Vt = tile("mla_V", [128, NTILE, 6, 65], BF16)
    k.memset("pool", Vt.v(), 1.0)
    pa = tile("mla_pa", [128, 672])
    junk = tile("mla_junk", [128, 384])
    st = tile("mla_st", [128, 8])
    cn = tile("mla_cn", [128, 640])
    cnT = tile("mla_cnT", [128, 5, 128], BF16)
    qt = tile("mla_q", [128, 6, 96])
    kvt = tile("mla_kv", [128, 6, 128])
    kf = tile("mla_kf", [128, 6, 96])
    rope = tile("mla_rope", [128, 32])
    rt = tile("mla_rt", [128, 4, 6, 2, 8])
    for ti in range(NTILE):
        tk = slice(ti * 128, (ti + 1) * 128)
        p0, p1 = pb[0], pb[1]
        for dc in range(8):
            k.mm(p0[:, 0:512], hT[:, dc, tk], wat[:, dc, 0:512], start=(dc == 0), stop=(dc == 7))
        for dc in range(8):
            k.mm(p1[:, 0:160], hT[:, dc, tk], wat[:, dc, 512:672], start=(dc == 0), stop=(dc == 7))
        k.cp("act", pa[:, 0:512], p0[:, 0:512])
        k.cp("dve", pa[:, 512:672], p1[:, 0:160])
        k.act(junk[:, 0:384], pa[:, 0:384], AF.Square, accum=st[:, 0:1])
        k.act(junk[:, 0:256], pa[:, 384:640], AF.Square, accum=st[:, 1:2])
        k.ts("dve", st[:, 2:3], st[:, 0:1], 1.0 / 384, OP.mult, LN_EPS, OP.add)
        k.ts("dve", st[:, 3:4], st[:, 1:2], 1.0 / 256, OP.mult, LN_EPS, OP.add)
        k.act(st[:, 4:6], st[:, 2:4], AF.Sqrt)
        k.recip(st[:, 6:8], st[:, 4:6])
        k.stt("dve", cn[:, 0:384], pa[:, 0:384], st[:, 6:7], gq.v(), OP.mult, OP.mult)
        k.stt("dve", cn[:, 384:640], pa[:, 384:640], st[:, 7:8], gkv.v(), OP.mult, OP.mult)
        p2, p3 = pb[2], pb[3]
        for c in range(4):
            k.mm(p2[:, c * 128:(c + 1) * 128], cn[:, c * 128:(c + 1) * 128], ident.v())
        k.mm(p3[:, 0:128], cn[:, 512:640], ident.v())
        k.cp("act", cnT[:, 0:4, :], rr(p2.v(), "p (c t) -> p c t", c=4))
        k.cp("dve", cnT[:, 4, :], p3[:, 0:128])
        p4, p5, p6, p7 = pb[4], pb[5], pb[6], pb[7]
        for c in range(3):
            k.mm(p4[:, 0:512], cnT[:, c, :], qup[:, c, 0:512], start=(c == 0), stop=(c == 2))
        for c in range(3):
            k.mm(p5[:, 0:64], cnT[:, c, :], qup[:, c, 512:576], start=(c == 0), stop=(c == 2))
        for c in range(2):
            k.mm(p6[:, 0:512], cnT[:, 3 + c, :], kvup[:, c, 0:512], start=(c == 0), stop=(c == 1))
        for c in range(2):
            k.mm(p7[:, 0:256], cnT[:, 3 + c, :], kvup[:, c, 512:768], start=(c == 0), stop=(c == 1))
        qf = rr(qt.v(), "p h e -> p (h e)")
        k.cp("act", qf[:, 0:512], p4[:, 0:512])
        k.cp("dve", qf[:, 512:576], p5[:, 0:64])
        kvf = rr(kvt.v(), "p h e -> p (h e)")
        k.cp("act", kvf[:, 0:512], p6[:, 0:512])
        k.cp("dve", kvf[:, 512:768], p7[:, 0:256])
        k.cp("pool", kf[:, :, 0:64], kvt[:, :, 0:64])
        k.cp("pool", Vt[:, ti, :, 0:64], kvt[:, :, 64:128])
        kr = pa[:, 640:672]
        if ti >= NCTX // 128:
            k.ld(rope.v(), P.din["c_rope"][(ti - NCTX // 128) * 128:(ti - NCTX // 128 + 1) * 128, :])
            cosv = rr(rope[:, 0:16], "p (a f) -> p a f", a=2)
            sinv = rr(rope[:, 16:32], "p (a f) -> p a f", a=2)
            for (xv, H_) in ((rr(qt[:, :, 64:96], "p h (a s f) -> p h a s f", a=2, s=2), 6),
                             (rr(V(kr.ap.unsqueeze(1), kr.res), "p h (a s f) -> p h a s f", a=2, s=2), 1)):
                x1 = xv[:, :, :, 0, :]; x2 = xv[:, :, :, 1, :]
                cb = bc(V(cosv.ap.unsqueeze(1), cosv.res), [128, H_, 2, 8])
                sb_ = bc(V(sinv.ap.unsqueeze(1), sinv.res), [128, H_, 2, 8])
                t1, t2, t3, t4 = [rt[:, i, 0:H_, :, :] for i in range(4)]
                k.tt("dve", t1, x1, cb, OP.mult)
                k.tt("pool", t2, x2, sb_, OP.mult)
                k.tt("dve", t3, x2, cb, OP.mult)
                k.tt("pool", t4, x1, sb_, OP.mult)
                k.tt("dve", x1, t1, t2, OP.subtract)
                k.tt("dve", x2, t3, t4, OP.add)
        k.cp("dve", kf[:, :, 64:96], bc(V(kr.ap.unsqueeze(1), kr.res), [128, 6, 32]))
        for (src, dstT, pp) in ((qt, QT, (pb[0], pb[1])), (kf, KT, (pb[2], pb[3]))):
            for half in range(2):
                pt = pp[half]
                for j in range(3):
                    h = half * 3 + j
                    k.mm(pt[0:96, j * 128:(j + 1) * 128], src[:, h, :], ident.v())
                k.cp("act" if half else "dve", dstT[:, half * 3:half * 3 + 3, tk],
                     rr(pt[0:96, 0:384], "p (j t) -> p j t", j=3))
    if "mla_QT" in P.debug:
        P.dump("mla_QT", QT.v()); P.dump("mla_KT", KT.v())
    E = [tile("mla_E%d" % i, [128, 512], BF16) for i in range(2)]
    P.mla_oT = [tile("mla_oT%d" % i, [128, 3, 128]) for i in range(2)]
    osb = tile("mla_o", [128, 4, 6, 64])
    rs = tile("mla_rs", [128, 4])
    qblocks = [(NCTX + i * 512, 512, list(range(NTILE))) for i in range(4)]
    if need_ctx:
        qblocks.append((0, 256, [0, 1]))
    ei = 0
    for (q0, qn, ktiles) in qblocks:
        nsub = qn // 128
        for h in range(6):
            for kidx, kt_ in enumerate(ktiles):
                ps = pb[4 + ei % 2]
                e_ = E[ei % 2]; ei += 1
                k.mm(ps[:, 0:qn], KT[:, h, kt_ * 128:(kt_ + 1) * 128], QT[:, h, q0:q0 + qn])
                k.act(e_[:, 0:qn], ps[:, 0:qn], AF.Exp, scale=MLA_SCALE)
                for j in range(nsub):
                    k.mm(pb[j][:, 0:65], e_[:, j * 128:(j + 1) * 128], Vt[:, kt_, h, :],
                         start=(kidx == 0), stop=(kidx == len(ktiles) - 1))
            for j in range(nsub):
                k.recip(rs[:, j:j + 1], pb[j][:, 64:65])
                k.ts("dve", osb[:, j, h, :], pb[j][:, 0:64], rs[:, j:j + 1], OP.mult)
        for j in range(nsub):
            pt = pb[6 + j % 2]
            of = rr(osb[:, j, :, :], "p h e -> p (h e)")
            for c in range(3):
                k.mm(pt[:, c * 128:(c + 1) * 128], of[:, c * 128:(c + 1) * 128], ident.v())
            ot = tile("mla_oT_%d_%d" % (q0, j), [128, 3, 128]) if False else P.mla_oT[j % 2]
            k.cp("act", ot.v(), rr(pt[:, 0:384], "p (c t) -> p c t", c=3))
            t0_ = q0 + j * 128
            k.dma(MIXT[5:8, :, t0_:t0_ + 128].rearrange("c p t -> p c t"), ot.h[:], reads=ot.res, writes=MIXT_res)
    k.barrier()
    es.close()


def phase_mixout(P, l, w_out, prm, MIXT, MIXT_res, src_ap, src_res, XD, XD_res, MODD, MOD_res, need_ctx=True):
    k = P.k
    nc = P.nc
    es = ExitStack()
    tile = lambda name, shape, dt=F32: T(es.enter_context(usb(nc, "t_" + name, shape, dt)), name)
    pb = P.pb
    wout = tile("wout", [128, 8, 1024], BF16)
    load_w_bf16(P, wout, w_out[l], 1024)
    ga = [tile("mo_ga%d" % s, [128, D]) for s in range(2)]
    for s in range(2):
        k.dma(ga[s].h[:], MODD[s, MOD_GA], reads=MOD_res, writes=ga[s].res)
    lng = tile("mo_lng", [128, D]); k.ld(lng.v(), prm["ln_mix_g"][l])
    lnb = tile("mo_lnb", [128, D]); k.ld(lnb.v(), prm["ln_mix_b"][l])
    mt = [tile("mo_mt%d" % i, [128, 8, 128]) for i in range(2)]
    mb = [tile("mo_mb%d" % i, [128, 8, 128], BF16) for i in range(2)]
    xt = [tile("mo_x%d" % i, [128, D]) for i in range(2)]
    ut = [tile("mo_u%d" % i, [128, D]) for i in range(2)]
    st = [tile("mo_st%d" % i, [128, 16]) for i in range(2)]
    t0 = 0 if need_ctx else NCTX // 128
    for ti in range(t0, NTILE):
        s = 1 if ti < NCTX // 128 else 0
        m_ = mt[ti % 2]; b_ = mb[ti % 2]; x = xt[ti % 2]; u = ut[ti % 2]; st_ = st[ti % 2]
        k.dma(m_.h[:], MIXT[:, :, ti * 128:(ti + 1) * 128].rearrange("c p t -> p c t"), reads=MIXT_res, writes=m_.res)
        k.dma(x.h[:], src_ap[ti * 128:(ti + 1) * 128, :], reads=src_res[ti], writes=x.res)
        k.cp("pool", b_.v(), m_.v())
        for half in range(2):
            pt = pb[(ti * 2 + half) % 4]
            for c in range(8):
                k.mm(pt.v(), b_[:, c, :], wout[:, c, half * 512:(half + 1) * 512], start=(c == 0), stop=(c == 7))
            k.tt("dve", u[:, half * 512:(half + 1) * 512], pt.v(), ga[s][:, half * 512:(half + 1) * 512], OP.mult)
        k.stt("dve", u.v(), x.v(), ALPHA, u.v(), OP.mult, OP.add)
        mean, rstd, nmr = ln_stats(P, u.v(), st_)
        k.act(u.v(), u.v(), AF.Identity, bias=nmr, scale=rstd)
        k.tt("pool", u.v(), u.v(), lng.v(), OP.mult)
        k.tt("dve", u.v(), u.v(), lnb.v(), OP.add)
        k.dma(XD[ti * 128:(ti + 1) * 128, :], u.h[:], reads=u.res, writes=XD_res[ti])
    k.barrier()
    es.close()


def phase_peer(P, l, prm, peer_w_q, peer_keys, peer_u, peer_v, XD, XD_res, dst_ap, MODD, MOD_res, need_ctx=True):
    import os
    k = P.k
    nc = P.nc
    es = ExitStack()
    tile = lambda name, shape, dt=F32: T(es.enter_context(usb(nc, "t_" + name, shape, dt)), name)
    pb = P.pb
    ident = P.ident
    NSLOT = int(os.environ.get("PEER_SLOTS", 128))
    wq = tile("pe_wq", [128, 8, 2048])
    for c0 in range(0, 2048, 512):
        k.ld(wq[:, :, c0:c0 + 512], peer_w_q[l, :, c0:c0 + 512].rearrange("(c p) n -> p c n", p=128))
    keysT = tile("pe_keysT", [128, 16, 128])
    kst = tile("pe_kst", [128, 4, 128])
    for g4 in range(4):
        k.ld(kst.v(), peer_keys[l].rearrange("h p k d -> k (h p) d")[:, g4 * 4:(g4 + 1) * 4, :])
        pt = pb[g4 % 2]
        for j in range(4):
            k.tr(pt[:, j * 128:(j + 1) * 128], kst[:, j, :], ident.v())
        k.cp("act", keysT[:, g4 * 4:(g4 + 1) * 4, :], rr(pt.v(), "p (j t) -> p j t", j=4))
    mods = {}
    for s in ([0, 1] if need_ctx else [0]):
        for j in (MOD_SHF, MOD_SCF, MOD_GF):
            mods[(s, j)] = tile("pe_mod%d_%d" % (s, j), [128, D])
            k.dma(mods[(s, j)].h[:], MODD[s, j], reads=MOD_res, writes=mods[(s, j)].res)
        k.ts("pool", mods[(s, MOD_SCF)].v(), mods[(s, MOD_SCF)].v(), 1.0, OP.add)
    lng = tile("pe_lng", [128, D]); k.ld(lng.v(), prm["ln_ffn_g"][l])
    lnb = tile("pe_lnb", [128, D]); k.ld(lnb.v(), prm["ln_ffn_b"][l])
    iota16 = tile("pe_iota", [128, 16]); k.ld(iota16.v(), P.din["c_iota16"])
    x = tile("pe_x", [128, D]); h = tile("pe_h", [128, D]); st = tile("pe_st", [128, 16])
    hT = tile("pe_hT", [128, 8, 128])
    qT = tile("pe_qT", [128, 16, 128])
    sc = tile("pe_s", [128, 16, 128])
    wk = tile("pe_wk", [128, 128])
    vals = tile("pe_vals", [128, 16, 16])
    idxu = tile("pe_idxu", [128, 16, 16], U32)
    idxf = tile("pe_idxf", [128, 16, 16])
    cand = tile("pe_cand", [128, 8, 16, 16])
    wk2 = tile("pe_wk2", [128, 256])
    best = tile("pe_best", [128, 8, 16])
    posu = tile("pe_posu", [128, 8, 16], U32)
    posu2 = tile("pe_posu2", [128, 2, 8, 16], U32)
    cu32 = tile("pe_cu32", [128, 2], U32); k.ld(cu32.v(), P.din["c_u32"])
    pa_ = tile("pe_pa", [128, 8, 16]); pb_ = tile("pe_pb", [128, 8, 16])
    oh = tile("pe_oh", [128, 8, 16, 16])
    eidf = tile("pe_eidf", [128, 8, 16]); eid2 = tile("pe_eid2", [128, 8, 16])
    eidu = tile("pe_eidu", [128, 128], U32)
    gate = tile("pe_gate", [128, 8, 16]); gs = tile("pe_gs", [128, 8])
    actv = tile("pe_act", [128, 128]); t1 = tile("pe_t1", [128, 128]); wgt = tile("pe_wgt", [128, 128])
    NB = 4
    ug = [tile("pe_ug%d" % i, [128, D]) for i in range(NB)]
    vg = ug
    junk = tile("pe_junk", [128, D])
    acc = tile("pe_acc", [128, D])
    t0 = 0 if need_ctx else NCTX // 128
    NTL = int(os.environ.get("PEER_TILES", NTILE))
    for ti in range(t0, min(NTILE, t0 + NTL)):
        s = 1 if ti < NCTX // 128 else 0
        k.dma(x.h[:], XD[ti * 128:(ti + 1) * 128, :], reads=XD_res[ti], writes=x.res)
        mean, rstd, nmr = ln_stats(P, x.v(), st)
        k.act(h.v(), x.v(), AF.Identity, bias=nmr, scale=rstd)
        k.tt("dve", h.v(), h.v(), mods[(s, MOD_SCF)].v(), OP.mult)
        k.tt("pool", h.v(), h.v(), mods[(s, MOD_SHF)].v(), OP.add)
        for g4 in range(2):
            pt = pb[g4]
            for j in range(4):
                k.tr(pt[:, j * 128:(j + 1) * 128], h[:, (g4 * 4 + j) * 128:(g4 * 4 + j + 1) * 128], ident.v())
            k.cp("act" if g4 else "dve", hT[:, g4 * 4:(g4 + 1) * 4, :], rr(pt.v(), "p (j t) -> p j t", j=4))
        for g4 in range(4):
            pt = pb[2 + g4 % 2]
            for j in range(4):
                c16 = g4 * 4 + j
                for dc in range(8):
                    k.mm(pt[:, j * 128:(j + 1) * 128], wq[:, dc, c16 * 128:(c16 + 1) * 128], hT[:, dc, :], start=(dc == 0), stop=(dc == 7))
            k.cp("act" if g4 % 2 else "dve", qT[:, g4 * 4:(g4 + 1) * 4, :], rr(pt.v(), "p (j t) -> p j t", j=4))
        for g4 in range(4):
            pt = pb[4 + g4 % 2]
            for j in range(4):
                c16 = g4 * 4 + j
                k.mm(pt[:, j * 128:(j + 1) * 128], qT[:, c16, :], keysT[:, c16, :])
            k.cp("act" if g4 % 2 else "dve", sc[:, g4 * 4:(g4 + 1) * 4, :], rr(pt.v(), "p (j t) -> p j t", j=4))
        for c16 in range(16):
            k.op("dve", lambda e: e.max(vals.h[:, c16, 0:8], sc.h[:, c16, :]), sc.res, vals.res)
            k.op("dve", lambda e: e.max_index(idxu.h[:, c16, 0:8], vals.h[:, c16, 0:8], sc.h[:, c16, :]), sc.res + vals.res, idxu.res)
            k.op("dve", lambda e: e.match_replace(wk.h[:], vals.h[:, c16, 0:8], sc.h[:, c16, :], -1e30), sc.res + vals.res, wk.res)
            k.op("dve", lambda e: e.max(vals.h[:, c16, 8:16], wk.h[:]), wk.res, vals.res)
            k.op("dve", lambda e: e.max_index(idxu.h[:, c16, 8:16], vals.h[:, c16, 8:16], wk.h[:]), wk.res + vals.res, idxu.res)
        k.cp("dve", idxf.v(), idxu.v())
        v4 = rr(vals.v(), "p (h q) a -> p h q a", q=2)
        i4 = rr(idxf.v(), "p (h q) a -> p h q a", q=2)
        v1 = v4[:, :, 0, :]; v2 = v4[:, :, 1, :]
        k.tt("dve", cand.v(), bc(V(v1.ap.unsqueeze(3), v1.res), [128, 8, 16, 16]), bc(V(v2.ap.unsqueeze(2), v2.res), [128, 8, 16, 16]), OP.add)
        for hh in range(8):
            cf = rr(cand[:, hh, :, :], "p a b -> p (a b)")
            k.op("dve", lambda e: e.max(best.h[:, hh, 0:8], cf.ap), cand.res, best.res)
            k.op("dve", lambda e: e.max_index(posu.h[:, hh, 0:8], best.h[:, hh, 0:8], cf.ap), cand.res + best.res, posu.res)
            k.op("dve", lambda e: e.match_replace(wk2.h[:], best.h[:, hh, 0:8], cf.ap, -1e30), cand.res + best.res, wk2.res)
            k.op("dve", lambda e: e.max(best.h[:, hh, 8:16], wk2.h[:]), wk2.res, best.res)
            k.op("dve", lambda e: e.max_index(posu.h[:, hh, 8:16], best.h[:, hh, 8:16], wk2.h[:]), wk2.res + best.res, posu.res)
        k.ts("dve", posu2[:, 0, :, :], posu.v(), cu32[:, 0:1], OP.bitwise_and)
        k.ts("dve", posu2[:, 1, :, :], posu.v(), cu32[:, 1:2], OP.logical_shift_right)
        k.cp("dve", pb_.v(), posu2[:, 0, :, :])
        k.cp("dve", pa_.v(), posu2[:, 1, :, :])
        io4 = bc(V(iota16.h[:].unsqueeze(1).unsqueeze(1), iota16.res), [128, 8, 16, 16])
        for (pp, isrc, dst) in ((pa_, i4[:, :, 0, :], eidf), (pb_, i4[:, :, 1, :], eid2)):
            k.tt("dve", oh.v(), io4, bc(V(pp.h[:].unsqueeze(3), pp.res), [128, 8, 16, 16]), OP.is_equal)
            k.tt("dve", oh.v(), oh.v(), bc(V(isrc.ap.unsqueeze(2), isrc.res), [128, 8, 16, 16]), OP.mult)
            k.red("dve", dst.v(), oh.v(), OP.add)
        k.stt("dve", eidf.v(), eidf.v(), 128.0, eid2.v(), OP.mult, OP.add)
        k.cp("dve", eidu.v(), rr(eidf.v(), "p h k -> p (h k)"))
        k.tt("dve", gate.v(), best.v(), bc(best[:, :, 0:1], [128, 8, 16]), OP.subtract)
        k.act(gate.v(), gate.v(), AF.Exp)
        k.red("dve", gs.v(), gate.v(), OP.add)
        k.recip(gs.v(), gs.v())
        k.tt("dve", gate.v(), gate.v(), bc(V(gs.h[:].unsqueeze(2), gs.res), [128, 8, 16]), OP.mult)
        k.memset("dve", actv.v(), 0.0)
        for sl in range(NSLOT):
            u_ = ug[sl % NB]
            k.dma_custom("pool", lambda e: e.indirect_dma_start(out=u_.h[:], out_offset=None, in_=peer_u.rearrange("l e d -> (l e) d"),
                         in_offset=bass.IndirectOffsetOnAxis(ap=eidu.h[:, sl:sl + 1], axis=0), element_offset=l * 16384 * D), eidu.res, u_.res)
            k.op("dve", lambda e: e.scalar_tensor_tensor(junk.h[:], u_.h[:], 1.0, h.h[:], op0=OP.mult, op1=OP.mult, accum_out=actv.h[:, sl:sl + 1]),
                 u_.res + h.res, junk.res + actv.res)
        k.tt("dve", t1.v(), actv.v(), actv.v(), OP.mult)
        k.ts("dve", t1.v(), t1.v(), 0.044715, OP.mult, 1.0, OP.add)
        k.tt("dve", t1.v(), t1.v(), actv.v(), OP.mult)
        k.act(t1.v(), t1.v(), AF.Sigmoid, scale=1.5957691216057308)
        k.tt("dve", wgt.v(), t1.v(), actv.v(), OP.mult)
        k.tt("dve", wgt.v(), wgt.v(), rr(gate.v(), "p h k -> p (h k)"), OP.mult)
        k.memset("pool", acc.v(), 0.0)
        for sl in range(NSLOT):
            v_ = vg[sl % NB]
            k.dma_custom("pool", lambda e: e.indirect_dma_start(out=v_.h[:], out_offset=None, in_=peer_v.rearrange("l e d -> (l e) d"),
                         in_offset=bass.IndirectOffsetOnAxis(ap=eidu.h[:, sl:sl + 1], axis=0), element_offset=l * 16384 * D), eidu.res, v_.res)
            k.stt("dve", acc.v(), v_.v(), wgt[:, sl:sl + 1], acc.v(), OP.mult, OP.add)
        if "peer_y" in P.debug:
            k.dma(P.dout["peer_y"][ti * 128:(ti + 1) * 128, :], acc.h[:], reads=acc.res)
        k.tt("dve", acc.v(), acc.v(), mods[(s, MOD_GF)].v(), OP.mult)
        k.stt("dve", acc.v(), x.v(), ALPHA, acc.v(), OP.mult, OP.add)
        mean, rstd, nmr = ln_stats(P, acc.v(), st)
        k.act(acc.v(), acc.v(), AF.Identity, bias=nmr, scale=rstd)
        k.tt("pool", acc.v(), acc.v(), lng.v(), OP.mult)
        k.tt("dve", acc.v(), acc.v(), lnb.v(), OP.add)
        d_ap, d_res = dst_ap(ti)
        k.dma(d_ap, acc.h[:], reads=acc.res, writes=d_res)
    k.barrier()
    es.close()


def phase_peer_route(P, l, prm, peer_w_q, peer_keys, XD, XD_res, MODD, MOD_res, RTD, RT_res, HFD, HF_res, need_ctx=True, prep=None):
    import os
    k = P.k
    nc = P.nc
    es = ExitStack()
    tile = lambda name, shape, dt=F32: T(es.enter_context(usb(nc, "t_" + name, shape, dt)), name)
    pb = P.pb
    ident = P.ident
    NSLOT = int(os.environ.get("PEER_SLOTS", 128))
    wq = tile("pe_wq", [128, 8, 2048])
    for c0 in range(0, 2048, 512):
        k.ld(wq[:, :, c0:c0 + 512], peer_w_q[l, :, c0:c0 + 512].rearrange("(c p) n -> p c n", p=128))
    keysT = tile("pe_keysT", [128, 16, 128])
    kst = tile("pe_kst", [128, 4, 128])
    for g4 in range(4):
        k.ld(kst.v(), peer_keys[l].rearrange("h p k d -> k (h p) d")[:, g4 * 4:(g4 + 1) * 4, :])
        pt = pb[g4 % 2]
        for j in range(4):
            k.tr(pt[:, j * 128:(j + 1) * 128], kst[:, j, :], ident.v())
        k.cp("act", keysT[:, g4 * 4:(g4 + 1) * 4, :], rr(pt.v(), "p (j t) -> p j t", j=4))
    mods = {}
    for s in ([0, 1] if need_ctx else [0]):
        for j in (MOD_SHF, MOD_SCF):
            mods[(s, j)] = tile("pe_mod%d_%d" % (s, j), [128, D])
            k.dma(mods[(s, j)].h[:], MODD[s, j], reads=MOD_res, writes=mods[(s, j)].res)
        k.ts("pool", mods[(s, MOD_SCF)].v(), mods[(s, MOD_SCF)].v(), 1.0, OP.add)
    rtT = tile("pe_rtT", [128, 3, 128]); hTb = tile("pe_hTb", [128, 8, 128], BF16)
    iota16 = tile("pe_iota", [128, 16]); k.ld(iota16.v(), P.din["c_iota16"])
    x = tile("pe_x", [128, D]); h = tile("pe_h", [128, D]); st = tile("pe_st", [128, 16])
    hT = tile("pe_hT", [128, 8, 128])
    qT = tile("pe_qT", [128, 16, 128])
    sc = tile("pe_s", [128, 16, 128])
    wk = tile("pe_wk", [128, 128])
    vals = tile("pe_vals", [128, 16, 16])
    idxu = tile("pe_idxu", [128, 16, 16], U32)
    idxf = tile("pe_idxf", [128, 16, 16])
    cand = tile("pe_cand", [128, 8, 16, 16])
    wk2 = tile("pe_wk2", [128, 256])
    best = tile("pe_best", [128, 8, 16])
    posu = tile("pe_posu", [128, 8, 16], U32)
    posu2 = tile("pe_posu2", [128, 2, 8, 16], U32)
    cu32 = tile("pe_cu32", [128, 2], U32); k.ld(cu32.v(), P.din["c_u32"])
    pa_ = tile("pe_pa", [128, 8, 16]); pb_ = tile("pe_pb", [128, 8, 16])
    oh = tile("pe_oh", [128, 8, 16, 16])
    eidf = tile("pe_eidf", [128, 8, 16]); eid2 = tile("pe_eid2", [128, 8, 16])
    eidu = tile("pe_eidu", [128, 128], U32)
    gate = tile("pe_gate", [128, 8, 16]); gs = tile("pe_gs", [128, 8])
    actv = tile("pe_act", [128, 128]); t1 = tile("pe_t1", [128, 128]); wgt = tile("pe_wgt", [128, 128])
    t0 = 0 if need_ctx else NCTX // 128
    NTL = int(os.environ.get("PEER_TILES", NTILE))
    pst = prep_alloc(P, tile) if prep is not None else None
    nprep = 0
    tiles = list(range(t0, min(NTILE, t0 + NTL)))
    for ti in tiles:
        s = 1 if ti < NCTX // 128 else 0
        if prep is not None:
            tgt = 128 if ti == tiles[-1] else min(128, (tiles.index(ti) + 1) * 8)
            while nprep < tgt:
                prep_chunk(P, pst, l, nprep, prep[0], prep[1], prep[2], prep[3], prep[4], [pb[7]])
                nprep += 1
        k.dma(x.h[:], XD[ti * 128:(ti + 1) * 128, :], reads=XD_res[ti], writes=x.res)
        mean, rstd, nmr = ln_stats(P, x.v(), st)
        k.act(h.v(), x.v(), AF.Identity, bias=nmr, scale=rstd)
        k.tt("dve", h.v(), h.v(), mods[(s, MOD_SCF)].v(), OP.mult)
        k.tt("pool", h.v(), h.v(), mods[(s, MOD_SHF)].v(), OP.add)
        for g4 in range(2):
            pt = pb[g4]
            for j in range(4):
                k.tr(pt[:, j * 128:(j + 1) * 128], h[:, (g4 * 4 + j) * 128:(g4 * 4 + j + 1) * 128], ident.v())
            k.cp("act" if g4 else "dve", hT[:, g4 * 4:(g4 + 1) * 4, :], rr(pt.v(), "p (j t) -> p j t", j=4))
        for g4 in range(4):
            pt = pb[2 + g4 % 2]
            for j in range(4):
                c16 = g4 * 4 + j
                for dc in range(8):
                    k.mm(pt[:, j * 128:(j + 1) * 128], wq[:, dc, c16 * 128:(c16 + 1) * 128], hT[:, dc, :], start=(dc == 0), stop=(dc == 7))
            k.cp("act" if g4 % 2 else "dve", qT[:, g4 * 4:(g4 + 1) * 4, :], rr(pt.v(), "p (j t) -> p j t", j=4))
        for g4 in range(4):
            pt = pb[4 + g4 % 2]
            for j in range(4):
                c16 = g4 * 4 + j
                k.mm(pt[:, j * 128:(j + 1) * 128], qT[:, c16, :], keysT[:, c16, :])
            k.cp("act" if g4 % 2 else "dve", sc[:, g4 * 4:(g4 + 1) * 4, :], rr(pt.v(), "p (j t) -> p j t", j=4))
        for c16 in range(16):
            k.op("dve", lambda e: e.max(vals.h[:, c16, 0:8], sc.h[:, c16, :]), sc.res, vals.res)
            k.op("dve", lambda e: e.max_index(idxu.h[:, c16, 0:8], vals.h[:, c16, 0:8], sc.h[:, c16, :]), sc.res + vals.res, idxu.res)
            k.op("dve", lambda e: e.match_replace(wk.h[:], vals.h[:, c16, 0:8], sc.h[:, c16, :], -1e30), sc.res + vals.res, wk.res)
            k.op("dve", lambda e: e.max(vals.h[:, c16, 8:16], wk.h[:]), wk.res, vals.res)
            k.op("dve", lambda e: e.max_index(idxu.h[:, c16, 8:16], vals.h[:, c16, 8:16], wk.h[:]), wk.res + vals.res, idxu.res)
        k.cp("dve", idxf.v(), idxu.v())
        v4 = rr(vals.v(), "p (h q) a -> p h q a", q=2)
        i4 = rr(idxf.v(), "p (h q) a -> p h q a", q=2)
        v1 = v4[:, :, 0, :]; v2 = v4[:, :, 1, :]
        k.tt("dve", cand.v(), bc(V(v1.ap.unsqueeze(3), v1.res), [128, 8, 16, 16]), bc(V(v2.ap.unsqueeze(2), v2.res), [128, 8, 16, 16]), OP.add)
        for hh in range(8):
            cf = rr(cand[:, hh, :, :], "p a b -> p (a b)")
            k.op("dve", lambda e: e.max(best.h[:, hh, 0:8], cf.ap), cand.res, best.res)
            k.op("dve", lambda e: e.max_index(posu.h[:, hh, 0:8], best.h[:, hh, 0:8], cf.ap), cand.res + best.res, posu.res)
            k.op("dve", lambda e: e.match_replace(wk2.h[:], best.h[:, hh, 0:8], cf.ap, -1e30), cand.res + best.res, wk2.res)
            k.op("dve", lambda e: e.max(best.h[:, hh, 8:16], wk2.h[:]), wk2.res, best.res)
            k.op("dve", lambda e: e.max_index(posu.h[:, hh, 8:16], best.h[:, hh, 8:16], wk2.h[:]), wk2.res + best.res, posu.res)
        k.ts("dve", posu2[:, 0, :, :], posu.v(), cu32[:, 0:1], OP.bitwise_and)
        k.ts("dve", posu2[:, 1, :, :], posu.v(), cu32[:, 1:2], OP.logical_shift_right)
        k.cp("dve", pb_.v(), posu2[:, 0, :, :])
        k.cp("dve", pa_.v(), posu2[:, 1, :, :])
        io4 = bc(V(iota16.h[:].unsqueeze(1).unsqueeze(1), iota16.res), [128, 8, 16, 16])
        for (pp, isrc, dst) in ((pa_, i4[:, :, 0, :], eidf), (pb_, i4[:, :, 1, :], eid2)):
            k.tt("dve", oh.v(), io4, bc(V(pp.h[:].unsqueeze(3), pp.res), [128, 8, 16, 16]), OP.is_equal)
            k.tt("dve", oh.v(), oh.v(), bc(V(isrc.ap.unsqueeze(2), isrc.res), [128, 8, 16, 16]), OP.mult)
            k.red("dve", dst.v(), oh.v(), OP.add)
        k.tt("dve", gate.v(), best.v(), bc(best[:, :, 0:1], [128, 8, 16]), OP.subtract)
        k.act(gate.v(), gate.v(), AF.Exp)
        k.red("dve", gs.v(), gate.v(), OP.add)
        k.recip(gs.v(), gs.v())
        k.tt("dve", gate.v(), gate.v(), bc(V(gs.h[:].unsqueeze(2), gs.res), [128, 8, 16]), OP.mult)
        pt = pb[6]
        k.tr(pt[:, 0:128], rr(eidf.v(), "p h k -> p (h k)"), ident.v())
        k.tr(pt[:, 128:256], rr(eid2.v(), "p h k -> p (h k)"), ident.v())
        k.tr(pt[:, 256:384], rr(gate.v(), "p h k -> p (h k)"), ident.v())
        k.cp("act", rtT.v(), rr(pt[:, 0:384], "p (c t) -> p c t", c=3))
        k.dma(RTD[:, :, ti * 128:(ti + 1) * 128].rearrange("c p t -> p c t"), rtT.h[:], reads=rtT.res, writes=RT_res)
        k.cp("pool", hTb.v(), hT.v())
        k.dma(HFD[:, :, ti * 128:(ti + 1) * 128], hTb.h[:], reads=hTb.res, writes=HF_res)
    k.barrier()
    es.close()


class PrepState:
    pass


def prep_alloc(P, tile):
    ps_ = PrepState()
    ps_.uf = [tile("pp_uf%d" % i, [128, D]) for i in range(2)]
    ps_.vf = [tile("pp_vf%d" % i, [128, D]) for i in range(2)]
    ps_.ub = [tile("pp_ub%d" % i, [128, 8, 128], BF16) for i in range(2)]
    ps_.vb = [tile("pp_vb%d" % i, [128, D], BF16) for i in range(2)]
    return ps_


def prep_chunk(P, ps_, l, i, peer_u, peer_v, UTD, VD, PW_res, banks):
    k = P.k
    u_ = ps_.uf[i % 2]; v_ = ps_.vf[i % 2]; ub_ = ps_.ub[i % 2]; vb_ = ps_.vb[i % 2]
    k.ld(u_.v(), peer_u[l, i * 128:(i + 1) * 128, :])
    k.ld(v_.v(), peer_v[l, i * 128:(i + 1) * 128, :])
    for g4 in range(2):
        pt = banks[(i * 2 + g4) % len(banks)]
        for j in range(4):
            dc = g4 * 4 + j
            k.tr(pt[:, j * 128:(j + 1) * 128], u_[:, dc * 128:(dc + 1) * 128], P.ident.v())
        k.cp("act" if g4 else "dve", ub_[:, g4 * 4:(g4 + 1) * 4, :], rr(pt.v(), "p (j e) -> p j e", j=4))
    k.dma(UTD[i], ub_.h[:], reads=ub_.res, writes=PW_res)
    k.cp("pool", vb_.v(), v_.v())
    k.dma(VD[i], vb_.h[:], reads=vb_.res, writes=PW_res)


def phase_peer_prep(P, l, peer_u, peer_v, UTD, VD, PW_res):
    k = P.k
    nc = P.nc
    es = ExitStack()
    tile = lambda name, shape, dt=F32: T(es.enter_context(usb(nc, "t_" + name, shape, dt)), name)
    ps_ = prep_alloc(P, tile)
    for i in range(128):
        prep_chunk(P, ps_, l, i, peer_u, peer_v, UTD, VD, PW_res, P.pb[0:4])
    k.barrier()
    es.close()


def phase_peer_dense(P, l, prm, UTD, VD, PW_res, RTD, RT_res, HFD, HF_res, XD, XD_res, dst_ap, MODD, MOD_res, need_ctx=True):
    import os
    k = P.k
    nc = P.nc
    es = ExitStack()
    tile = lambda name, shape, dt=F32: T(es.enter_context(usb(nc, "t_" + name, shape, dt)), name)
    pb = P.pb
    GT = 256
    mods = {}
    for s in ([0, 1] if need_ctx else [0]):
        mods[s] = tile("pd_gf%d" % s, [128, D])
        k.dma(mods[s].h[:], MODD[s, MOD_GF], reads=MOD_res, writes=mods[s].res)
    lng = tile("pd_lng", [128, D]); k.ld(lng.v(), prm["ln_ffn_g"][l])
    lnb = tile("pd_lnb", [128, D]); k.ld(lnb.v(), prm["ln_ffn_b"][l])
    iota = tile("pd_iota", [128, 128]); k.ld(iota.v(), P.din["c_iota128"])
    G = tile("pd_G", [128, GT, 128], BF16)
    rt = tile("pd_rt", [128, 3, GT])
    hfg = tile("pd_hfg", [128, 8, GT], BF16)
    qtb = tile("pd_qtb", [128, 32, 128], BF16)
    amt = tile("pd_amt", [128, 32, 128], BF16)
    amb = tile("pd_amb", [128, 32, 128], BF16)
    NBUF = 6
    utp = [tile("pd_ut%d" % i, [128, 2, 8, 128], BF16) for i in range(NBUF)]
    vp = [tile("pd_vp%d" % i, [128, 2, D], BF16) for i in range(NBUF)]
    ga = [tile("pd_ga%d" % i, [128, 2, GT]) for i in range(2)]
    GA = [tile("pd_GA%d" % i, [128, 2, GT], BF16) for i in range(2)]
    x = tile("pd_x", [128, D]); acc = tile("pd_acc", [128, D]); st = tile("pd_st", [128, 16])
    g_start = 0 if need_ctx else NCTX
    NG = int(os.environ.get("PEER_GROUPS", 99))
    NCP = int(os.environ.get("PEER_CP", 64))
    groups = list(range(g_start, NT, GT))[:NG]
    io3 = bc(V(iota.h[:].unsqueeze(1), iota.res), [128, 32, 128])
    pA = [pb[0], pb[1]]; pO = [pb[2], pb[3], pb[4], pb[5]]; pG = [pb[6], pb[7]]
    for g0 in groups:
        k.dma(rt.h[:], RTD[:, :, g0:g0 + GT].rearrange("c p t -> p c t"), reads=RT_res, writes=rt.res)
        k.dma(hfg.h[:], HFD[:, :, g0:g0 + GT], reads=HF_res, writes=hfg.res)
        for blk in range(GT // 32):
            ts_ = slice(blk * 32, blk * 32 + 32)
            B3 = lambda v: bc(V(v.ap.unsqueeze(2), v.res), [128, 32, 128])
            k.tt("dve", qtb.v(), io3, B3(rt[:, 1, ts_]), OP.is_equal)
            k.tt("dve", amt.v(), io3, B3(rt[:, 0, ts_]), OP.is_equal)
            k.tt("pool", amb.v(), amt.v(), B3(rt[:, 2, ts_]), OP.mult)
            for q4 in range(8):
                pg = pG[q4 % 2]
                for t4 in range(4):
                    tt_ = q4 * 4 + t4
                    k.mm(pg[:, t4 * 128:(t4 + 1) * 128], qtb[:, tt_, :], amb[:, tt_, :])
                k.cp("act" if q4 % 2 else "dve", G[:, blk * 32 + q4 * 4:blk * 32 + q4 * 4 + 4, :], rr(pg.v(), "p (t i) -> p t i", t=4))

        PF = NBUF - 2

        def emit_dma(cp):
            u_ = utp[cp % NBUF]; v_ = vp[cp % NBUF]
            k.dma(u_.h[:], UTD[2 * cp:2 * cp + 2].rearrange("c d k e -> d c k e"), reads=PW_res, writes=u_.res)
            k.dma(v_.h[:], VD[2 * cp:2 * cp + 2].rearrange("c e n -> e c n"), reads=PW_res, writes=v_.res)

        def emit_A(cp):
            u_ = utp[cp % NBUF]
            pa = pA[cp % 2]
            for cc in range(2):
                for dc in range(8):
                    k.mm(pa[:, cc * GT:(cc + 1) * GT], u_[:, cc, dc, :], hfg[:, dc, :], start=(dc == 0), stop=(dc == 7))

        for cp in range(min(PF + 1, NCP)):
            emit_dma(cp)
        emit_A(0)
        for cp in range(NCP):
            if cp + PF + 1 < NCP:
                emit_dma(cp + PF + 1)
            if cp + 1 < NCP:
                emit_A(cp + 1)
            pa = pA[cp % 2]; ga_ = ga[cp % 2]; GA_ = GA[cp % 2]; v_ = vp[cp % NBUF]
            k.act(rr(ga_.v(), "p c t -> p (c t)"), pa.v(), AF.Gelu_apprx_tanh)
            k.tt("dve", GA_.v(), ga_.v(), V(G.h[:, :, 2 * cp:2 * cp + 2].rearrange("j t i -> j i t"), G.res), OP.mult)
            for tt_ in range(GT // 128):
                for half in range(2):
                    for cc in range(2):
                        k.mm(pO[tt_ * 2 + half].v(), GA_[:, cc, tt_ * 128:(tt_ + 1) * 128], v_[:, cc, half * 512:(half + 1) * 512],
                             start=(cp == 0 and cc == 0), stop=(cp == NCP - 1 and cc == 1))
        for tt_ in range(GT // 128):
            ti = g0 // 128 + tt_
            s = 1 if ti < NCTX // 128 else 0
            k.dma(x.h[:], XD[ti * 128:(ti + 1) * 128, :], reads=XD_res[ti], writes=x.res)
            for half in range(2):
                k.tt("dve", acc[:, half * 512:(half + 1) * 512], pO[tt_ * 2 + half].v(), mods[s][:, half * 512:(half + 1) * 512], OP.mult)
            if "peer_y" in P.debug:
                for half in range(2):
                    k.cp("act", x[:, half * 512:(half + 1) * 512], pO[tt_ * 2 + half].v())
                k.dma(P.dout["peer_y"][ti * 128:(ti + 1) * 128, :], x.h[:], reads=x.res)
                k.dma(x.h[:], XD[ti * 128:(ti + 1) * 128, :], reads=XD_res[ti], writes=x.res)
            k.stt("dve", acc.v(), x.v(), ALPHA, acc.v(), OP.mult, OP.add)
            mean, rstd, nmr = ln_stats(P, acc.v(), st)
            k.act(acc.v(), acc.v(), AF.Identity, bias=nmr, scale=rstd)
            k.tt("pool", acc.v(), acc.v(), lng.v(), OP.mult)
            k.tt("dve", acc.v(), acc.v(), lnb.v(), OP.add)
            d_ap, d_res = dst_ap(ti)
            k.dma(d_ap, acc.h[:], reads=acc.res, writes=d_res)
    k.barrier()
    es.close()


PARAM_SHAPES = {
    "ml_cw": [2, 128, 4, 4], "ml_gb": [2, 128, 16], "ml_ng": [2, 128, 256],
    "rw_muT": [2, 128, 12, 2], "rw_wup": [2, 128, 384], "rw_aup": [2, 128, 384], "rw_gup": [2, 128, 384],
    "rw_cols": [2, 128, 3, 9], "mla_gq": [2, 128, 384], "mla_gkv": [2, 128, 256],
    "ln_mix_g": [2, 128, D], "ln_mix_b": [2, 128, D], "ln_ffn_g": [2, 128, D], "ln_ffn_b": [2, 128, D],
}
WEIGHT_SHAPES = {
    "w_mod": [2, D, 6 * D], "b_mod": [2, 6 * D], "w_in": [2, D, N_IN], "w_out": [2, D, D],
    "peer_w_q": [2, D, 2048], "peer_keys": [2, 8, 2, 128, 128], "peer_u": [2, 16384, D], "peer_v": [2, 16384, D],
    "mla_q_up": [2, 384, 576], "mla_kv_up": [2, 256, 768],
}


def build_program(debug=(), layers=(0, 1), stop_after=None, peer_mode="dense"):
    P = Prog(debug)
    setup_common(P)
    k = P.k
    nc = P.nc
    W = {n: P.inp(n, s) for n, s in WEIGHT_SHAPES.items()}
    prm = {n: P.inp(n, s) for n, s in PARAM_SHAPES.items()}
    prm["mla_q_up"] = W["mla_q_up"]; prm["mla_kv_up"] = W["mla_kv_up"]
    XD = P.scratch("XD1", [NT, D]); XD_res = [[Res("XD%d" % i)] for i in range(NTILE)]
    MODD = P.scratch("MODD", [2, 6, 128, D]); MOD_res = [Res("MODD")]
    PRW = P.scratch("PRW", [12, 128, NT]); PRW_res = [Res("PRW")]
    MIXT = P.scratch("MIXT", [8, 128, NT]); MIXT_res = [Res("MIXT")]
    OUT = P.outp("out", [NT - NCTX, D])
    P.peer_mode = peer_mode
    UTD = P.scratch("UTD", [128, 128, 8, 128], BF16); VD = P.scratch("VD", [128, 128, D], BF16); PW_res = [Res("PW")]
    RTD = P.scratch("RTD", [3, 128, NT]); RT_res = [Res("RT")]
    HFD = P.scratch("HFD", [128, 8, NT], BF16); HF_res = [Res("HF")]
    for l in layers:
        need_ctx = (l == 0)
        first = (l == layers[0])
        src = P.xin if first else XD
        src_res = [[] for _ in range(NTILE)] if first else XD_res
        phase_mod(P, l, W["w_mod"], W["b_mod"], MODD, MOD_res)
        es1 = ExitStack()
        hT = T(es1.enter_context(usb(nc, "t_hT_l%d" % l, [128, 8, NT], BF16)), "hT")
        phase_hT(P, src, src_res, MODD, MOD_res, MOD_SHA, MOD_SCA, hT)
        phase_mlstm(P, l, hT, W["w_in"], prm, MIXT, MIXT_res)
        phase_rwkv_proj(P, l, hT, W["w_in"], prm, PRW, PRW_res)
        phase_mla(P, l, hT, W["w_in"], prm, MIXT, MIXT_res, need_ctx)
        es1.close()
        phase_rwkv(P, l, prm, PRW, PRW_res, MIXT, MIXT_res)
        if stop_after == "mix":
            break
        phase_mixout(P, l, W["w_out"], prm, MIXT, MIXT_res, src, src_res, XD, XD_res, MODD, MOD_res, need_ctx)
        if stop_after == "mixout":
            break
        if l == 1:
            dst = lambda ti: (OUT[(ti - NCTX // 128) * 128:(ti - NCTX // 128 + 1) * 128, :], [])
        else:
            dst = lambda ti: (XD[ti * 128:(ti + 1) * 128, :], XD_res[ti])
        if P.peer_mode == "gather":
            phase_peer(P, l, prm, W["peer_w_q"], W["peer_keys"], W["peer_u"], W["peer_v"], XD, XD_res, dst, MODD, MOD_res, need_ctx)
        else:
            phase_peer_route(P, l, prm, W["peer_w_q"], W["peer_keys"], XD, XD_res, MODD, MOD_res, RTD, RT_res, HFD, HF_res, need_ctx,
                             prep=(W["peer_u"], W["peer_v"], UTD, VD, PW_res))
            phase_peer_dense(P, l, prm, UTD, VD, PW_res, RTD, RT_res, HFD, HF_res, XD, XD_res, dst, MODD, MOD_res, need_ctx)
    k.wait_all("sp")
    P.es.close()
    return P


def make_in_maps(inputs, n_cores=8):
    consts = host_consts()
    hp = host_params(inputs)
    maps = []
    for b in range(n_cores):
        m = dict(consts)
        m.update(hp)
        for n in WEIGHT_SHAPES:
            m[n] = np.ascontiguousarray(inputs[n])
        m["xin"] = np.ascontiguousarray(np.concatenate([inputs["ctx"][b], inputs["x"][b]], 0))
        cv = np.stack([inputs["c"][b], inputs["c_ctx"]], -1)
        m["cvec"] = np.ascontiguousarray(cv.reshape(8, 128, 2).transpose(1, 0, 2))
        maps.append(m)
    return maps


_CACHE = {}


def kernel(**inputs):
    from concourse.bass_utils import run_bass_kernel_spmd
    inputs = {k_: np.asarray(v) for k_, v in inputs.items()}
    if "P" not in _CACHE:
        _CACHE["P"] = build_program()
    P = _CACHE["P"]
    maps = make_in_maps(inputs, 8)
    maps = [{kk: v for kk, v in m.items() if kk in P.din} for m in maps]
    res = run_bass_kernel_spmd(P.nc, maps, core_ids=list(range(8)))
    out = np.stack([np.asarray(res.results[b]["out"]) for b in range(8)], 0).astype(np.float32)
    return out
```
